# Optimizing a Trainium2 kernel written in Bass

```python
import math
import jax, jax.numpy as jnp
from jax import lax
import numpy as np

D_MODEL = 1024
BATCH = 16
SEQ = 4096
DEPTH = 2

N_MIXERS = 2
DA_HEADS = 8
DA_HEAD_DIM = 64
DA_VDIM = 2 * DA_HEAD_DIM
Q_BLOCK = 128
REL_BUCKETS = 32
REL_MAX_DIST = 128
SG_CHUNK = 128
SG_GROUPS = 8
SG_HALF = 3 * D_MODEL
SG_GROUP_DIM = SG_HALF // SG_GROUPS
MOE_GROUPS = 8
MOE_PER_GROUP = 8
MOE_EXPERTS = MOE_GROUPS * MOE_PER_GROUP
MOE_TOPK = 2
MOE_HIDDEN = D_MODEL // 2
MOE_BLOCK = 128
LN_EPS = 1e-5
DN_ALPHA = (2 * DEPTH) ** 0.25
DN_BETA = (8 * DEPTH) ** -0.25
N_ATTN_LAYERS = (DEPTH + 1) // 2
N_SG_LAYERS = DEPTH // 2

kernel_name = "hybrid_diffattn_gmlp_hmoe_deepnorm"


def layer_norm(x, g, b):
    xf = x.astype(jnp.float32)
    mu = jnp.mean(xf, axis=-1, keepdims=True)
    var = jnp.mean(jnp.square(xf - mu), axis=-1, keepdims=True)
    y = (xf - mu) * lax.rsqrt(var + LN_EPS)
    return (y * g.astype(jnp.float32) + b.astype(jnp.float32)).astype(x.dtype)


def rms_norm(x, g):
    xf = x.astype(jnp.float32)
    y = xf * lax.rsqrt(jnp.mean(jnp.square(xf), axis=-1, keepdims=True) + LN_EPS)
    return (y * g.astype(jnp.float32)).astype(x.dtype)


def t5_bucket(dist):
    n = jnp.maximum(dist, 0)
    max_exact = REL_BUCKETS // 2
    nf = jnp.maximum(n, 1).astype(jnp.float32)
    large = max_exact + (jnp.log(nf / max_exact) / math.log(REL_MAX_DIST / max_exact)
                         * (REL_BUCKETS - max_exact)).astype(jnp.int32)
    large = jnp.minimum(large, REL_BUCKETS - 1)
    return jnp.where(n < max_exact, n, large)


def diff_attention(x, w_in, w_out, lam_vecs, subln_g, rel_table, lam_init):
    B, S, _ = x.shape
    H, d = DA_HEADS, DA_HEAD_DIM
    qk_w = H * 2 * d
    qkv = x @ w_in
    q = qkv[..., :qk_w].reshape(B, S, H, 2, d).transpose(0, 2, 3, 1, 4) * (d ** -0.5)
    k = qkv[..., qk_w:2 * qk_w].reshape(B, S, H, 2, d).transpose(0, 2, 3, 1, 4)
    v = qkv[..., 2 * qk_w:].reshape(B, S, H, DA_VDIM).transpose(0, 2, 1, 3)
    lv = lam_vecs.astype(jnp.float32)
    lam = jnp.exp(jnp.sum(lv[0] * lv[1])) - jnp.exp(jnp.sum(lv[2] * lv[3])) + lam_init
    nq = S // Q_BLOCK
    q_blocks = jnp.moveaxis(q.reshape(B, H, 2, nq, Q_BLOCK, d), 3, 0)
    kpos = jnp.arange(S, dtype=jnp.int32)

    def block_fn(args):
        qb, i = args
        qpos = i * Q_BLOCK + jnp.arange(Q_BLOCK, dtype=jnp.int32)
        dist = qpos[:, None] - kpos[None, :]
        bias = jnp.transpose(rel_table[t5_bucket(dist)], (2, 0, 1))
        logits = jnp.einsum('bhmqd,bhmkd->bhmqk', qb, k).astype(jnp.float32)
        logits = logits + bias[None, :, None].astype(jnp.float32)
        logits = jnp.where((dist >= 0)[None, None, None], logits, -jnp.inf)
        p = jax.nn.softmax(logits, axis=-1)
        attn = p[:, :, 0] - lam * p[:, :, 1]
        return jnp.einsum('bhqk,bhke->bhqe', attn.astype(v.dtype), v)

    o = lax.map(block_fn, (q_blocks, jnp.arange(nq, dtype=jnp.int32)))
    o = jnp.moveaxis(o, 0, 2).reshape(B, H, S, DA_VDIM)
    o = rms_norm(o, subln_g) * (1.0 - lam_init)
    o = o.transpose(0, 2, 1, 3).reshape(B, S, H * DA_VDIM)
    return o @ w_out


def spatial_gating(x, w_in, b_in, ln_g, ln_b, w_s, b_s, w_out):
    B, S, _ = x.shape
    z = jax.nn.gelu(x @ w_in + b_in)
    u, v = z[..., :SG_HALF], z[..., SG_HALF:]
    v = layer_norm(v, ln_g, ln_b)
    nc = S // SG_CHUNK
    v = v.reshape(B, nc, SG_CHUNK, SG_GROUPS, SG_GROUP_DIM)
    causal = jnp.tril(jnp.ones((SG_CHUNK, SG_CHUNK), dtype=bool))
    w = jnp.where(causal[None], w_s, jnp.zeros_like(w_s))
    s = jnp.einsum('gts,bnsgc->bntgc', w, v) + jnp.transpose(b_s)[:, :, None]
    s = s.reshape(B, S, SG_HALF)
    return (u * s) @ w_out


def hier_moe(x, w_group, b_group, w_router, b_router, w_gate, w_up, w_down):
    B, S, D = x.shape
    N = B * S
    xt = x.reshape(N, D)
    g_logits = (xt @ w_group).astype(jnp.float32) + b_group.astype(jnp.float32)
    g_prob = jax.nn.softmax(g_logits, axis=-1)
    g_idx = jnp.argmax(g_logits, axis=-1).astype(jnp.int32)
    g_gate = jnp.take_along_axis(g_prob, g_idx[:, None], axis=-1)
    e_logits = ((xt @ w_router).astype(jnp.float32) + b_router.astype(jnp.float32))
    e_logits = e_logits.reshape(N, MOE_GROUPS, MOE_PER_GROUP)
    e_logits = jnp.take_along_axis(e_logits, g_idx[:, None, None], axis=1)[:, 0]
    top_val, top_idx = lax.top_k(e_logits, MOE_TOPK)
    gates = g_gate * jax.nn.softmax(top_val, axis=-1)
    expert = g_idx[:, None] * MOE_PER_GROUP + top_idx.astype(jnp.int32)

    A = N * MOE_TOPK
    a_exp = expert.reshape(A)
    a_tok = jnp.arange(A, dtype=jnp.int32) // MOE_TOPK
    a_gate = gates.reshape(A)
    order = jnp.argsort(a_exp)
    s_exp = a_exp[order]
    counts = jnp.zeros((MOE_EXPERTS,), jnp.int32).at[a_exp].add(1)
    padded = (counts + MOE_BLOCK - 1) // MOE_BLOCK * MOE_BLOCK
    starts = jnp.cumsum(counts) - counts
    pends = jnp.cumsum(padded)
    pstarts = pends - padded
    dest = pstarts[s_exp] + jnp.arange(A, dtype=jnp.int32) - starts[s_exp]
    n_blocks = -(-A // MOE_BLOCK) + MOE_EXPERTS
    P = n_blocks * MOE_BLOCK
    row_tok = jnp.full((P,), N, jnp.int32).at[dest].set(a_tok[order])
    row_gate = jnp.zeros((P,), jnp.float32).at[dest].set(a_gate[order])
    block_start = jnp.arange(n_blocks, dtype=jnp.int32) * MOE_BLOCK
    block_exp = jnp.minimum(jnp.searchsorted(pends, block_start, side='right'),
                            MOE_EXPERTS - 1).astype(jnp.int32)
    x_pad = jnp.concatenate([xt, jnp.zeros((1, D), xt.dtype)], axis=0)
    rows = x_pad[row_tok].reshape(n_blocks, MOE_BLOCK, D)

    def expert_block(args):
        r, e = args
        h = jax.nn.silu(r @ w_gate[e]) * (r @ w_up[e])
        return h @ w_down[e]

    y = lax.map(expert_block, (rows, block_exp)).reshape(P, D)
    out = jnp.zeros((N + 1, D), y.dtype).at[row_tok].add(y * row_gate[:, None].astype(y.dtype))
    return out[:N].reshape(B, S, D)


def setup_inputs(seed: int = 0) -> dict:
    key = jax.random.key(seed)
    ks = jax.random.split(key, 24)
    f32 = jnp.float32
    nrm = lambda k, shape, s: jax.random.normal(k, shape, f32) * s
    D, H, d = D_MODEL, DA_HEADS, DA_HEAD_DIM
    NA, NB, L = N_ATTN_LAYERS, N_SG_LAYERS, DEPTH
    return {
        "x": nrm(ks[0], (BATCH, SEQ, D), 1.0),
        "rel_bias": nrm(ks[1], (REL_BUCKETS, H), 0.5),
        "attn_w_in": nrm(ks[2], (NA, D, 2 * H * 2 * d + H * DA_VDIM), D ** -0.5),
        "attn_w_out": nrm(ks[3], (NA, H * DA_VDIM, D), (H * DA_VDIM) ** -0.5 * DN_BETA),
        "attn_lambda": nrm(ks[4], (NA, 4, d), 0.1),
        "attn_subln_g": 1.0 + nrm(ks[5], (NA, DA_VDIM), 0.02),
        "sg_w_in": nrm(ks[6], (NB, D, 2 * SG_HALF), D ** -0.5),
        "sg_b_in": nrm(ks[7], (NB, 2 * SG_HALF), 0.02),
        "sg_ln_g": 1.0 + nrm(ks[8], (NB, SG_HALF), 0.02),
        "sg_ln_b": nrm(ks[9], (NB, SG_HALF), 0.02),
        "sg_w_s": nrm(ks[10], (NB, SG_GROUPS, SG_CHUNK, SG_CHUNK), SG_CHUNK ** -0.5),
        "sg_b_s": 1.0 + nrm(ks[11], (NB, SG_GROUPS, SG_CHUNK), 0.1),
        "sg_w_out": nrm(ks[12], (NB, SG_HALF, D), SG_HALF ** -0.5 * DN_BETA),
        "moe_w_group": nrm(ks[13], (L, D, MOE_GROUPS), D ** -0.5),
        "moe_b_group": nrm(ks[14], (L, MOE_GROUPS), 0.01),
        "moe_w_router": nrm(ks[15], (L, D, MOE_EXPERTS), D ** -0.5),
        "moe_b_router": nrm(ks[16], (L, MOE_EXPERTS), 0.01),
        "moe_w_gate": nrm(ks[17], (L, MOE_EXPERTS, D, MOE_HIDDEN), D ** -0.5),
        "moe_w_up": nrm(ks[18], (L, MOE_EXPERTS, D, MOE_HIDDEN), D ** -0.5),
        "moe_w_down": nrm(ks[19], (L, MOE_EXPERTS, MOE_HIDDEN, D), MOE_HIDDEN ** -0.5 * DN_BETA),
        "ln_g": 1.0 + nrm(ks[20], (L, 2, D), 0.02),
        "ln_b": nrm(ks[21], (L, 2, D), 0.02),
    }


def reference(x, rel_bias, attn_w_in, attn_w_out, attn_lambda, attn_subln_g,
              sg_w_in, sg_b_in, sg_ln_g, sg_ln_b, sg_w_s, sg_b_s, sg_w_out,
              moe_w_group, moe_b_group, moe_w_router, moe_b_router,
              moe_w_gate, moe_w_up, moe_w_down, ln_g, ln_b):
    h = x
    for i in range(DEPTH):
        j = i // N_MIXERS
        if i % N_MIXERS == 0:
            lam_init = 0.8 - 0.6 * math.exp(-0.3 * i)
            mix = diff_attention(h, attn_w_in[j], attn_w_out[j], attn_lambda[j],
                                 attn_subln_g[j], rel_bias, lam_init)
        else:
            mix = spatial_gating(h, sg_w_in[j], sg_b_in[j], sg_ln_g[j], sg_ln_b[j],
                                 sg_w_s[j], sg_b_s[j], sg_w_out[j])
        h = layer_norm(DN_ALPHA * h + mix, ln_g[i, 0], ln_b[i, 0])
        ffn = hier_moe(h, moe_w_group[i], moe_b_group[i], moe_w_router[i], moe_b_router[i],
                       moe_w_gate[i], moe_w_up[i], moe_w_down[i])
        h = layer_norm(DN_ALPHA * h + ffn, ln_g[i, 1], ln_b[i, 1])
    return h
```

```python
import math
import numpy as np
import concourse.bass as bass
import concourse.mybir as mybir
from concourse.bass_utils import run_bass_kernel_spmd
from concourse.alu_op_type import AluOpType as ALU

AF = mybir.ActivationFunctionType
F32, BF16, I32, U32 = mybir.dt.float32, mybir.dt.bfloat16, mybir.dt.int32, mybir.dt.uint32

D = 1024
NH = 8
NE = 64
HID = 512
SGH = 3072
LN_EPS = 1e-5
DEPTH = 2
DN_ALPHA = (2 * DEPTH) ** 0.25
N_CORES = 8


class Buf:
    __slots__ = ("name", "w", "r")

    def __init__(self, name=""):
        self.name = name
        self.w = None
        self.r = {}


class Prog:
    NDMA = {"sp": 24, "act": 4, "pool": 24}

    def __init__(self):
        nc = self.nc = bass.Bass("TRN2", target_bir_lowering=False)
        self.engs = {"pe": nc.tensor, "act": nc.scalar, "dve": nc.vector,
                     "pool": nc.gpsimd, "sp": nc.sync}
        self.sems = {}
        self.count = {}
        for e in self.engs:
            self.sems[e] = nc.alloc_semaphore(name="c_" + e)
            self.count[e] = 0
        self.dma_pool = {}
        self.dma_rr = {}
        self.dma_uses = {}
        for q, n in self.NDMA.items():
            keys = []
            for i in range(n):
                k = "d_%s_%d" % (q, i)
                self.sems[k] = nc.alloc_semaphore(name=k)
                self.dma_uses[k] = 0
                keys.append(k)
            self.dma_pool[q] = keys
            self.dma_rr[q] = 0
        self.known = {e: {} for e in self.engs}
        self.n_inst = 0
        self._names = 0

    def sb(self, name, shape, dt):
        self._names += 1
        return self.nc.alloc_sbuf_tensor("%s_%d" % (name, self._names), list(shape), dt)

    def ps(self, name, shape, dt=F32):
        self._names += 1
        return self.nc.alloc_psum_tensor("%s_%d" % (name, self._names), list(shape), dt)

    def _wait(self, eng, key, val):
        if val <= 0:
            return
        kn = self.known[eng]
        if kn.get(key, 0) >= val:
            return
        self.engs[eng].wait_ge(self.sems[key], val)
        kn[key] = val

    def _deps(self, eng, reads, writes):
        deps = {}

        def add(tok, raw):
            if tok is None:
                return
            k, v = tok
            if k == eng and (not raw or eng == "pe"):
                return
            if deps.get(k, 0) < v:
                deps[k] = v
        for b in reads:
            add(b.w, True)
        for b in writes:
            add(b.w, False)
            for k, v in b.r.items():
                add((k, v), False)
        return deps

    def _mark(self, tok, reads, writes):
        k, v = tok
        for b in reads:
            if b.r.get(k, 0) < v:
                b.r[k] = v
        for b in writes:
            b.w = tok
            b.r = {}

    def op(self, eng, fn, reads=(), writes=()):
        for k, v in self._deps(eng, reads, writes).items():
            self._wait(eng, k, v)
        ins = fn(self.engs[eng])
        self.count[eng] += 1
        ins.then_inc(self.sems[eng], 1)
        tok = (eng, self.count[eng])
        self._mark(tok, reads, writes)
        self.n_inst += 1
        return tok

    def dma(self, q, fn, reads=(), writes=()):
        for k, v in self._deps(q, reads, writes).items():
            self._wait(q, k, v)
        pool = self.dma_pool[q]
        key = pool[self.dma_rr[q] % len(pool)]
        self.dma_rr[q] += 1
        prior = self.dma_uses[key]
        self._wait(q, key, 16 * prior)
        ins = fn(self.engs[q])
        ins.then_inc(self.sems[key], 16)
        self.dma_uses[key] = prior + 1
        tok = (key, 16 * (prior + 1))
        self._mark(tok, reads, writes)
        self.n_inst += 1
        return tok

    def barrier(self):
        for e in self.engs:
            for e2 in self.engs:
                if e2 != e:
                    self._wait(e, e2, self.count[e2])
            for k, n in self.dma_uses.items():
                self._wait(e, k, 16 * n)

    def finish(self):
        for e in self.engs:
            if e != "sp":
                self._wait("sp", e, self.count[e])
        for k, n in self.dma_uses.items():
            self._wait("sp", k, 16 * n)


class Ring:
    def __init__(self, P, name, n, shape, dt, psum=False):
        self.slots = []
        for i in range(n):
            t = P.ps(name, shape, dt) if psum else P.sb(name, shape, dt)
            self.slots.append((t, Buf(name)))
        self.i = 0

    def next(self):
        s = self.slots[self.i % len(self.slots)]
        self.i += 1
        return s


class K:
    def __init__(self, nseq=2, S=4096, C=384, test_outputs=(), lite=False):
        self.nseq, self.S, self.C = nseq, S, C
        self.lite = lite
        self.NT = nseq * S
        self.NTL = self.NT // 128
        self.P = Prog()
        self.nc = self.P.nc
        self.test_outputs = set(test_outputs)
        self._declare_dram()

    def _dt(self, name, shape, dt, kind=None):
        if kind is None:
            kind = "ExternalOutput" if name in self.test_outputs else "Internal"
        return self.nc.dram_tensor(name, list(shape), dt, kind=kind)

    def _declare_dram(self):
        NT, S, nseq, C = self.NT, self.S, self.nseq, self.C
        i = lambda n, s: self._dt(n, s, F32, kind="ExternalInput")
        self.x = i("x", [NT, D])
        self.bt = i("bt", [2, NH, 128, 128])
        self.cfar = i("cfar", [1, NH])
        self.a_w_in = i("attn_w_in", [D, 3 * D])
        self.a_w_out = i("attn_w_out", [D, D])
        self.a_lam = i("attn_lambda", [1, 256])
        self.a_g = i("attn_subln_g", [1, 128])
        self.s_w_in = i("sg_w_in", [D, 2 * SGH])
        self.s_b_in = i("sg_b_in", [1, 2 * SGH])
        self.s_ln_g = i("sg_ln_g", [1, SGH])
        self.s_ln_b = i("sg_ln_b", [1, SGH])
        self.s_w_s = i("sg_w_s", [8, 128, 128])
        self.s_b_s = i("sg_b_s", [8, 128])
        self.s_w_out = i("sg_w_out", [SGH, D])
        self.m_wr = i("moe_wr", [2, D, 72])
        self.m_br = i("moe_br", [2, 72])
        ne = 1 if self.lite else NE
        self.m_wg = i("moe_w_gate", [2, ne, D, HID])
        self.m_wu = i("moe_w_up", [2, ne, D, HID])
        self.m_wd = i("moe_w_down", [2, ne, HID, D])
        self.ln_g = i("ln_g", [2, 2, D])
        self.ln_b = i("ln_b", [2, 2, D])
        self.out = self._dt("out", [NT, D], F32, kind="ExternalOutput")
        self.QKT = self._dt("QKT", [nseq, 16, 128, S], BF16)
        self.V = self._dt("V", [nseq, S, D], BF16)
        self.OT = self._dt("OT", [nseq, NH, 128, S], BF16)
        self.H1 = self._dt("H1", [NT, D], F32)
        self.H2 = self._dt("H2", [NT, D], F32)
        self.H3 = self._dt("H3", [NT, D], F32)
        self.XS = self._dt("XS", [NE * C, D], BF16)
        self.YS = self._dt("YS", [NE * C, D], BF16)
        self.b_QKT = [Buf() for _ in range(nseq)]
        self.b_V = [Buf() for _ in range(nseq)]
        self.b_OT = [Buf() for _ in range(nseq)]
        self.b_H = {n: Buf(n) for n in ("H1", "H2", "H3", "out")}
        self.b_XS = Buf("XS")
        self.b_YS = Buf("YS")

    def setup(self):
        P = self.P
        self.zero_reg = self.nc.gpsimd.to_reg(0.0)
        self.ident = P.sb("ident", [128, 128], BF16); self.b_const = Buf("const")
        bc = self.b_const
        P.op("pool", lambda e: e.memset(self.ident[:], 1.0), writes=[bc])
        P.op("pool", lambda e: e.affine_select(out=self.ident[:], in_=self.ident[:], pattern=[[-1, 128]],
                                                compare_op=ALU.is_equal, fill=self.zero_reg, base=0, channel_multiplier=1),
             reads=[bc], writes=[bc])
        self.ones = P.sb("ones", [128, 128], BF16)
        P.op("pool", lambda e: e.memset(self.ones[:], 1.0), writes=[bc])
        self.ustrict = P.sb("ustrict", [128, 128], BF16)
        P.op("pool", lambda e: e.memset(self.ustrict[:], 1.0), writes=[bc])
        P.op("pool", lambda e: e.affine_select(out=self.ustrict[:], in_=self.ustrict[:], pattern=[[1, 128]],
                                                compare_op=ALU.is_gt, fill=self.zero_reg, base=0, channel_multiplier=-1),
             reads=[bc], writes=[bc])
        self.iota_i = P.sb("iota_i", [128, NE], I32)
        self.iota_e = P.sb("iota_e", [128, NE], F32)
        P.op("pool", lambda e: e.iota(self.iota_i[:], pattern=[[1, NE]], base=0, channel_multiplier=0), writes=[bc])
        P.op("dve", lambda e: e.tensor_copy(out=self.iota_e[:], in_=self.iota_i[:]), reads=[bc], writes=[bc])
        self.mhalf = P.sb("mhalf", [128, 1], F32)
        P.op("dve", lambda e: e.memset(self.mhalf[:], -0.5), writes=[bc])

    def setup_attn(self):
        P = self.P
        bc = self.b_attn_c = Buf("attn_c")
        lam_init = 0.8 - 0.6 * math.exp(-0.3 * 0)
        self.cb = P.sb("cb", [128, NH], F32)
        self.ncb = P.sb("ncb", [128, NH], F32)
        P.dma("sp", lambda e: e.dma_start(out=self.cb[:], in_=self.cfar[0].partition_broadcast(128)), writes=[bc])
        P.op("dve", lambda e: e.tensor_scalar(out=self.ncb[:], in0=self.cb[:], scalar1=-1.0, scalar2=None, op0=ALU.mult),
             reads=[bc], writes=[bc])
        btf = P.sb("btf", [128, 2, NH, 128], F32)
        self.Eb = P.sb("Eb", [128, 2, NH, 128], BF16)
        P.dma("sp", lambda e: e.dma_start(out=btf[:], in_=self.bt.ap().rearrange("d h k q -> k d h q")), writes=[bc])
        for d in range(2):
            for h in range(NH):
                P.op("act", lambda e: e.activation(out=btf[:, d, h, :], in_=btf[:, d, h, :], func=AF.Exp,
                                                   bias=self.ncb[:, h:h + 1], scale=1.0), reads=[bc], writes=[bc])
        for h in range(NH):
            P.op("pool", lambda e: e.affine_select(out=btf[:, 0, h, :], in_=btf[:, 0, h, :], pattern=[[1, 128]],
                                                    compare_op=ALU.is_ge, fill=self.zero_reg, base=0, channel_multiplier=-1),
                 reads=[bc], writes=[bc])
        P.op("dve", lambda e: e.tensor_copy(out=self.Eb[:], in_=btf[:]), reads=[bc], writes=[bc])
        lv = P.sb("lv", [128, 256], F32)
        P.dma("sp", lambda e: e.dma_start(out=lv[:], in_=self.a_lam[0].partition_broadcast(128)), writes=[bc])
        junk = P.sb("junk", [128, 64], F32)
        s12 = P.sb("s12", [128, 2], F32)
        for i in range(2):
            P.op("dve", lambda e: e.scalar_tensor_tensor(out=junk[:], in0=lv[:, 128 * i:128 * i + 64], scalar=1.0, in1=lv[:, 128 * i + 64:128 * i + 128], op0=ALU.mult, op1=ALU.mult, accum_out=s12[:, i:i + 1]),
                 reads=[bc], writes=[bc])
        P.op("act", lambda e: e.activation(out=s12[:], in_=s12[:], func=AF.Exp), reads=[bc], writes=[bc])
        self.neg_lam = P.sb("neg_lam", [128, 1], F32)
        P.op("dve", lambda e: e.scalar_tensor_tensor(out=self.neg_lam[:], in0=s12[:, 1:2], scalar=-lam_init,
                                                     in1=s12[:, 0:1], op0=ALU.add, op1=ALU.subtract),
             reads=[bc], writes=[bc])
        self.gsub = P.sb("gsub", [128, 128], F32)
        P.dma("sp", lambda e: e.dma_start(out=self.gsub[:], in_=self.a_g[0].partition_broadcast(128)), writes=[bc])
        P.op("dve", lambda e: e.tensor_scalar(out=self.gsub[:], in0=self.gsub[:], scalar1=1.0 - lam_init, scalar2=None,
                                              op0=ALU.mult), reads=[bc], writes=[bc])

    def proj_qkv(self):
        P, S = self.P, self.S
        wi = P.sb("wi", [128, 8, 3 * D], BF16); b_wi = Buf("wi")
        for c in range(8):
            for hf in range(2):
                P.dma("pool", lambda e: e.dma_start(out=wi[:, c, hf * 1536:(hf + 1) * 1536],
                                                    in_=self.a_w_in[c * 128:(c + 1) * 128, hf * 1536:(hf + 1) * 1536]),
                      writes=[b_wi])
        xb_r = Ring(P, "xb", 3, [128, D], BF16)
        pT_r = Ring(P, "pT", 2, [128, 8, 128], BF16, psum=True)
        xT_r = Ring(P, "xT", 2, [128, 8, 512], BF16)
        pq_r = Ring(P, "pq", 4, [128, 512], F32, psum=True)
        qs_r = Ring(P, "qs", 2, [128, 16, 512], BF16)
        pv_r = Ring(P, "pv", 2, [128, 512], F32, psum=True)
        vs_r = Ring(P, "vs", 2, [128, D], BF16)
        ev = 0
        for s in range(self.nseq):
            for g in range(S // 512):
                xT, b_xT = xT_r.next()
                for tl in range(4):
                    r0 = s * S + g * 512 + tl * 128
                    xb, b_xb = xb_r.next()
                    P.dma("pool", lambda e: e.dma_start(out=xb[:], in_=self.x[r0:r0 + 128, :]), writes=[b_xb])
                    pT, b_pT = pT_r.next()
                    for c in range(8):
                        P.op("pe", lambda e: e.transpose(pT[:, c, :], xb[:, c * 128:(c + 1) * 128], self.ident[:]),
                             reads=[b_xb, self.b_const], writes=[b_pT])
                    P.op("dve", lambda e: e.tensor_copy(out=xT[:, :, tl * 128:(tl + 1) * 128], in_=pT[:]),
                         reads=[b_pT], writes=[b_xT])
                qs, b_qs = qs_r.next()
                for cg in range(16):
                    pq, b_pq = pq_r.next()
                    for c in range(8):
                        P.op("pe", lambda e: e.matmul(pq[:], lhsT=wi[:, c, cg * 128:(cg + 1) * 128], rhs=xT[:, c, :],
                                                      start=(c == 0), stop=(c == 7)), reads=[b_wi, b_xT], writes=[b_pq])
                    sc = 0.125 if cg < 8 else 1.0
                    if ev % 2 == 0:
                        P.op("act", lambda e: e.activation(out=qs[:, cg, :], in_=pq[:], func=AF.Copy, scale=sc),
                             reads=[b_pq], writes=[b_qs])
                    else:
                        P.op("dve", lambda e: e.tensor_scalar(out=qs[:, cg, :], in0=pq[:], scalar1=sc, scalar2=None,
                                                              op0=ALU.mult), reads=[b_pq], writes=[b_qs])
                    ev += 1
                for q8 in range(2):
                    P.dma("sp", lambda e: e.dma_start(out=self.QKT[s, q8 * 8:(q8 + 1) * 8, :, g * 512:(g + 1) * 512].rearrange("g p t -> p g t"),
                                                      in_=qs[:, q8 * 8:(q8 + 1) * 8, :]), reads=[b_qs])
                for tl in range(4):
                    vs, b_vs = vs_r.next()
                    for hf in range(2):
                        pv, b_pv = pv_r.next()
                        for c in range(8):
                            P.op("pe", lambda e: e.matmul(pv[:], lhsT=xT[:, c, tl * 128:(tl + 1) * 128],
                                                          rhs=wi[:, c, 2048 + hf * 512:2048 + (hf + 1) * 512],
                                                          start=(c == 0), stop=(c == 7)), reads=[b_wi, b_xT], writes=[b_pv])
                        if ev % 2 == 0:
                            P.op("act", lambda e: e.activation(out=vs[:, hf * 512:(hf + 1) * 512], in_=pv[:], func=AF.Copy),
                                 reads=[b_pv], writes=[b_vs])
                        else:
                            P.op("dve", lambda e: e.tensor_copy(out=vs[:, hf * 512:(hf + 1) * 512], in_=pv[:]),
                                 reads=[b_pv], writes=[b_vs])
                        ev += 1
                    t0 = g * 512 + tl * 128
                    P.dma("sp", lambda e: e.dma_start(out=self.V[s, t0:t0 + 128, :], in_=vs[:]),
                          reads=[b_vs])

    def attn(self):
        P, S = self.P, self.S
        nqb = S // 128
        nsb = nqb // 4
        QT_r = Ring(P, "QT", 2, [128, S], BF16)
        KT_r = Ring(P, "KT", 2, [128, S], BF16)
        Va_r = Ring(P, "Va", 2, [128, nqb, 132], BF16)
        for va, b in Va_r.slots:
            P.op("pool", lambda e: e.memset(va[:, :, 128:129], 1.0), writes=[b])
        S_r = [Ring(P, "S%d" % m, 2, [128, 512], F32, psum=True) for m in range(2)]
        PT_r = Ring(P, "PT", 6, [128, 512], BF16)
        Ob = [P.ps("Ob", [128, 512], F32) for _ in range(3)]
        b_Ob = [Buf("Ob%d" % i) for i in range(3)]
        acc = {}
        order = [(0, 0), (0, 1), (0, 2), (0, 3), (1, 0), (1, 1), (1, 2), (1, 3)]
        for n, key in enumerate(order):
            acc[key] = (n // 3, (n % 3) * 130)
        Tb, b_Tb = P.ps("Tb", [128, 4, 128], BF16), Buf("Tb")
        os_r = Ring(P, "os", 2, [128, 512], BF16)

        steps = []
        for s in range(self.nseq):
            for h in range(NH):
                for sbi in range(nsb):
                    i0 = 4 * sbi
                    for j in range(i0 + 4):
                        steps.append((s, h, sbi, j))
        cur = {}

        def load_head(s, h):
            QT, b_QT = QT_r.next(); KT, b_KT = KT_r.next(); Va, b_Va = Va_r.next()
            P.dma("sp", lambda e: e.dma_start(out=QT[:], in_=self.QKT[s, h]), reads=[self.b_QKT[s]], writes=[b_QT])
            P.dma("sp", lambda e: e.dma_start(out=KT[:], in_=self.QKT[s, 8 + h]), reads=[self.b_QKT[s]], writes=[b_KT])
            JB = 8
            for j0 in range(0, nqb, JB):
                j1 = min(nqb, j0 + JB)
                P.dma("sp", lambda e: e.dma_start(out=Va[:, j0:j1, 0:128],
                                                  in_=self.V[s, j0 * 128:j1 * 128, h * 128:(h + 1) * 128].rearrange("(j p) e -> p j e", p=128)),
                      reads=[self.b_V[s]], writes=[b_Va])
            return (QT, b_QT, KT, b_KT, Va, b_Va)

        heads = {}
        hl = [(s, h) for s in range(self.nseq) for h in range(NH)]
        heads[hl[0]] = load_head(*hl[0])

        def emit_S(step):
            s, h, sbi, j = step
            if (s, h) not in heads:
                heads[(s, h)] = load_head(s, h)
            QT, b_QT, KT, b_KT, Va, b_Va = heads[(s, h)]
            i0 = 4 * sbi
            qlo = max(j, i0)
            N = (i0 + 4 - qlo) * 128
            res = []
            for m in range(2):
                St, b_S = S_r[m].next()
                P.op("pe", lambda e: e.matmul(St[:, 0:N], lhsT=KT[m * 64:(m + 1) * 64, j * 128:(j + 1) * 128],
                                              rhs=QT[m * 64:(m + 1) * 64, qlo * 128:(i0 + 4) * 128],
                                              start=True, stop=True), reads=[b_KT, b_QT], writes=[b_S])
                res.append((St, b_S))
            return res

        def emit_rest(step, Sres, touched):
            s, h, sbi, j = step
            QT, b_QT, KT, b_KT, Va, b_Va = heads[(s, h)]
            i0 = 4 * sbi
            qlo = max(j, i0)
            N = (i0 + 4 - qlo) * 128
            for m in range(2):
                St, b_S = Sres[m]
                PT, b_PT = PT_r.next()
                P.op("act", lambda e: e.activation(out=PT[:, 0:N], in_=St[:, 0:N], func=AF.Exp),
                     reads=[b_S], writes=[b_PT])
                for ii in range(qlo - i0, 4):
                    dd = (i0 + ii) - j
                    if dd in (0, 1):
                        c0 = (ii - (qlo - i0)) * 128
                        P.op("dve", lambda e: e.tensor_tensor(out=PT[:, c0:c0 + 128], in0=PT[:, c0:c0 + 128],
                                                              in1=self.Eb[:, dd, h, :], op=ALU.mult),
                             reads=[b_PT, self.b_attn_c], writes=[b_PT])
                for ii in range(qlo - i0, 4):
                    bank, off = acc[(m, ii)]
                    c0 = (ii - (qlo - i0)) * 128
                    first = bank not in touched
                    touched.add(bank)
                    P.op("pe", lambda e: e.matmul(Ob[bank][:, off:off + 129], lhsT=PT[:, c0:c0 + 128],
                                                  rhs=Va[:, j, 0:129], start=first, stop=(j == i0 + ii),
                                                  skip_group_check=True),
                         reads=[b_PT, b_Va], writes=[b_Ob[bank]])

        Osb_r = Ring(P, "Osb", 2, [128, 8, 130], F32)
        cm_r = Ring(P, "cm", 2, [128, 32], F32)
        t4_r = Ring(P, "t4", 2, [128, 4, 128], F32)
        o4_r = Ring(P, "o4", 2, [128, 4, 128], F32)
        q4_r = Ring(P, "q4", 2, [128, 4, 128], F32)
        ob3_r = Ring(P, "ob3", 3, [128, 4, 128], BF16)

        def combine_gen(s, h, sbi):
            i0 = 4 * sbi
            Osb, b_Osb = Osb_r.next()
            for bank in range(3):
                n0 = bank * 3
                n1 = min(8, n0 + 3)
                P.op("dve", lambda e: e.tensor_copy(out=Osb[:, n0:n1, :].rearrange("p a b -> p (a b)"),
                                                    in_=Ob[bank][:, 0:(n1 - n0) * 130]),
                     reads=[b_Ob[bank]], writes=[b_Osb])
            yield
            cm, b_cm = cm_r.next()
            R = [b_cm]
            bc3 = lambda ap: ap.unsqueeze(2).to_broadcast([128, 4, 128])
            P.op("dve", lambda e: e.reciprocal(out=cm[:, 0:8].unsqueeze(2), in_=Osb[:, :, 128:129]), reads=[b_Osb], writes=R)
            yield
            P.op("dve", lambda e: e.tensor_scalar(out=cm[:, 8:12], in0=cm[:, 4:8], scalar1=self.neg_lam[:, 0:1], scalar2=None,
                                                  op0=ALU.mult), reads=R + [self.b_attn_c], writes=R)
            t4, b_t4 = t4_r.next()
            P.op("dve", lambda e: e.tensor_tensor(out=t4[:], in0=Osb[:, 0:4, 0:128], in1=bc3(cm[:, 0:4]), op=ALU.mult),
                 reads=[b_Osb] + R, writes=[b_t4])
            yield
            o4, b_o4 = o4_r.next()
            P.op("dve", lambda e: e.tensor_tensor(out=o4[:], in0=Osb[:, 4:8, 0:128], in1=bc3(cm[:, 8:12]), op=ALU.mult),
                 reads=[b_Osb] + R, writes=[b_o4])
            yield
            P.op("dve", lambda e: e.tensor_tensor(out=o4[:], in0=o4[:], in1=t4[:], op=ALU.add), reads=[b_o4, b_t4], writes=[b_o4])
            yield
            q4, b_q4 = q4_r.next()
            P.op("dve", lambda e: e.tensor_tensor(out=q4[:], in0=o4[:], in1=o4[:], op=ALU.mult), reads=[b_o4], writes=[b_q4])
            yield
            P.op("dve", lambda e: e.tensor_reduce(out=cm[:, 12:16], in_=q4[:], axis=mybir.AxisListType.X, op=ALU.add),
                 reads=[b_q4], writes=R)
            yield
            P.op("dve", lambda e: e.tensor_scalar(out=cm[:, 16:20], in0=cm[:, 12:16], scalar1=1.0 / 128, scalar2=LN_EPS,
                                                  op0=ALU.mult, op1=ALU.add), reads=R, writes=R)
            P.op("pool", lambda e: e.tensor_tensor(out=cm[:, 20:24], in0=cm[:, 16:20], in1=self.mhalf[:, 0:1].to_broadcast([128, 4]),
                                                   op=ALU.pow), reads=R + [self.b_const], writes=R)
            yield
            yield
            P.op("dve", lambda e: e.tensor_tensor(out=o4[:], in0=o4[:], in1=bc3(cm[:, 20:24]), op=ALU.mult),
                 reads=[b_o4] + R, writes=[b_o4])
            yield
            ob, b_ob = ob3_r.next()
            P.op("dve", lambda e: e.tensor_tensor(out=ob[:], in0=o4[:], in1=self.gsub[:].unsqueeze(1).to_broadcast([128, 4, 128]),
                                                  op=ALU.mult), reads=[b_o4, self.b_attn_c], writes=[b_ob])
            yield
            yield
            for ii in range(4):
                P.op("pe", lambda e: e.transpose(Tb[:, ii, :], ob[:, ii, :], self.ident[:]),
                     reads=[b_ob, self.b_const], writes=[b_Tb])
            os_, b_os = os_r.next()
            P.op("dve", lambda e: e.tensor_copy(out=os_[:], in_=Tb[:].rearrange("p a b -> p (a b)")),
                 reads=[b_Tb], writes=[b_os])
            P.dma("sp", lambda e: e.dma_start(out=self.OT[s, h, :, i0 * 128:(i0 + 4) * 128], in_=os_[:]),
                  reads=[b_os])

        def finish_gen(g):
            for _ in g:
                pass

        Sres_next = emit_S(steps[0])
        touched = set()
        active = []
        for t, step in enumerate(steps):
            Sres = Sres_next
            s, h, sbi, j = step
            if j == 0 and sbi == 0:
                idx = hl.index((s, h))
                if idx + 1 < len(hl) and hl[idx + 1] not in heads:
                    heads[hl[idx + 1]] = load_head(*hl[idx + 1])
            if t + 1 < len(steps):
                Sres_next = emit_S(steps[t + 1])
            if j == 0:
                touched = set()
            emit_rest(step, Sres, touched)
            for g in list(active):
                try:
                    next(g)
                except StopIteration:
                    active.remove(g)
            if j == 4 * sbi + 3:
                while len(active) >= 2:
                    finish_gen(active.pop(0))
                g = combine_gen(s, h, sbi)
                next(g)
                active.append(g)
        for g in active:
            finish_gen(g)

    def phase(self, fn, *a, **k):
        nc = self.nc
        snap = (nc.psum_base, nc.psum_top, nc.sbuf_base, nc.sbuf_top)
        fn(*a, **k)
        self.P.barrier()
        nc.psum_base, nc.psum_top, nc.sbuf_base, nc.sbuf_top = snap

    def setup_route(self):
        P = self.P
        self.SL = P.sb("SL", [128, self.NTL, 2], I32)
        self.GT = P.sb("GT", [128, self.NTL, 2], F32)
        self.cnt = P.sb("cnt", [128, NE], F32)
        self.b_SL = [Buf("SL%d" % i) for i in range(self.NTL)]
        self.b_cnt = Buf("cnt")
        self.bound_reg = self.nc.gpsimd.to_reg(NE * self.C - 1)

    def ln_alloc(self, layer, j, route, share_pT=False, nbuf=2, nhb=None, xn_psum=0):
        P = self.P

        class L:
            pass
        L.layer, L.j, L.route = layer, j, route
        L.g = P.sb("lng", [128, D], F32); L.b = P.sb("lnb", [128, D], F32); L.b_gb = Buf("lngb")
        P.dma("sp", lambda e: e.dma_start(out=L.g[:], in_=self.ln_g[layer, j].partition_broadcast(128)), writes=[L.b_gb])
        P.dma("sp", lambda e: e.dma_start(out=L.b[:], in_=self.ln_b[layer, j].partition_broadcast(128)), writes=[L.b_gb])
        L.tt_r = Ring(P, "ltt", nbuf, [128, D], F32)
        L.st_r = Ring(P, "lst", nbuf, [128, 2, 6], F32)
        L.mv_r = Ring(P, "lmv", nbuf, [128, 8], F32)
        L.xn_psum = xn_psum
        if xn_psum:
            L.xn_r = Ring(P, "lxnp", xn_psum, [128, D], F32, psum=True)
        else:
            L.xn_r = Ring(P, "lxn", max(1, nbuf - 1), [128, D], F32)
        L.h_r = Ring(P, "lh", nbuf, [128, D], F32)
        if route:
            L.hb_r = Ring(P, "lhb", nhb or nbuf, [128, D], BF16)
            L.pT_r = Ring(P, "lpT", 1, [128, 8, 128], BF16, psum=True)
            L.hT_r = Ring(P, "lhT", max(1, nbuf - 1), [128, 8, 128], BF16)
            L.wr = P.sb("wr", [128, 8, 72], BF16); L.br = P.sb("br", [128, 72], F32); L.b_wr = Buf("wr")
            P.dma("pool", lambda e: e.dma_start(out=L.wr[:], in_=self.m_wr[layer].rearrange("(c p) f -> p c f", p=128)),
                  writes=[L.b_wr])
            P.dma("sp", lambda e: e.dma_start(out=L.br[:], in_=self.m_br[layer].partition_broadcast(128)), writes=[L.b_wr])
            L.pLC = P.ps("pLC", [128, 512], F32); L.b_pLC = Buf("pLC")
            L.rt_r = Ring(P, "rt", nbuf, [128, 200], F32)
            L.i8_r = Ring(P, "i8", nbuf, [128, 8], U32)
            L.oh_r = Ring(P, "oh", nbuf, [128, 2, NE], F32)
            L.M_r = Ring(P, "Mb", nbuf, [128, NE], BF16)
            L.pf_r = Ring(P, "pf", nbuf, [128, NE], F32)
            L.jk_r = Ring(P, "rjk", nbuf, [128, NE], F32)
            L.ss_r = Ring(P, "ss", nbuf + 3, [128, 2], I32)
            P.op("dve", lambda e: e.memset(self.cnt[:], 0.0), writes=[self.b_cnt])
        return L

    @staticmethod
    def run_gens(gens):
        gens = list(gens)
        while gens:
            for g in list(gens):
                try:
                    next(g)
                except StopIteration:
                    gens.remove(g)

    @staticmethod
    def run_fg_bg(fg, bg):
        fg = list(fg)
        while fg:
            for g in list(fg):
                try:
                    next(g)
                except StopIteration:
                    fg.remove(g)
            for g in list(bg):
                try:
                    next(g)
                except StopIteration:
                    bg.remove(g)

    def ln_part_gen(self, L, mix, b_mix, resid, b_resid, Hd, b_Hd, ti, gmul="pool"):
        P = self.P
        tt, b_tt = L.tt_r.next()
        P.op("dve", lambda e: e.scalar_tensor_tensor(out=tt[:], in0=resid, scalar=DN_ALPHA, in1=mix,
                                                     op0=ALU.mult, op1=ALU.add), reads=[b_resid, b_mix], writes=[b_tt])
        yield
        h, b_h = yield from self.ln_core_gen(L, tt, b_tt, gmul)
        P.dma("sp", lambda e: e.dma_start(out=Hd[ti * 128:(ti + 1) * 128, :], in_=h[:]), reads=[b_h])
        if not L.route:
            return None
        hb, b_hb = L.hb_r.next()
        P.op("act", lambda e: e.activation(out=hb[:], in_=h[:], func=AF.Copy), reads=[b_h], writes=[b_hb])
        yield
        return hb, b_hb

    def ln_route_gen(self, L, mix, b_mix, resid, b_resid, Hd, b_Hd, ti, gmul="pool"):
        r = yield from self.ln_part_gen(L, mix, b_mix, resid, b_resid, Hd, b_Hd, ti, gmul)
        if r is not None:
            yield from self.route_gen(L, r[0], r[1], ti)

    def ln_core_gen(self, L, tt, b_tt, gmul="pool", xn_slot=None):
        P = self.P
        st, b_st = L.st_r.next()
        for i in range(2):
            P.op("dve", lambda e: e.bn_stats(out=st[:, i, :], in_=tt[:, i * 512:(i + 1) * 512]), reads=[b_tt], writes=[b_st])
        yield
        mv, b_mv = L.mv_r.next()
        P.op("dve", lambda e: e.bn_aggr(out=mv[:, 0:2], in_=st[:].rearrange("p a b -> p (a b)")), reads=[b_st], writes=[b_mv])
        yield
        P.op("dve", lambda e: e.tensor_scalar(out=mv[:, 2:3], in0=mv[:, 1:2], scalar1=LN_EPS, scalar2=None, op0=ALU.add),
             reads=[b_mv], writes=[b_mv])
        yield
        P.op("pool", lambda e: e.tensor_tensor(out=mv[:, 3:4], in0=mv[:, 2:3], in1=self.mhalf[:], op=ALU.pow),
             reads=[b_mv, self.b_const], writes=[b_mv])
        yield
        P.op("dve", lambda e: e.scalar_tensor_tensor(out=mv[:, 4:5], in0=mv[:, 0:1], scalar=-1.0, in1=mv[:, 3:4],
                                                     op0=ALU.mult, op1=ALU.mult), reads=[b_mv], writes=[b_mv])
        yield
        xn, b_xn = xn_slot if xn_slot is not None else L.xn_r.next()
        P.op("act", lambda e: e.activation(out=xn[:], in_=tt[:], func=AF.Identity, bias=mv[:, 4:5], scale=mv[:, 3:4]),
             reads=[b_tt, b_mv], writes=[b_xn])
        P.op("dve" if L.xn_psum else gmul, lambda e: e.tensor_tensor(out=xn[:], in0=xn[:], in1=L.g[:], op=ALU.mult),
             reads=[b_xn, L.b_gb], writes=[b_xn])
        h, b_h = L.h_r.next()
        P.op("dve", lambda e: e.tensor_tensor(out=h[:], in0=xn[:], in1=L.b[:], op=ALU.add),
             reads=[b_xn, L.b_gb], writes=[b_h])
        yield
        return h, b_h

    def route_gen(self, L, hb, b_hb, ti, defer=None):
        P, C = self.P, self.C
        BIG = 1.0e4
        pT, b_pT = L.pT_r.next()
        for c in range(8):
            P.op("pe", lambda e: e.transpose(pT[:, c, :], hb[:, c * 128:(c + 1) * 128], self.ident[:]),
                 reads=[b_hb, self.b_const], writes=[b_pT])
        hT, b_hT = L.hT_r.next()
        P.op("dve", lambda e: e.tensor_copy(out=hT[:], in_=pT[:]), reads=[b_pT], writes=[b_hT])
        pL, b_pL = L.pLC[:, 0:128], L.b_pLC
        for c in range(8):
            P.op("pe", lambda e: e.matmul(pL[:, 0:72], lhsT=hT[:, c, :], rhs=L.wr[:, c, :], start=(c == 0), stop=(c == 7)),
                 reads=[b_hT, L.b_wr], writes=[b_pL])
        rt, b_rt = L.rt_r.next()
        R = [b_rt]

        def dve(fn, reads=(), writes=()):
            P.op("dve", fn, reads=list(reads) + R, writes=list(writes) + R)
        P.op("dve", lambda e: e.tensor_tensor(out=rt[:, 0:72], in0=pL[:, 0:72], in1=L.br[:], op=ALU.add),
             reads=[b_pL, L.b_wr], writes=R)
        yield
        dve(lambda e: e.max(out=rt[:, 72:80], in_=rt[:, 0:8]))
        yield
        dve(lambda e: e.tensor_scalar(out=rt[:, 80:81], in0=rt[:, 72:73], scalar1=-1.0, scalar2=None, op0=ALU.mult))
        dve(lambda e: e.tensor_scalar(out=rt[:, 91:99], in0=rt[:, 0:8], scalar1=rt[:, 72:73], scalar2=None, op0=ALU.is_equal))
        yield
        P.op("act", lambda e: e.activation(out=rt[:, 81:89], in_=rt[:, 0:8], func=AF.Exp, bias=rt[:, 80:81], scale=1.0,
                                           accum_out=rt[:, 89:90]), reads=R, writes=R)
        dve(lambda e: e.tensor_scalar(out=rt[:, 99:107], in0=rt[:, 91:99], scalar1=BIG, scalar2=-BIG, op0=ALU.mult, op1=ALU.add))
        yield
        dve(lambda e: e.tensor_tensor(out=rt[:, 107:171].rearrange("p (g j) -> p g j", g=8),
                                      in0=rt[:, 8:72].rearrange("p (g j) -> p g j", g=8),
                                      in1=rt[:, 99:107].unsqueeze(2).to_broadcast([128, 8, 8]), op=ALU.add))
        yield
        dve(lambda e: e.max(out=rt[:, 171:179], in_=rt[:, 107:171]))
        yield
        i8, b_i8 = L.i8_r.next()
        dve(lambda e: e.max_index(out=i8[:], in_max=rt[:, 171:179], in_values=rt[:, 107:171]), writes=[b_i8])
        dve(lambda e: e.tensor_tensor(out=rt[:, 181:182], in0=rt[:, 172:173], in1=rt[:, 171:172], op=ALU.subtract))
        yield
        dve(lambda e: e.tensor_copy(out=rt[:, 179:181], in_=i8[:, 0:2]), reads=[b_i8])
        P.op("act", lambda e: e.activation(out=rt[:, 182:183], in_=rt[:, 181:182], func=AF.Exp), reads=R, writes=R)
        yield
        oh, b_oh = L.oh_r.next()
        for k in range(2):
            dve(lambda e: e.tensor_scalar(out=oh[:, k, :], in0=self.iota_e[:], scalar1=rt[:, 179 + k:180 + k], scalar2=None,
                                          op0=ALU.is_equal), reads=[self.b_const], writes=[b_oh])
        yield
        Mb, b_M = L.M_r.next()
        P.op("dve", lambda e: e.tensor_tensor(out=Mb[:], in0=oh[:, 0, :], in1=oh[:, 1, :], op=ALU.add),
             reads=[b_oh], writes=[b_M])
        yield
        pC, b_pC = L.pLC[:, 128:256].rearrange("p (a b) -> p a b", a=2), L.b_pLC
        P.op("pe", lambda e: e.matmul(pC[:, 0, :], lhsT=self.ustrict[:], rhs=Mb[:], start=True, stop=True),
             reads=[b_M, self.b_const], writes=[b_pC])
        P.op("pe", lambda e: e.matmul(pC[:, 1, :], lhsT=self.ones[:], rhs=Mb[:], start=True, stop=True),
             reads=[b_M, self.b_const], writes=[b_pC])
        pf, b_pf = L.pf_r.next()
        P.op("dve", lambda e: e.tensor_tensor(out=pf[:], in0=pC[:, 0, :], in1=self.cnt[:], op=ALU.add),
             reads=[b_pC, self.b_cnt], writes=[b_pf])
        P.op("dve", lambda e: e.tensor_tensor(out=self.cnt[:], in0=pC[:, 1, :], in1=self.cnt[:], op=ALU.add),
             reads=[b_pC, self.b_cnt], writes=[self.b_cnt])
        yield
        dve(lambda e: e.reciprocal(out=rt[:, 90:91], in_=rt[:, 89:90]))
        dve(lambda e: e.tensor_scalar(out=rt[:, 183:184], in0=rt[:, 182:183], scalar1=1.0, scalar2=None, op0=ALU.add))
        yield
        dve(lambda e: e.reciprocal(out=rt[:, 184:185], in_=rt[:, 183:184]))
        yield
        dve(lambda e: e.tensor_tensor(out=rt[:, 185:186], in0=rt[:, 184:185], in1=rt[:, 90:91], op=ALU.mult))
        yield
        dve(lambda e: e.tensor_tensor(out=rt[:, 186:187], in0=rt[:, 185:186], in1=rt[:, 182:183], op=ALU.mult))
        yield
        jk, b_jk = L.jk_r.next()
        for k in range(2):
            dve(lambda e: e.scalar_tensor_tensor(out=jk[:], in0=oh[:, k, :], scalar=1.0, in1=pf[:],
                                                 op0=ALU.mult, op1=ALU.mult, accum_out=rt[:, 187 + k:188 + k]),
                reads=[b_oh, b_pf], writes=[b_jk])
        yield
        dve(lambda e: e.tensor_scalar(out=rt[:, 189:191], in0=rt[:, 187:189], scalar1=float(C), scalar2=None, op0=ALU.is_lt))
        dve(lambda e: e.scalar_tensor_tensor(out=rt[:, 191:193], in0=rt[:, 179:181], scalar=float(C), in1=rt[:, 187:189],
                                             op0=ALU.mult, op1=ALU.add))
        yield
        dve(lambda e: e.tensor_tensor(out=rt[:, 193:195], in0=rt[:, 191:193], in1=rt[:, 189:191], op=ALU.mult))
        dve(lambda e: e.tensor_scalar(out=rt[:, 195:197], in0=rt[:, 189:191], scalar1=-1.0e6, scalar2=1.0e6,
                                      op0=ALU.mult, op1=ALU.add))
        yield
        dve(lambda e: e.tensor_copy(out=self.SL[:, ti, :], in_=rt[:, 193:195]), writes=[self.b_SL[ti]])
        dve(lambda e: e.tensor_tensor(out=rt[:, 197:199], in0=rt[:, 195:197], in1=rt[:, 191:193], op=ALU.add))
        yield
        ss, b_ss = L.ss_r.next()
        dve(lambda e: e.tensor_copy(out=ss[:], in_=rt[:, 197:199]), writes=[b_ss])
        dve(lambda e: e.tensor_tensor(out=self.GT[:, ti, :], in0=rt[:, 185:187], in1=rt[:, 189:191], op=ALU.mult),
            writes=[self.b_SL[ti]])
        yield
        def scatter():
            for k in range(2):
                P.dma("pool", lambda e: e.indirect_dma_start(out=self.XS[:, :],
                                                             out_offset=bass.IndirectOffsetOnAxis(ap=ss[:, k:k + 1], axis=0),
                                                             in_=hb[:, :], in_offset=None,
                                                             bounds_check=self.bound_reg, oob_is_err=False),
                      reads=[b_hb, b_ss])
        if defer is None:
            scatter()
        else:
            defer.append(scatter)

    def post_attn(self):
        P, S = self.P, self.S
        wo = P.sb("wo", [128, NH, D], BF16); b_wo = Buf("wo")
        P.dma("pool", lambda e: e.dma_start(out=wo[:], in_=self.a_w_out.ap().rearrange("(h p) f -> p h f", p=128)),
              writes=[b_wo])
        L = self.ln_alloc(0, 0, True, nbuf=4, xn_psum=1, nhb=8)
        bgr = []
        ot_r = Ring(P, "ot4", 2, [128, NH, 512], BF16)
        x_r = Ring(P, "xres", 3, [128, D], F32)
        mix_r = Ring(P, "mix", 2, [128, D], F32, psum=True)
        prev = []
        for s in range(self.nseq):
            for g in range(S // 512):
                ot, b_ot = ot_r.next()
                P.dma("sp", lambda e: e.dma_start(out=ot[:], in_=self.OT[s, :, :, g * 512:(g + 1) * 512].rearrange("h p t -> p h t")),
                      reads=[self.b_OT[s]], writes=[b_ot])
                cur = []
                lgens = []
                for tl in range(4):
                    ti = (s * S + g * 512 + tl * 128) // 128
                    xr, b_xr = x_r.next()
                    P.dma("sp", lambda e: e.dma_start(out=xr[:], in_=self.x[ti * 128:(ti + 1) * 128, :]), writes=[b_xr])
                    mix, b_mix = mix_r.next()
                    for hf in range(2):
                        for h in range(NH):
                            P.op("pe", lambda e: e.matmul(mix[:, hf * 512:(hf + 1) * 512], lhsT=ot[:, h, tl * 128:(tl + 1) * 128],
                                                          rhs=wo[:, h, hf * 512:(hf + 1) * 512], start=(h == 0), stop=(h == NH - 1)),
                                 reads=[b_ot, b_wo], writes=[b_mix])

                    def lgen(mix=mix, b_mix=b_mix, xr=xr, b_xr=b_xr, ti=ti):
                        r = yield from self.ln_part_gen(L, mix[:], b_mix, xr[:], b_xr, self.H1, self.b_H["H1"], ti)
                        cur.append((r[0], r[1], ti))
                    gen = lgen()
                    next(gen)
                    lgens.append(gen)
                    if len(lgens) == 2:
                        self.run_fg_bg(lgens, bgr)
                        lgens = []
                self.run_gens(bgr)
                bgr = [self.route_gen(L, hb, b_hb, ti) for hb, b_hb, ti in cur]
        self.run_gens(bgr)

    def moe(self, layer):
        P, C = self.P, self.C
        nb = C // 128
        wg_r = Ring(P, "wg", 2, [128, 8, HID], BF16)
        wu_r = Ring(P, "wu", 2, [128, 8, HID], BF16)
        wd_r = Ring(P, "wd", 2, [128, 4, D], BF16)
        xs_r = Ring(P, "xs", 2, [128, nb, D], BF16)
        pT_r = Ring(P, "mpT", 2, [128, 8, 128], BF16, psum=True)
        xT_r = Ring(P, "mxT", 2, [128, 8, C], BF16)
        pG_r = Ring(P, "pG", 2, [128, 512], F32, psum=True)
        pU_r = Ring(P, "pU", 2, [128, 512], F32, psum=True)
        pY_r = Ring(P, "pY", 2, [128, 512], F32, psum=True)
        sg_r = Ring(P, "sg", 2, [128, C], F32)
        hT_r = Ring(P, "hT", 2, [128, 4, C], BF16)
        y_r = Ring(P, "y", 3, [128, D], BF16)
        loaded = {}

        def load(e):
            wg, b_wg = wg_r.next(); wu, b_wu = wu_r.next(); wd, b_wd = wd_r.next(); xs, b_xs = xs_r.next()
            P.dma("sp", lambda en: en.dma_start(out=xs[:], in_=self.XS[e * C:(e + 1) * C, :].rearrange("(b p) d -> p b d", p=128)),
                  reads=[self.b_XS], writes=[b_xs])
            P.dma("pool", lambda en: en.dma_start(out=wg[:], in_=self.m_wg[layer, e].rearrange("(c p) f -> p c f", p=128)),
                  writes=[b_wg])
            P.dma("pool", lambda en: en.dma_start(out=wu[:], in_=self.m_wu[layer, e].rearrange("(c p) f -> p c f", p=128)),
                  writes=[b_wu])
            for hf in range(2):
                P.dma("pool", lambda en: en.dma_start(out=wd[:, :, hf * 512:(hf + 1) * 512],
                                                      in_=self.m_wd[layer, e, :, hf * 512:(hf + 1) * 512].rearrange("(c p) f -> p c f", p=128)),
                      writes=[b_wd])
            loaded[e] = (wg, b_wg, wu, b_wu, wd, b_wd, xs, b_xs)

        load(0)
        ev = 0
        for e in range(NE):
            if e + 1 < NE:
                load(e + 1)
            wg, b_wg, wu, b_wu, wd, b_wd, xs, b_xs = loaded.pop(e)
            xT, b_xT = xT_r.next()
            for b in range(nb):
                pT, b_pT = pT_r.next()
                for c in range(8):
                    P.op("pe", lambda en: en.transpose(pT[:, c, :], xs[:, b, c * 128:(c + 1) * 128], self.ident[:]),
                         reads=[b_xs, self.b_const], writes=[b_pT])
                if ev % 2 == 0:
                    P.op("act", lambda en: en.activation(out=xT[:, :, b * 128:(b + 1) * 128], in_=pT[:], func=AF.Copy),
                         reads=[b_pT], writes=[b_xT])
                else:
                    P.op("dve", lambda en: en.tensor_copy(out=xT[:, :, b * 128:(b + 1) * 128], in_=pT[:]),
                         reads=[b_pT], writes=[b_xT])
                ev += 1
            hT, b_hT = hT_r.next()
            for hc in range(4):
                pG, b_pG = pG_r.next(); pU, b_pU = pU_r.next()
                for c in range(8):
                    P.op("pe", lambda en: en.matmul(pG[:, 0:C], lhsT=wg[:, c, hc * 128:(hc + 1) * 128], rhs=xT[:, c, :],
                                                    start=(c == 0), stop=(c == 7)), reads=[b_wg, b_xT], writes=[b_pG])
                for c in range(8):
                    P.op("pe", lambda en: en.matmul(pU[:, 0:C], lhsT=wu[:, c, hc * 128:(hc + 1) * 128], rhs=xT[:, c, :],
                                                    start=(c == 0), stop=(c == 7)), reads=[b_wu, b_xT], writes=[b_pU])
                sg, b_sg = sg_r.next()
                P.op("act", lambda en: en.activation(out=sg[:], in_=pG[:, 0:C], func=AF.Silu), reads=[b_pG], writes=[b_sg])
                P.op("dve", lambda en: en.tensor_tensor(out=hT[:, hc, :], in0=sg[:], in1=pU[:, 0:C], op=ALU.mult),
                     reads=[b_sg, b_pU], writes=[b_hT])
            for b in range(nb):
                y, b_y = y_r.next()
                for hf in range(2):
                    pY, b_pY = pY_r.next()
                    for hc in range(4):
                        P.op("pe", lambda en: en.matmul(pY[:], lhsT=hT[:, hc, b * 128:(b + 1) * 128],
                                                        rhs=wd[:, hc, hf * 512:(hf + 1) * 512], start=(hc == 0), stop=(hc == 3)),
                             reads=[b_hT, b_wd], writes=[b_pY])
                    if ev % 2 == 0:
                        P.op("act", lambda en: en.activation(out=y[:, hf * 512:(hf + 1) * 512], in_=pY[:], func=AF.Copy),
                             reads=[b_pY], writes=[b_y])
                    else:
                        P.op("dve", lambda en: en.tensor_copy(out=y[:, hf * 512:(hf + 1) * 512], in_=pY[:]),
                             reads=[b_pY], writes=[b_y])
                    ev += 1
                r0 = e * C + b * 128
                P.dma("sp", lambda en: en.dma_start(out=self.YS[r0:r0 + 128, :], in_=y[:]), reads=[b_y])

    def combine(self, layer, Hin, b_Hin, Hout, b_Hout):
        P = self.P
        L = self.ln_alloc(layer, 1, False, nbuf=3, xn_psum=3)
        PF = 3
        G = 3
        y0_r = Ring(P, "y0", PF + G, [128, D], BF16)
        y1_r = Ring(P, "y1", PF + G, [128, D], BF16)
        hi_r = Ring(P, "hin", PF + G, [128, D], F32)
        loads = {}

        def issue(ti):
            ys = []
            for k, r in enumerate((y0_r, y1_r)):
                y, b_y = r.next()
                P.dma("pool", lambda e: e.indirect_dma_start(out=y[:, :], out_offset=None, in_=self.YS[:, :],
                                                             in_offset=bass.IndirectOffsetOnAxis(ap=self.SL[:, ti, k:k + 1], axis=0)),
                      reads=[self.b_YS, self.b_SL[ti]], writes=[b_y])
                ys.append((y, b_y))
            hi, b_hi = hi_r.next()
            P.dma("sp", lambda e: e.dma_start(out=hi[:], in_=Hin[ti * 128:(ti + 1) * 128, :]), reads=[b_Hin], writes=[b_hi])
            loads[ti] = (ys, hi, b_hi)

        def tile_gen(ti):
            ys, hi, b_hi = loads.pop(ti)
            u, b_u = L.xn_r.next()
            P.op("act", lambda e: e.activation(out=u[:], in_=hi[:], func=AF.Copy, scale=DN_ALPHA), reads=[b_hi], writes=[b_u])
            yield
            P.op("dve", lambda e: e.scalar_tensor_tensor(out=u[:], in0=ys[0][0][:], scalar=self.GT[:, ti, 0:1], in1=u[:],
                                                         op0=ALU.mult, op1=ALU.add),
                 reads=[ys[0][1], self.b_SL[ti], b_u], writes=[b_u])
            yield
            tt, b_tt = L.tt_r.next()
            P.op("dve", lambda e: e.scalar_tensor_tensor(out=tt[:], in0=ys[1][0][:], scalar=self.GT[:, ti, 1:2], in1=u[:],
                                                         op0=ALU.mult, op1=ALU.add),
                 reads=[ys[1][1], self.b_SL[ti], b_u], writes=[b_tt])
            yield
            h, b_h = yield from self.ln_core_gen(L, tt, b_tt, "dve", xn_slot=(u, b_u))
            P.dma("sp", lambda e: e.dma_start(out=Hout[ti * 128:(ti + 1) * 128, :], in_=h[:]), reads=[b_h])

        NTL = self.NTL
        for ti in range(min(PF, NTL)):
            issue(ti)
        for t0 in range(0, NTL, G):
            tiles = list(range(t0, min(NTL, t0 + G)))
            for ti in tiles:
                if ti + PF < NTL:
                    issue(ti + PF)
            self.run_gens([tile_gen(ti) for ti in tiles])

    def gmlp(self):
        P, nc = self.P, self.nc
        NSUP = self.NT // 512
        bc = Buf("gconst")
        wo = P.sb("gwo", [128, 24, D], BF16)
        for q in range(6):
            P.dma("pool", lambda e: e.dma_start(out=wo[:, q * 4:(q + 1) * 4, :],
                                                in_=self.s_w_out[q * 512:(q + 1) * 512, :].rearrange("(c p) f -> p c f", p=128)),
                  writes=[bc])
        bu = P.sb("gbu", [128, 24], F32)
        with nc.allow_non_contiguous_dma(reason="one-time per-partition bias columns"):
            P.dma("sp", lambda e: e.dma_start(out=bu[:], in_=self.s_b_in[0, 0:SGH].rearrange("(c p) -> p c", p=128)), writes=[bc])
        lgb = P.sb("glgb", [128, SGH], BF16)
        for hf in range(2):
            P.dma("pool", lambda e: e.dma_start(out=lgb[:, hf * 1536:(hf + 1) * 1536],
                                                in_=self.s_ln_g[0, hf * 1536:(hf + 1) * 1536].partition_broadcast(128)), writes=[bc])
        sel32 = P.sb("gsel32", [128, 128], BF16)
        P.op("pool", lambda e: e.memset(sel32[:], 0.0), writes=[bc])
        P.op("pool", lambda e: e.memset(sel32[32:33, :], 1.0), reads=[bc], writes=[bc])
        wTb = P.sb("gwTb", [128, 8, 128], BF16)
        L4 = P.sb("gL4", [128, SGH], BF16)
        P.op("pool", lambda e: e.memset(L4[:], 0.0), writes=[bc])
        bv = L4[32:33, :]
        P.dma("pool", lambda e: e.dma_start(out=bv, in_=self.s_b_in[0:1, SGH:2 * SGH]), reads=[bc], writes=[bc])
        R4 = P.sb("gR4", [128, 8, 128], BF16)
        P.op("pool", lambda e: e.memset(R4[:], 0.0), writes=[bc])
        snap_sb = (nc.sbuf_base, nc.sbuf_top)
        identf = P.sb("gidf", [128, 128], F32)
        P.op("dve", lambda e: e.tensor_copy(out=identf[:], in_=self.ident[:]), reads=[self.b_const], writes=[bc])
        onesf = P.sb("gonesf", [128, 1], F32)
        P.op("dve", lambda e: e.memset(onesf[:], 1.0), writes=[bc])
        wnat = P.sb("gwnat", [128, 8, 128], F32)
        P.dma("sp", lambda e: e.dma_start(out=wnat[:], in_=self.s_w_s.ap().rearrange("g t s -> t g s")), writes=[bc])
        for g in range(8):
            P.op("pool", lambda e: e.affine_select(out=wnat[:, g, :], in_=wnat[:, g, :], pattern=[[-1, 128]],
                                                    compare_op=ALU.is_ge, fill=self.zero_reg, base=0, channel_multiplier=1),
                 reads=[bc], writes=[bc])
        wTf = P.sb("gwTf", [128, 8, 128], F32)
        pS = P.ps("gpS", [128, 512], F32); b_pS = Buf("gpS")
        rowf = P.sb("growf", [1, 2, SGH], F32)
        rowb = P.sb("growb", [1, 2, SGH], BF16)
        wsf = P.sb("gwsf", [1, 8, 128], F32)
        bsf = P.sb("gbsf", [1, 2, 8, 128], F32)
        row2 = P.sb("grow2", [1, 3, 8, 128], BF16)
        for g in range(8):
            P.op("pe", lambda e: e.transpose(pS[:, 0:128], wnat[:, g, :], identf[:]), reads=[bc], writes=[b_pS])
            P.op("dve", lambda e: e.tensor_copy(out=wTf[:, g, :], in_=pS[:, 0:128]), reads=[b_pS], writes=[bc])
            P.op("act", lambda e: e.activation(out=wTb[:, g, :], in_=pS[:, 0:128], func=AF.Copy), reads=[b_pS], writes=[bc])
            P.op("pe", lambda e: e.matmul(pS[0:1, 128:256], lhsT=onesf[:], rhs=wTf[:, g, :], start=True, stop=True),
                 reads=[bc], writes=[b_pS])
            P.op("dve", lambda e: e.tensor_copy(out=wsf[0:1, g, :], in_=pS[0:1, 128:256]), reads=[b_pS], writes=[bc])
        P.op("dve", lambda e: e.tensor_copy(out=row2[:, 0, :, :], in_=wsf[:]), reads=[bc], writes=[bc])
        P.dma("sp", lambda e: e.dma_start(out=rowf[:, 0, :], in_=self.s_ln_b[0:1, :]), writes=[bc])
        P.op("dve", lambda e: e.tensor_copy(out=rowb[:, 0, :], in_=rowf[:, 0, :]), reads=[bc], writes=[bc])
        P.op("dve", lambda e: e.tensor_copy(out=rowf[:, 1, :], in_=rowb[:, 0, :]), reads=[bc], writes=[bc])
        P.op("dve", lambda e: e.tensor_tensor(out=rowb[:, 1, :], in0=rowf[:, 0, :], in1=rowf[:, 1, :], op=ALU.subtract),
             reads=[bc], writes=[bc])
        P.dma("sp", lambda e: e.dma_start(out=bsf[:, 0, :, :], in_=self.s_b_s.ap().rearrange("(o g) t -> o g t", o=1)), writes=[bc])
        P.op("dve", lambda e: e.tensor_copy(out=row2[:, 1, :, :], in_=bsf[:, 0, :, :]), reads=[bc], writes=[bc])
        P.op("dve", lambda e: e.tensor_copy(out=bsf[:, 1, :, :], in_=row2[:, 1, :, :]), reads=[bc], writes=[bc])
        P.op("dve", lambda e: e.tensor_tensor(out=row2[:, 2, :, :], in0=bsf[:, 0, :, :], in1=bsf[:, 1, :, :], op=ALU.subtract),
             reads=[bc], writes=[bc])
        P.op("dve", lambda e: e.memset(L4[0:4, :], 1.0), reads=[bc], writes=[bc])
        P.dma("sp", lambda e: e.dma_start(out=L4[0:1, :], in_=rowb[:, 0, :]), reads=[bc], writes=[bc])
        P.dma("sp", lambda e: e.dma_start(out=L4[1:2, :], in_=rowb[:, 1, :]), reads=[bc], writes=[bc])
        P.dma("sp", lambda e: e.dma_start(out=R4[0:1, :, :], in_=row2[:, 0, :, :]), reads=[bc], writes=[bc])
        P.dma("sp", lambda e: e.dma_start(out=R4[1:2, :, :], in_=row2[:, 0, :, :]), reads=[bc], writes=[bc])
        P.dma("sp", lambda e: e.dma_start(out=R4[2:3, :, :], in_=row2[:, 1, :, :]), reads=[bc], writes=[bc])
        P.dma("sp", lambda e: e.dma_start(out=R4[3:4, :, :], in_=row2[:, 2, :, :]), reads=[bc], writes=[bc])

        P.barrier()
        nc.sbuf_base, nc.sbuf_top = snap_sb
        L = self.ln_alloc(1, 0, True, share_pT=True, nbuf=2, nhb=4)
        hb_r = Ring(P, "ghb", 4, [128, D], BF16)
        hT, b_hT = P.sb("ghT", [128, 8, 512], BF16), Buf("ghT")
        uT, b_uT = P.sb("guT", [128, 24, 512], BF16), [Buf("uT%d" % i) for i in range(24)]
        wp_r = Ring(P, "gwp", 3, [128, 8, 512], BF16)
        pUV_r = Ring(P, "gpUV", 2, [128, 512], F32, psum=True)
        pR2 = P.ps("gpR2", [128, 512], F32)
        pR_slots = [(pS, b_pS), (pR2, Buf("gpR2"))]
        mix_r = Ring(P, "gmix", 1, [128, D], F32, psum=True)
        vgb = P.sb("gvgb", [128, 4, SGH], BF16); b_vgb = [Buf("vgb%d" % i) for i in range(4)]
        st = P.sb("gst", [128, 4, 6, 6], F32); b_st = [Buf("gst%d" % i) for i in range(4)]
        mv_r = Ring(P, "gmv", 4, [128, 8], F32)
        res_r = Ring(P, "gres", 2, [128, D], F32)

        pieces = []
        for sp in range(NSUP):
            for pv in range(6):
                pieces.append(SGH + pv * 512)
            for pu in range(6):
                pieces.append(pu * 512)
        wq = []
        nxt = [0]

        def prefetch(n):
            while nxt[0] < len(pieces) and len(wq) < n:
                c0 = pieces[nxt[0]]
                wp, b_wp = wp_r.next()
                P.dma("pool", lambda e: e.dma_start(out=wp[:], in_=self.s_w_in[:, c0:c0 + 512].rearrange("(c p) f -> p c f", p=128)),
                      writes=[b_wp])
                wq.append((wp, b_wp))
                nxt[0] += 1

        def take():
            prefetch(1)
            w = wq.pop(0)
            prefetch(2)
            return w

        hbq = []

        def load_hb(sp):
            for tl in range(4):
                ti = sp * 4 + tl
                hb, b_hb = hb_r.next()
                P.dma("pool", lambda e: e.dma_start(out=hb[:], in_=self.H2[ti * 128:(ti + 1) * 128, :]),
                      reads=[self.b_H["H2"]], writes=[b_hb])
                hbq.append((hb, b_hb))

        def stage_A(sp):
            for tl in range(4):
                hb, b_hb = hbq.pop(0)
                pT, b_pT = L.pT_r.next()
                for c in range(8):
                    P.op("pe", lambda e: e.transpose(pT[:, c, :], hb[:, c * 128:(c + 1) * 128], self.ident[:]),
                         reads=[b_hb, self.b_const], writes=[b_pT])
                P.op("dve", lambda e: e.tensor_copy(out=hT[:, :, tl * 128:(tl + 1) * 128], in_=pT[:]), reads=[b_pT], writes=[b_hT])
            for pv in range(6):
                wp, b_wp = take()
                for tl in range(4):
                    pV, b_pV = pUV_r.next()
                    for c in range(8):
                        P.op("pe", lambda e: e.matmul(pV[:], lhsT=hT[:, c, tl * 128:(tl + 1) * 128], rhs=wp[:, c, :],
                                                      start=(c == 0), stop=False), reads=[b_wp, b_hT], writes=[b_pV])
                    P.op("pe", lambda e: e.matmul(pV[:], lhsT=sel32[:], rhs=L4[:, pv * 512:(pv + 1) * 512],
                                                  start=False, stop=True), reads=[bc], writes=[b_pV])
                    P.op("act", lambda e: e.activation(out=vgb[:, tl, pv * 512:(pv + 1) * 512], in_=pV[:], func=AF.Gelu_apprx_tanh),
                         reads=[b_pV], writes=[b_vgb[tl]])
                    P.op("dve", lambda e: e.bn_stats(out=st[:, tl, pv, :], in_=vgb[:, tl, pv * 512:(pv + 1) * 512]),
                         reads=[b_vgb[tl]], writes=[b_st[tl]])
                    tick()
            for tl in range(4):
                mv, b_mv = mv_r.next()
                P.op("dve", lambda e: e.bn_aggr(out=mv[:, 0:2], in_=st[:, tl, :, :].rearrange("p a b -> p (a b)")),
                     reads=[b_st[tl]], writes=[b_mv])
                P.op("dve", lambda e: e.tensor_scalar(out=mv[:, 2:3], in0=mv[:, 1:2], scalar1=LN_EPS, scalar2=None, op0=ALU.add),
                     reads=[b_mv], writes=[b_mv])
                P.op("pool", lambda e: e.tensor_tensor(out=mv[:, 3:4], in0=mv[:, 2:3], in1=self.mhalf[:], op=ALU.pow),
                     reads=[b_mv, self.b_const], writes=[b_mv])
                P.op("dve", lambda e: e.tensor_scalar(out=vgb[:, tl, :], in0=vgb[:, tl, :], scalar1=mv[:, 0:1], scalar2=mv[:, 3:4],
                                                      op0=ALU.subtract, op1=ALU.mult), reads=[b_vgb[tl], b_mv], writes=[b_vgb[tl]])
                P.op("dve", lambda e: e.tensor_tensor(out=vgb[:, tl, :], in0=vgb[:, tl, :], in1=lgb[:], op=ALU.mult),
                     reads=[b_vgb[tl], bc], writes=[b_vgb[tl]])

        def stage_B(sp):
            for pu in range(6):
                wp, b_wp = take()
                for f4 in range(4):
                    fc = pu * 4 + f4
                    pU, b_pU = pUV_r.next()
                    for c in range(8):
                        P.op("pe", lambda e: e.matmul(pU[:], lhsT=wp[:, c, f4 * 128:(f4 + 1) * 128], rhs=hT[:, c, :],
                                                      start=(c == 0), stop=(c == 7)), reads=[b_wp, b_hT], writes=[b_pU])
                    P.op("act", lambda e: e.activation(out=uT[:, fc, :], in_=pU[:], func=AF.Gelu_apprx_tanh,
                                                       bias=bu[:, fc:fc + 1], scale=1.0), reads=[b_pU, bc], writes=[b_uT[fc]])

        def stage_C(sp):
            for fc in range(24):
                g = fc // 3
                pR, b_pR = pR_slots[fc % 2]
                P.op("pe", lambda e: e.matmul(pR[:], lhsT=L4[:, fc * 128:(fc + 1) * 128],
                                              rhs=R4[:, g, :].unsqueeze(1).to_broadcast([128, 4, 128]),
                                              start=True, stop=False), reads=[bc], writes=[b_pR])
                for tl in range(4):
                    P.op("pe", lambda e: e.matmul(pR[:, tl * 128:(tl + 1) * 128], lhsT=vgb[:, tl, fc * 128:(fc + 1) * 128], rhs=wTb[:, g, :],
                                                  start=False, stop=(tl == 3)), reads=[b_vgb[tl], bc], writes=[b_pR])
                P.op("dve", lambda e: e.tensor_tensor(out=uT[:, fc, :], in0=pR[:], in1=uT[:, fc, :], op=ALU.mult),
                     reads=[b_pR, b_uT[fc]], writes=[b_uT[fc]])
                tick()

        pend_route = []

        def finish_ln(gen, ti):
            try:
                while True:
                    next(gen)
            except StopIteration as stop:
                hb, b_hb = stop.value
            pend_route.append((hb, b_hb, ti))

        def stage_Dproj(sp):
            prev_gen = None
            for tl in range(4):
                ti = sp * 4 + tl
                res, b_res = res_r.next()
                P.dma("sp", lambda e: e.dma_start(out=res[:], in_=self.H2[ti * 128:(ti + 1) * 128, :]),
                      reads=[self.b_H["H2"]], writes=[b_res])
                mix, b_mix = mix_r.next()
                for hf in range(2):
                    for fc in range(24):
                        P.op("pe", lambda e: e.matmul(mix[:, hf * 512:(hf + 1) * 512], lhsT=uT[:, fc, tl * 128:(tl + 1) * 128],
                                                      rhs=wo[:, fc, hf * 512:(hf + 1) * 512], start=(fc == 0), stop=(fc == 23)),
                             reads=[b_uT[fc], bc], writes=[b_mix])
                gen = self.ln_part_gen(L, mix[:], b_mix, res[:], b_res, self.H3, self.b_H["H3"], ti, gmul="dve")
                next(gen)
                if prev_gen is not None:
                    finish_ln(*prev_gen)
                prev_gen = (gen, ti)
            finish_ln(*prev_gen)

        bg = []
        scat = []

        def tick():
            if not bg and pend_route:
                for _ in range(min(2, len(pend_route))):
                    hb, b_hb, ti = pend_route.pop(0)
                    bg.append(self.route_gen(L, hb, b_hb, ti, defer=scat))
            for g_ in list(bg):
                try:
                    next(g_)
                except StopIteration:
                    bg.remove(g_)

        def stage_Droute():
            while bg or pend_route:
                tick()
            while scat:
                scat.pop(0)()

        load_hb(0)
        prefetch(2)
        stage_A(0)
        stage_B(0)
        for sp in range(NSUP):
            if sp + 1 < NSUP:
                load_hb(sp + 1)
            stage_C(sp)
            if sp + 1 < NSUP:
                stage_A(sp + 1)
            stage_Droute()
            stage_Dproj(sp)
            if sp + 1 < NSUP:
                stage_B(sp + 1)
        stage_Droute()

    def build(self, upto=99):
        self.setup()
        self.setup_route()
        if upto >= 1:
            self.phase(self._attn_layer)
        if upto >= 2:
            self.phase(self.moe, 0)
        if upto >= 3:
            self.phase(self.combine, 0, self.H1, self.b_H["H1"], self.H2, self.b_H["H2"])
        if upto >= 4:
            self.phase(self.gmlp)
        if upto >= 5:
            self.phase(self.moe, 1)
        if upto >= 6:
            self.phase(self.combine, 1, self.H3, self.b_H["H3"], self.out, self.b_H["out"])
        self.P.finish()
        return self.nc

    def _attn_layer(self):
        self.setup_attn()
        self.phase(self.proj_qkv)
        self.phase(self.attn)
        self.phase(self.post_attn)


def _t5_bucket_np(n):
    n = np.maximum(n, 0)
    nf = np.maximum(n, 1).astype(np.float32)
    large = 16 + (np.log(nf / np.float32(16)) / np.float32(math.log(128 / 16)) * np.float32(16)).astype(np.int32)
    large = np.minimum(large, 31)
    return np.where(n < 16, n, large)


def prep_shared(inp):
    f = lambda a: np.ascontiguousarray(np.asarray(a, dtype=np.float32))
    rel = f(inp["rel_bias"])
    k = np.arange(128)[:, None]
    q = np.arange(128)[None, :]
    bt = np.stack([rel[_t5_bucket_np(q - k)], rel[_t5_bucket_np(128 + q - k)]], 0)
    bt = np.ascontiguousarray(np.transpose(bt, (0, 3, 1, 2)))
    sh = {
        "bt": bt, "cfar": f(rel[31:32, :]),
        "attn_w_in": f(inp["attn_w_in"][0]), "attn_w_out": f(inp["attn_w_out"][0]),
        "attn_lambda": f(np.asarray(inp["attn_lambda"][0]).reshape(1, 256)), "attn_subln_g": f(inp["attn_subln_g"]),
        "sg_w_in": f(inp["sg_w_in"][0]), "sg_b_in": f(inp["sg_b_in"]), "sg_ln_g": f(inp["sg_ln_g"]), "sg_ln_b": f(inp["sg_ln_b"]),
        "sg_w_s": f(inp["sg_w_s"][0]), "sg_b_s": f(inp["sg_b_s"][0]), "sg_w_out": f(inp["sg_w_out"][0]),
        "moe_wr": f(np.concatenate([np.asarray(inp["moe_w_group"]), np.asarray(inp["moe_w_router"])], axis=-1)),
        "moe_br": f(np.concatenate([np.asarray(inp["moe_b_group"]), np.asarray(inp["moe_b_router"])], axis=-1)),
        "moe_w_gate": f(inp["moe_w_gate"]), "moe_w_up": f(inp["moe_w_up"]), "moe_w_down": f(inp["moe_w_down"]),
        "ln_g": f(inp["ln_g"]), "ln_b": f(inp["ln_b"]),
    }
    return sh


_CACHE = {}


def kernel(**inputs):
    sh = prep_shared(inputs)
    x = np.ascontiguousarray(np.asarray(inputs["x"], dtype=np.float32))
    B, S, _ = x.shape
    nseq = B // N_CORES
    if "nc" not in _CACHE:
        k = K(nseq=nseq, S=S, C=384)
        _CACHE["nc"] = k.build()
    nc = _CACHE["nc"]
    in_maps = []
    for c in range(N_CORES):
        m = dict(sh)
        m["x"] = x[c * nseq:(c + 1) * nseq].reshape(nseq * S, D)
        in_maps.append(m)
    res = run_bass_kernel_spmd(nc, in_maps, core_ids=list(range(N_CORES)))
    out = np.concatenate([np.asarray(r["out"]).reshape(nseq, S, D) for r in res.results], axis=0)
    return out.astype(np.float32, copy=False)
```

```python
import math
import numpy as np
import concourse.bass as bass
import concourse.mybir as mybir
from concourse.bass_utils import run_bass_kernel_spmd
from concourse.alu_op_type import AluOpType as ALU

AF = mybir.ActivationFunctionType
F32, BF16, I32, U32 = mybir.dt.float32, mybir.dt.bfloat16, mybir.dt.int32, mybir.dt.uint32

D = 1024
NH = 8
NE = 64
HID = 512
SGH = 3072
LN_EPS = 1e-5
DEPTH = 2
DN_ALPHA = (2 * DEPTH) ** 0.25
N_CORES = 8


class Buf:
    __slots__ = ("name", "w", "r")

    def __init__(self, name=""):
        self.name = name
        self.w = None
        self.r = {}


class Prog:
    NDMA = {"sp": 24, "act": 4, "pool": 24}

    def __init__(self):
        nc = self.nc = bass.Bass("TRN2", target_bir_lowering=False)
        self.engs = {"pe": nc.tensor, "act": nc.scalar, "dve": nc.vector,
                     "pool": nc.gpsimd, "sp": nc.sync}
        self.sems = {}
        self.count = {}
        for e in self.engs:
            self.sems[e] = nc.alloc_semaphore(name="c_" + e)
            self.count[e] = 0
        self.dma_pool = {}
        self.dma_rr = {}
        self.dma_uses = {}
        for q, n in self.NDMA.items():
            keys = []
            for i in range(n):
                k = "d_%s_%d" % (q, i)
                self.sems[k] = nc.alloc_semaphore(name=k)
                self.dma_uses[k] = 0
                keys.append(k)
            self.dma_pool[q] = keys
            self.dma_rr[q] = 0
        self.known = {e: {} for e in self.engs}
        self.n_inst = 0
        self._names = 0

    def sb(self, name, shape, dt):
        self._names += 1
        return self.nc.alloc_sbuf_tensor("%s_%d" % (name, self._names), list(shape), dt)

    def ps(self, name, shape, dt=F32):
        self._names += 1
        return self.nc.alloc_psum_tensor("%s_%d" % (name, self._names), list(shape), dt)

    def _wait(self, eng, key, val):
        if val <= 0:
            return
        kn = self.known[eng]
        if kn.get(key, 0) >= val:
            return
        self.engs[eng].wait_ge(self.sems[key], val)
        kn[key] = val

    def _deps(self, eng, reads, writes):
        deps = {}

        def add(tok, raw):
            if tok is None:
                return
            k, v = tok
            if k == eng and (not raw or eng == "pe"):
                return
            if deps.get(k, 0) < v:
                deps[k] = v
        for b in reads:
            add(b.w, True)
        for b in writes:
            add(b.w, False)
            for k, v in b.r.items():
                add((k, v), False)
        return deps

    def _mark(self, tok, reads, writes):
        k, v = tok
        for b in reads:
            if b.r.get(k, 0) < v:
                b.r[k] = v
        for b in writes:
            b.w = tok
            b.r = {}

    def op(self, eng, fn, reads=(), writes=()):
        for k, v in self._deps(eng, reads, writes).items():
            self._wait(eng, k, v)
        ins = fn(self.engs[eng])
        self.count[eng] += 1
        ins.then_inc(self.sems[eng], 1)
        tok = (eng, self.count[eng])
        self._mark(tok, reads, writes)
        self.n_inst += 1
        return tok

    def dma(self, q, fn, reads=(), writes=()):
        for k, v in self._deps(q, reads, writes).items():
            self._wait(q, k, v)
        pool = self.dma_pool[q]
        key = pool[self.dma_rr[q] % len(pool)]
        self.dma_rr[q] += 1
        prior = self.dma_uses[key]
        self._wait(q, key, 16 * prior)
        ins = fn(self.engs[q])
        ins.then_inc(self.sems[key], 16)
        self.dma_uses[key] = prior + 1
        tok = (key, 16 * (prior + 1))
        self._mark(tok, reads, writes)
        self.n_inst += 1
        return tok

    def barrier(self):
        for e in self.engs:
            for e2 in self.engs:
                if e2 != e:
                    self._wait(e, e2, self.count[e2])
            for k, n in self.dma_uses.items():
                self._wait(e, k, 16 * n)

    def finish(self):
        for e in self.engs:
            if e != "sp":
                self._wait("sp", e, self.count[e])
        for k, n in self.dma_uses.items():
            self._wait("sp", k, 16 * n)


class Ring:
    def __init__(self, P, name, n, shape, dt, psum=False):
        self.slots = []
        for i in range(n):
            t = P.ps(name, shape, dt) if psum else P.sb(name, shape, dt)
            self.slots.append((t, Buf(name)))
        self.i = 0

    def next(self):
        s = self.slots[self.i % len(self.slots)]
        self.i += 1
        return s


class K:
    def __init__(self, nseq=2, S=4096, C=384, test_outputs=(), lite=False):
        self.nseq, self.S, self.C = nseq, S, C
        self.lite = lite
        self.NT = nseq * S
        self.NTL = self.NT // 128
        self.P = Prog()
        self.nc = self.P.nc
        self.test_outputs = set(test_outputs)
        self._declare_dram()

    def _dt(self, name, shape, dt, kind=None):
        if kind is None:
            kind = "ExternalOutput" if name in self.test_outputs else "Internal"
        return self.nc.dram_tensor(name, list(shape), dt, kind=kind)

    def _declare_dram(self):
        NT, S, nseq, C = self.NT, self.S, self.nseq, self.C
        i = lambda n, s: self._dt(n, s, F32, kind="ExternalInput")
        self.x = i("x", [NT, D])
        self.bt = i("bt", [2, NH, 128, 128])
        self.cfar = i("cfar", [1, NH])
        self.a_w_in = i("attn_w_in", [D, 3 * D])
        self.a_w_out = i("attn_w_out", [D, D])
        self.a_lam = i("attn_lambda", [1, 256])
        self.a_g = i("attn_subln_g", [1, 128])
        self.s_w_in = i("sg_w_in", [D, 2 * SGH])
        self.s_b_in = i("sg_b_in", [1, 2 * SGH])
        self.s_ln_g = i("sg_ln_g", [1, SGH])
        self.s_ln_b = i("sg_ln_b", [1, SGH])
        self.s_w_s = i("sg_w_s", [8, 128, 128])
        self.s_b_s = i("sg_b_s", [8, 128])
        self.s_w_out = i("sg_w_out", [SGH, D])
        self.m_wr = i("moe_wr", [2, D, 72])
        self.m_br = i("moe_br", [2, 72])
        ne = 1 if self.lite else NE
        self.m_wg = i("moe_w_gate", [2, ne, D, HID])
        self.m_wu = i("moe_w_up", [2, ne, D, HID])
        self.m_wd = i("moe_w_down", [2, ne, HID, D])
        self.ln_g = i("ln_g", [2, 2, D])
        self.ln_b = i("ln_b", [2, 2, D])
        self.out = self._dt("out", [NT, D], F32, kind="ExternalOutput")
        self.QKT = self._dt("QKT", [nseq, 16, 128, S], BF16)
        self.V = self._dt("V", [nseq, S, D], BF16)
        self.OT = self._dt("OT", [nseq, NH, 128, S], BF16)
        self.H1 = self._dt("H1", [NT, D], F32)
        self.H2 = self._dt("H2", [NT, D], F32)
        self.H3 = self._dt("H3", [NT, D], F32)
        self.XS = self._dt("XS", [NE * C, D], BF16)
        self.YS = self._dt("YS", [NE * C, D], BF16)
        self.b_QKT = [Buf() for _ in range(nseq)]
        self.b_V = [Buf() for _ in range(nseq)]
        self.b_OT = [Buf() for _ in range(nseq)]
        self.b_H = {n: Buf(n) for n in ("H1", "H2", "H3", "out")}
        self.b_XS = Buf("XS")
        self.b_YS = Buf("YS")

    def setup(self):
        P = self.P
        self.zero_reg = self.nc.gpsimd.to_reg(0.0)
        self.ident = P.sb("ident", [128, 128], BF16); self.b_const = Buf("const")
        bc = self.b_const
        P.op("pool", lambda e: e.memset(self.ident[:], 1.0), writes=[bc])
        P.op("pool", lambda e: e.affine_select(out=self.ident[:], in_=self.ident[:], pattern=[[-1, 128]],
                                                compare_op=ALU.is_equal, fill=self.zero_reg, base=0, channel_multiplier=1),
             reads=[bc], writes=[bc])
        self.ones = P.sb("ones", [128, 128], BF16)
        P.op("pool", lambda e: e.memset(self.ones[:], 1.0), writes=[bc])
        self.ustrict = P.sb("ustrict", [128, 128], BF16)
        P.op("pool", lambda e: e.memset(self.ustrict[:], 1.0), writes=[bc])
        P.op("pool", lambda e: e.affine_select(out=self.ustrict[:], in_=self.ustrict[:], pattern=[[1, 128]],
                                                compare_op=ALU.is_gt, fill=self.zero_reg, base=0, channel_multiplier=-1),
             reads=[bc], writes=[bc])
        self.iota_i = P.sb("iota_i", [128, NE], I32)
        self.iota_e = P.sb("iota_e", [128, NE], F32)
        P.op("pool", lambda e: e.iota(self.iota_i[:], pattern=[[1, NE]], base=0, channel_multiplier=0), writes=[bc])
        P.op("dve", lambda e: e.tensor_copy(out=self.iota_e[:], in_=self.iota_i[:]), reads=[bc], writes=[bc])
        self.mhalf = P.sb("mhalf", [128, 1], F32)
        P.op("dve", lambda e: e.memset(self.mhalf[:], -0.5), writes=[bc])

    def setup_attn(self):
        P = self.P
        bc = self.b_attn_c = Buf("attn_c")
        lam_init = 0.8 - 0.6 * math.exp(-0.3 * 0)
        self.cb = P.sb("cb", [128, NH], F32)
        self.ncb = P.sb("ncb", [128, NH], F32)
        P.dma("sp", lambda e: e.dma_start(out=self.cb[:], in_=self.cfar[0].partition_broadcast(128)), writes=[bc])
        P.op("dve", lambda e: e.tensor_scalar(out=self.ncb[:], in0=self.cb[:], scalar1=-1.0, scalar2=None, op0=ALU.mult),
             reads=[bc], writes=[bc])
        btf = P.sb("btf", [128, 2, NH, 128], F32)
        self.Eb = P.sb("Eb", [128, 2, NH, 128], BF16)
        P.dma("sp", lambda e: e.dma_start(out=btf[:], in_=self.bt.ap().rearrange("d h k q -> k d h q")), writes=[bc])
        for d in range(2):
            for h in range(NH):
                P.op("act", lambda e: e.activation(out=btf[:, d, h, :], in_=btf[:, d, h, :], func=AF.Exp,
                                                   bias=self.ncb[:, h:h + 1], scale=1.0), reads=[bc], writes=[bc])
        for h in range(NH):
            P.op("pool", lambda e: e.affine_select(out=btf[:, 0, h, :], in_=btf[:, 0, h, :], pattern=[[1, 128]],
                                                    compare_op=ALU.is_ge, fill=self.zero_reg, base=0, channel_multiplier=-1),
                 reads=[bc], writes=[bc])
        P.op("dve", lambda e: e.tensor_copy(out=self.Eb[:], in_=btf[:]), reads=[bc], writes=[bc])
        lv = P.sb("lv", [128, 256], F32)
        P.dma("sp", lambda e: e.dma_start(out=lv[:], in_=self.a_lam[0].partition_broadcast(128)), writes=[bc])
        junk = P.sb("junk", [128, 64], F32)
        s12 = P.sb("s12", [128, 2], F32)
        for i in range(2):
            P.op("dve", lambda e: e.scalar_tensor_tensor(out=junk[:], in0=lv[:, 128 * i:128 * i + 64], scalar=1.0, in1=lv[:, 128 * i + 64:128 * i + 128], op0=ALU.mult, op1=ALU.mult, accum_out=s12[:, i:i + 1]),
                 reads=[bc], writes=[bc])
        P.op("act", lambda e: e.activation(out=s12[:], in_=s12[:], func=AF.Exp), reads=[bc], writes=[bc])
        self.neg_lam = P.sb("neg_lam", [128, 1], F32)
        P.op("dve", lambda e: e.scalar_tensor_tensor(out=self.neg_lam[:], in0=s12[:, 1:2], scalar=-lam_init,
                                                     in1=s12[:, 0:1], op0=ALU.add, op1=ALU.subtract),
             reads=[bc], writes=[bc])
        self.gsub = P.sb("gsub", [128, 128], F32)
        P.dma("sp", lambda e: e.dma_start(out=self.gsub[:], in_=self.a_g[0].partition_broadcast(128)), writes=[bc])
        P.op("dve", lambda e: e.tensor_scalar(out=self.gsub[:], in0=self.gsub[:], scalar1=1.0 - lam_init, scalar2=None,
                                              op0=ALU.mult), reads=[bc], writes=[bc])

    def proj_qkv(self):
        P, S = self.P, self.S
        wi = P.sb("wi", [128, 8, 3 * D], BF16); b_wi = Buf("wi")
        for c in range(8):
            for hf in range(2):
                P.dma("pool", lambda e: e.dma_start(out=wi[:, c, hf * 1536:(hf + 1) * 1536],
                                                    in_=self.a_w_in[c * 128:(c + 1) * 128, hf * 1536:(hf + 1) * 1536]),
                      writes=[b_wi])
        xb_r = Ring(P, "xb", 3, [128, D], BF16)
        pT_r = Ring(P, "pT", 2, [128, 8, 128], BF16, psum=True)
        xT_r = Ring(P, "xT", 2, [128, 8, 512], BF16)
        pq_r = Ring(P, "pq", 4, [128, 512], F32, psum=True)
        qs_r = Ring(P, "qs", 2, [128, 16, 512], BF16)
        pv_r = Ring(P, "pv", 2, [128, 512], F32, psum=True)
        vs_r = Ring(P, "vs", 2, [128, D], BF16)
        ev = 0
        for s in range(self.nseq):
            for g in range(S // 512):
                xT, b_xT = xT_r.next()
                for tl in range(4):
                    r0 = s * S + g * 512 + tl * 128
                    xb, b_xb = xb_r.next()
                    P.dma("pool", lambda e: e.dma_start(out=xb[:], in_=self.x[r0:r0 + 128, :]), writes=[b_xb])
                    pT, b_pT = pT_r.next()
                    for c in range(8):
                        P.op("pe", lambda e: e.transpose(pT[:, c, :], xb[:, c * 128:(c + 1) * 128], self.ident[:]),
                             reads=[b_xb, self.b_const], writes=[b_pT])
                    P.op("dve", lambda e: e.tensor_copy(out=xT[:, :, tl * 128:(tl + 1) * 128], in_=pT[:]),
                         reads=[b_pT], writes=[b_xT])
                qs, b_qs = qs_r.next()
                for cg in range(16):
                    pq, b_pq = pq_r.next()
                    for c in range(8):
                        P.op("pe", lambda e: e.matmul(pq[:], lhsT=wi[:, c, cg * 128:(cg + 1) * 128], rhs=xT[:, c, :],
                                                      start=(c == 0), stop=(c == 7)), reads=[b_wi, b_xT], writes=[b_pq])
                    sc = 0.125 if cg < 8 else 1.0
                    if ev % 2 == 0:
                        P.op("act", lambda e: e.activation(out=qs[:, cg, :], in_=pq[:], func=AF.Copy, scale=sc),
                             reads=[b_pq], writes=[b_qs])
                    else:
                        P.op("dve", lambda e: e.tensor_scalar(out=qs[:, cg, :], in0=pq[:], scalar1=sc, scalar2=None,
                                                              op0=ALU.mult), reads=[b_pq], writes=[b_qs])
                    ev += 1
                for q8 in range(2):
                    P.dma("sp", lambda e: e.dma_start(out=self.QKT[s, q8 * 8:(q8 + 1) * 8, :, g * 512:(g + 1) * 512].rearrange("g p t -> p g t"),
                                                      in_=qs[:, q8 * 8:(q8 + 1) * 8, :]), reads=[b_qs])
                for tl in range(4):
                    vs, b_vs = vs_r.next()
                    for hf in range(2):
                        pv, b_pv = pv_r.next()
                        for c in range(8):
                            P.op("pe", lambda e: e.matmul(pv[:], lhsT=xT[:, c, tl * 128:(tl + 1) * 128],
                                                          rhs=wi[:, c, 2048 + hf * 512:2048 + (hf + 1) * 512],
                                                          start=(c == 0), stop=(c == 7)), reads=[b_wi, b_xT], writes=[b_pv])
                        if ev % 2 == 0:
                            P.op("act", lambda e: e.activation(out=vs[:, hf * 512:(hf + 1) * 512], in_=pv[:], func=AF.Copy),
                                 reads=[b_pv], writes=[b_vs])
                        else:
                            P.op("dve", lambda e: e.tensor_copy(out=vs[:, hf * 512:(hf + 1) * 512], in_=pv[:]),
                                 reads=[b_pv], writes=[b_vs])
                        ev += 1
                    t0 = g * 512 + tl * 128
                    P.dma("sp", lambda e: e.dma_start(out=self.V[s, t0:t0 + 128, :], in_=vs[:]),
                          reads=[b_vs])

    def attn(self):
        P, S = self.P, self.S
        nqb = S // 128
        nsb = nqb // 4
        QT_r = Ring(P, "QT", 2, [128, S], BF16)
        KT_r = Ring(P, "KT", 2, [128, S], BF16)
        Va_r = Ring(P, "Va", 2, [128, nqb, 132], BF16)
        for va, b in Va_r.slots:
            P.op("pool", lambda e: e.memset(va[:, :, 128:129], 1.0), writes=[b])
        S_r = [Ring(P, "S%d" % m, 2, [128, 512], F32, psum=True) for m in range(2)]
        PT_r = Ring(P, "PT", 6, [128, 512], BF16)
        Ob = [P.ps("Ob", [128, 512], F32) for _ in range(3)]
        b_Ob = [Buf("Ob%d" % i) for i in range(3)]
        acc = {}
        order = [(0, 0), (0, 1), (0, 2), (0, 3), (1, 0), (1, 1), (1, 2), (1, 3)]
        for n, key in enumerate(order):
            acc[key] = (n // 3, (n % 3) * 130)
        Tb, b_Tb = P.ps("Tb", [128, 4, 128], BF16), Buf("Tb")
        os_r = Ring(P, "os", 2, [128, 512], BF16)

        steps = []
        for s in range(self.nseq):
            for h in range(NH):
                for sbi in range(nsb):
                    i0 = 4 * sbi
                    for j in range(i0 + 4):
                        steps.append((s, h, sbi, j))
        cur = {}

        def load_head(s, h):
            QT, b_QT = QT_r.next(); KT, b_KT = KT_r.next(); Va, b_Va = Va_r.next()
            P.dma("sp", lambda e: e.dma_start(out=QT[:], in_=self.QKT[s, h]), reads=[self.b_QKT[s]], writes=[b_QT])
            P.dma("sp", lambda e: e.dma_start(out=KT[:], in_=self.QKT[s, 8 + h]), reads=[self.b_QKT[s]], writes=[b_KT])
            JB = 8
            for j0 in range(0, nqb, JB):
                j1 = min(nqb, j0 + JB)
                P.dma("sp", lambda e: e.dma_start(out=Va[:, j0:j1, 0:128],
                                                  in_=self.V[s, j0 * 128:j1 * 128, h * 128:(h + 1) * 128].rearrange("(j p) e -> p j e", p=128)),
                      reads=[self.b_V[s]], writes=[b_Va])
            return (QT, b_QT, KT, b_KT, Va, b_Va)

        heads = {}
        hl = [(s, h) for s in range(self.nseq) for h in range(NH)]
        heads[hl[0]] = load_head(*hl[0])

        def emit_S(step):
            s, h, sbi, j = step
            if (s, h) not in heads:
                heads[(s, h)] = load_head(s, h)
            QT, b_QT, KT, b_KT, Va, b_Va = heads[(s, h)]
            i0 = 4 * sbi
            qlo = max(j, i0)
            N = (i0 + 4 - qlo) * 128
            res = []
            for m in range(2):
                St, b_S = S_r[m].next()
                P.op("pe", lambda e: e.matmul(St[:, 0:N], lhsT=KT[m * 64:(m + 1) * 64, j * 128:(j + 1) * 128],
                                              rhs=QT[m * 64:(m + 1) * 64, qlo * 128:(i0 + 4) * 128],
                                              start=True, stop=True), reads=[b_KT, b_QT], writes=[b_S])
                res.append((St, b_S))
            return res

        def emit_rest(step, Sres, touched):
            s, h, sbi, j = step
            QT, b_QT, KT, b_KT, Va, b_Va = heads[(s, h)]
            i0 = 4 * sbi
            qlo = max(j, i0)
            N = (i0 + 4 - qlo) * 128
            for m in range(2):
                St, b_S = Sres[m]
                PT, b_PT = PT_r.next()
                P.op("act", lambda e: e.activation(out=PT[:, 0:N], in_=St[:, 0:N], func=AF.Exp),
                     reads=[b_S], writes=[b_PT])
                for ii in range(qlo - i0, 4):
                    dd = (i0 + ii) - j
                    if dd in (0, 1):
                        c0 = (ii - (qlo - i0)) * 128
                        P.op("dve", lambda e: e.tensor_tensor(out=PT[:, c0:c0 + 128], in0=PT[:, c0:c0 + 128],
                                                              in1=self.Eb[:, dd, h, :], op=ALU.mult),
                             reads=[b_PT, self.b_attn_c], writes=[b_PT])
                for ii in range(qlo - i0, 4):
                    bank, off = acc[(m, ii)]
                    c0 = (ii - (qlo - i0)) * 128
                    first = bank not in touched
                    touched.add(bank)
                    P.op("pe", lambda e: e.matmul(Ob[bank][:, off:off + 129], lhsT=PT[:, c0:c0 + 128],
                                                  rhs=Va[:, j, 0:129], start=first, stop=(j == i0 + ii),
                                                  skip_group_check=True),
                         reads=[b_PT, b_Va], writes=[b_Ob[bank]])

        Osb_r = Ring(P, "Osb", 2, [128, 8, 130], F32)
        cm_r = Ring(P, "cm", 2, [128, 32], F32)
        t4_r = Ring(P, "t4", 2, [128, 4, 128], F32)
        o4_r = Ring(P, "o4", 2, [128, 4, 128], F32)
        q4_r = Ring(P, "q4", 2, [128, 4, 128], F32)
        ob3_r = Ring(P, "ob3", 3, [128, 4, 128], BF16)

        def combine_gen(s, h, sbi):
            i0 = 4 * sbi
            Osb, b_Osb = Osb_r.next()
            for bank in range(3):
                n0 = bank * 3
                n1 = min(8, n0 + 3)
                P.op("dve", lambda e: e.tensor_copy(out=Osb[:, n0:n1, :].rearrange("p a b -> p (a b)"),
                                                    in_=Ob[bank][:, 0:(n1 - n0) * 130]),
                     reads=[b_Ob[bank]], writes=[b_Osb])
            yield
            cm, b_cm = cm_r.next()
            R = [b_cm]
            bc3 = lambda ap: ap.unsqueeze(2).to_broadcast([128, 4, 128])
            P.op("dve", lambda e: e.reciprocal(out=cm[:, 0:8].unsqueeze(2), in_=Osb[:, :, 128:129]), reads=[b_Osb], writes=R)
            yield
            P.op("dve", lambda e: e.tensor_scalar(out=cm[:, 8:12], in0=cm[:, 4:8], scalar1=self.neg_lam[:, 0:1], scalar2=None,
                                                  op0=ALU.mult), reads=R + [self.b_attn_c], writes=R)
            t4, b_t4 = t4_r.next()
            P.op("dve", lambda e: e.tensor_tensor(out=t4[:], in0=Osb[:, 0:4, 0:128], in1=bc3(cm[:, 0:4]), op=ALU.mult),
                 reads=[b_Osb] + R, writes=[b_t4])
            yield
            o4, b_o4 = o4_r.next()
            P.op("dve", lambda e: e.tensor_tensor(out=o4[:], in0=Osb[:, 4:8, 0:128], in1=bc3(cm[:, 8:12]), op=ALU.mult),
                 reads=[b_Osb] + R, writes=[b_o4])
            yield
            P.op("dve", lambda e: e.tensor_tensor(out=o4[:], in0=o4[:], in1=t4[:], op=ALU.add), reads=[b_o4, b_t4], writes=[b_o4])
            yield
            q4, b_q4 = q4_r.next()
            P.op("dve", lambda e: e.tensor_tensor(out=q4[:], in0=o4[:], in1=o4[:], op=ALU.mult), reads=[b_o4], writes=[b_q4])
            yield
            P.op("dve", lambda e: e.tensor_reduce(out=cm[:, 12:16], in_=q4[:], axis=mybir.AxisListType.X, op=ALU.add),
                 reads=[b_q4], writes=R)
            yield
            P.op("dve", lambda e: e.tensor_scalar(out=cm[:, 16:20], in0=cm[:, 12:16], scalar1=1.0 / 128, scalar2=LN_EPS,
                                                  op0=ALU.mult, op1=ALU.add), reads=R, writes=R)
            P.op("pool", lambda e: e.tensor_tensor(out=cm[:, 20:24], in0=cm[:, 16:20], in1=self.mhalf[:, 0:1].to_broadcast([128, 4]),
                                                   op=ALU.pow), reads=R + [self.b_const], writes=R)
            yield
            yield
            P.op("dve", lambda e: e.tensor_tensor(out=o4[:], in0=o4[:], in1=bc3(cm[:, 20:24]), op=ALU.mult),
                 reads=[b_o4] + R, writes=[b_o4])
            yield
            ob, b_ob = ob3_r.next()
            P.op("dve", lambda e: e.tensor_tensor(out=ob[:], in0=o4[:], in1=self.gsub[:].unsqueeze(1).to_broadcast([128, 4, 128]),
                                                  op=ALU.mult), reads=[b_o4, self.b_attn_c], writes=[b_ob])
            yield
            yield
            for ii in range(4):
                P.op("pe", lambda e: e.transpose(Tb[:, ii, :], ob[:, ii, :], self.ident[:]),
                     reads=[b_ob, self.b_const], writes=[b_Tb])
            os_, b_os = os_r.next()
            P.op("dve", lambda e: e.tensor_copy(out=os_[:], in_=Tb[:].rearrange("p a b -> p (a b)")),
                 reads=[b_Tb], writes=[b_os])
            P.dma("sp", lambda e: e.dma_start(out=self.OT[s, h, :, i0 * 128:(i0 + 4) * 128], in_=os_[:]),
                  reads=[b_os])

        def finish_gen(g):
            for _ in g:
                pass

        Sres_next = emit_S(steps[0])
        touched = set()
        active = []
        for t, step in enumerate(steps):
            Sres = Sres_next
            s, h, sbi, j = step
            if j == 0 and sbi == 0:
                idx = hl.index((s, h))
                if idx + 1 < len(hl) and hl[idx + 1] not in heads:
                    heads[hl[idx + 1]] = load_head(*hl[idx + 1])
            if t + 1 < len(steps):
                Sres_next = emit_S(steps[t + 1])
            if j == 0:
                touched = set()
            emit_rest(step, Sres, touched)
            for g in list(active):
                try:
                    next(g)
                except StopIteration:
                    active.remove(g)
            if j == 4 * sbi + 3:
                while len(active) >= 2:
                    finish_gen(active.pop(0))
                g = combine_gen(s, h, sbi)
                next(g)
                active.append(g)
        for g in active:
            finish_gen(g)

    def phase(self, fn, *a, **k):
        nc = self.nc
        snap = (nc.psum_base, nc.psum_top, nc.sbuf_base, nc.sbuf_top)
        fn(*a, **k)
        self.P.barrier()
        nc.psum_base, nc.psum_top, nc.sbuf_base, nc.sbuf_top = snap

    def setup_route(self):
        P = self.P
        self.SL = P.sb("SL", [128, self.NTL, 2], I32)
        self.GT = P.sb("GT", [128, self.NTL, 2], F32)
        self.cnt = P.sb("cnt", [128, NE], F32)
        self.b_SL = [Buf("SL%d" % i) for i in range(self.NTL)]
        self.b_cnt = Buf("cnt")
        self.bound_reg = self.nc.gpsimd.to_reg(NE * self.C - 1)

    def ln_alloc(self, layer, j, route, share_pT=False, nbuf=2, nhb=None, xn_psum=0):
        P = self.P

        class L:
            pass
        L.layer, L.j, L.route = layer, j, route
        L.g = P.sb("lng", [128, D], F32); L.b = P.sb("lnb", [128, D], F32); L.b_gb = Buf("lngb")
        P.dma("sp", lambda e: e.dma_start(out=L.g[:], in_=self.ln_g[layer, j].partition_broadcast(128)), writes=[L.b_gb])
        P.dma("sp", lambda e: e.dma_start(out=L.b[:], in_=self.ln_b[layer, j].partition_broadcast(128)), writes=[L.b_gb])
        L.tt_r = Ring(P, "ltt", nbuf, [128, D], F32)
        L.st_r = Ring(P, "lst", nbuf, [128, 2, 6], F32)
        L.mv_r = Ring(P, "lmv", nbuf, [128, 8], F32)
        L.xn_psum = xn_psum
        if xn_psum:
            L.xn_r = Ring(P, "lxnp", xn_psum, [128, D], F32, psum=True)
        else:
            L.xn_r = Ring(P, "lxn", max(1, nbuf - 1), [128, D], F32)
        L.h_r = Ring(P, "lh", nbuf, [128, D], F32)
        if route:
            L.hb_r = Ring(P, "lhb", nhb or nbuf, [128, D], BF16)
            L.pT_r = Ring(P, "lpT", 1, [128, 8, 128], BF16, psum=True)
            L.hT_r = Ring(P, "lhT", max(1, nbuf - 1), [128, 8, 128], BF16)
            L.wr = P.sb("wr", [128, 8, 72], BF16); L.br = P.sb("br", [128, 72], F32); L.b_wr = Buf("wr")
            P.dma("pool", lambda e: e.dma_start(out=L.wr[:], in_=self.m_wr[layer].rearrange("(c p) f -> p c f", p=128)),
                  writes=[L.b_wr])
            P.dma("sp", lambda e: e.dma_start(out=L.br[:], in_=self.m_br[layer].partition_broadcast(128)), writes=[L.b_wr])
            L.pLC = P.ps("pLC", [128, 512], F32); L.b_pLC = Buf("pLC")
            L.rt_r = Ring(P, "rt", nbuf, [128, 200], F32)
            L.i8_r = Ring(P, "i8", nbuf, [128, 8], U32)
            L.oh_r = Ring(P, "oh", nbuf, [128, 2, NE], F32)
            L.M_r = Ring(P, "Mb", nbuf, [128, NE], BF16)
            L.pf_r = Ring(P, "pf", nbuf, [128, NE], F32)
            L.jk_r = Ring(P, "rjk", nbuf, [128, NE], F32)
            L.ss_r = Ring(P, "ss", nbuf + 3, [128, 2], I32)
            P.op("dve", lambda e: e.memset(self.cnt[:], 0.0), writes=[self.b_cnt])
        return L

    @staticmethod
    def run_gens(gens):
        gens = list(gens)
        while gens:
            for g in list(gens):
                try:
                    next(g)
                except StopIteration:
                    gens.remove(g)

    @staticmethod
    def run_fg_bg(fg, bg):
        fg = list(fg)
        while fg:
            for g in list(fg):
                try:
                    next(g)
                except StopIteration:
                    fg.remove(g)
            for g in list(bg):
                try:
                    next(g)
                except StopIteration:
                    bg.remove(g)

    def ln_part_gen(self, L, mix, b_mix, resid, b_resid, Hd, b_Hd, ti, gmul="pool"):
        P = self.P
        tt, b_tt = L.tt_r.next()
        P.op("dve", lambda e: e.scalar_tensor_tensor(out=tt[:], in0=resid, scalar=DN_ALPHA, in1=mix,
                                                     op0=ALU.mult, op1=ALU.add), reads=[b_resid, b_mix], writes=[b_tt])
        yield
        h, b_h = yield from self.ln_core_gen(L, tt, b_tt, gmul)
        P.dma("sp", lambda e: e.dma_start(out=Hd[ti * 128:(ti + 1) * 128, :], in_=h[:]), reads=[b_h])
        if not L.route:
            return None
        hb, b_hb = L.hb_r.next()
        P.op("act", lambda e: e.activation(out=hb[:], in_=h[:], func=AF.Copy), reads=[b_h], writes=[b_hb])
        yield
        return hb, b_hb

    def ln_route_gen(self, L, mix, b_mix, resid, b_resid, Hd, b_Hd, ti, gmul="pool"):
        r = yield from self.ln_part_gen(L, mix, b_mix, resid, b_resid, Hd, b_Hd, ti, gmul)
        if r is not None:
            yield from self.route_gen(L, r[0], r[1], ti)

    def ln_core_gen(self, L, tt, b_tt, gmul="pool", xn_slot=None):
        P = self.P
        st, b_st = L.st_r.next()
        for i in range(2):
            P.op("dve", lambda e: e.bn_stats(out=st[:, i, :], in_=tt[:, i * 512:(i + 1) * 512]), reads=[b_tt], writes=[b_st])
        yield
        mv, b_mv = L.mv_r.next()
        P.op("dve", lambda e: e.bn_aggr(out=mv[:, 0:2], in_=st[:].rearrange("p a b -> p (a b)")), reads=[b_st], writes=[b_mv])
        yield
        P.op("dve", lambda e: e.tensor_scalar(out=mv[:, 2:3], in0=mv[:, 1:2], scalar1=LN_EPS, scalar2=None, op0=ALU.add),
             reads=[b_mv], writes=[b_mv])
        yield
        P.op("pool", lambda e: e.tensor_tensor(out=mv[:, 3:4], in0=mv[:, 2:3], in1=self.mhalf[:], op=ALU.pow),
             reads=[b_mv, self.b_const], writes=[b_mv])
        yield
        P.op("dve", lambda e: e.scalar_tensor_tensor(out=mv[:, 4:5], in0=mv[:, 0:1], scalar=-1.0, in1=mv[:, 3:4],
                                                     op0=ALU.mult, op1=ALU.mult), reads=[b_mv], writes=[b_mv])
        yield
        xn, b_xn = xn_slot if xn_slot is not None else L.xn_r.next()
        P.op("act", lambda e: e.activation(out=xn[:], in_=tt[:], func=AF.Identity, bias=mv[:, 4:5], scale=mv[:, 3:4]),
             reads=[b_tt, b_mv], writes=[b_xn])
        P.op("dve" if L.xn_psum else gmul, lambda e: e.tensor_tensor(out=xn[:], in0=xn[:], in1=L.g[:], op=ALU.mult),
             reads=[b_xn, L.b_gb], writes=[b_xn])
        h, b_h = L.h_r.next()
        P.op("dve", lambda e: e.tensor_tensor(out=h[:], in0=xn[:], in1=L.b[:], op=ALU.add),
             reads=[b_xn, L.b_gb], writes=[b_h])
        yield
        return h, b_h

    def route_gen(self, L, hb, b_hb, ti, defer=None):
        P, C = self.P, self.C
        BIG = 1.0e4
        pT, b_pT = L.pT_r.next()
        for c in range(8):
            P.op("pe", lambda e: e.transpose(pT[:, c, :], hb[:, c * 128:(c + 1) * 128], self.ident[:]),
                 reads=[b_hb, self.b_const], writes=[b_pT])
        hT, b_hT = L.hT_r.next()
        P.op("dve", lambda e: e.tensor_copy(out=hT[:], in_=pT[:]), reads=[b_pT], writes=[b_hT])
        pL, b_pL = L.pLC[:, 0:128], L.b_pLC
        for c in range(8):
            P.op("pe", lambda e: e.matmul(pL[:, 0:72], lhsT=hT[:, c, :], rhs=L.wr[:, c, :], start=(c == 0), stop=(c == 7)),
                 reads=[b_hT, L.b_wr], writes=[b_pL])
        rt, b_rt = L.rt_r.next()
        R = [b_rt]

        def dve(fn, reads=(), writes=()):
            P.op("dve", fn, reads=list(reads) + R, writes=list(writes) + R)
        P.op("dve", lambda e: e.tensor_tensor(out=rt[:, 0:72], in0=pL[:, 0:72], in1=L.br[:], op=ALU.add),
             reads=[b_pL, L.b_wr], writes=R)
        yield
        dve(lambda e: e.max(out=rt[:, 72:80], in_=rt[:, 0:8]))
        yield
        dve(lambda e: e.tensor_scalar(out=rt[:, 80:81], in0=rt[:, 72:73], scalar1=-1.0, scalar2=None, op0=ALU.mult))
        dve(lambda e: e.tensor_scalar(out=rt[:, 91:99], in0=rt[:, 0:8], scalar1=rt[:, 72:73], scalar2=None, op0=ALU.is_equal))
        yield
        P.op("act", lambda e: e.activation(out=rt[:, 81:89], in_=rt[:, 0:8], func=AF.Exp, bias=rt[:, 80:81], scale=1.0,
                                           accum_out=rt[:, 89:90]), reads=R, writes=R)
        dve(lambda e: e.tensor_scalar(out=rt[:, 99:107], in0=rt[:, 91:99], scalar1=BIG, scalar2=-BIG, op0=ALU.mult, op1=ALU.add))
        yield
        dve(lambda e: e.tensor_tensor(out=rt[:, 107:171].rearrange("p (g j) -> p g j", g=8),
                                      in0=rt[:, 8:72].rearrange("p (g j) -> p g j", g=8),
                                      in1=rt[:, 99:107].unsqueeze(2).to_broadcast([128, 8, 8]), op=ALU.add))
        yield
        dve(lambda e: e.max(out=rt[:, 171:179], in_=rt[:, 107:171]))
        yield
        i8, b_i8 = L.i8_r.next()
        dve(lambda e: e.max_index(out=i8[:], in_max=rt[:, 171:179], in_values=rt[:, 107:171]), writes=[b_i8])
        dve(lambda e: e.tensor_tensor(out=rt[:, 181:182], in0=rt[:, 172:173], in1=rt[:, 171:172], op=ALU.subtract))
        yield
        dve(lambda e: e.tensor_copy(out=rt[:, 179:181], in_=i8[:, 0:2]), reads=[b_i8])
        P.op("act", lambda e: e.activation(out=rt[:, 182:183], in_=rt[:, 181:182], func=AF.Exp), reads=R, writes=R)
        yield
        oh, b_oh = L.oh_r.next()
        for k in range(2):
            dve(lambda e: e.tensor_scalar(out=oh[:, k, :], in0=self.iota_e[:], scalar1=rt[:, 179 + k:180 + k], scalar2=None,
                                          op0=ALU.is_equal), reads=[self.b_const], writes=[b_oh])
        yield
        Mb, b_M = L.M_r.next()
        P.op("dve", lambda e: e.tensor_tensor(out=Mb[:], in0=oh[:, 0, :], in1=oh[:, 1, :], op=ALU.add),
             reads=[b_oh], writes=[b_M])
        yield
        pC, b_pC = L.pLC[:, 128:256].rearrange("p (a b) -> p a b", a=2), L.b_pLC
        P.op("pe", lambda e: e.matmul(pC[:, 0, :], lhsT=self.ustrict[:], rhs=Mb[:], start=True, stop=True),
             reads=[b_M, self.b_const], writes=[b_pC])
        P.op("pe", lambda e: e.matmul(pC[:, 1, :], lhsT=self.ones[:], rhs=Mb[:], start=True, stop=True),
             reads=[b_M, self.b_const], writes=[b_pC])
        pf, b_pf = L.pf_r.next()
        P.op("dve", lambda e: e.tensor_tensor(out=pf[:], in0=pC[:, 0, :], in1=self.cnt[:], op=ALU.add),
             reads=[b_pC, self.b_cnt], writes=[b_pf])
        P.op("dve", lambda e: e.tensor_tensor(out=self.cnt[:], in0=pC[:, 1, :], in1=self.cnt[:], op=ALU.add),
             reads=[b_pC, self.b_cnt], writes=[self.b_cnt])
        yield
        dve(lambda e: e.reciprocal(out=rt[:, 90:91], in_=rt[:, 89:90]))
        dve(lambda e: e.tensor_scalar(out=rt[:, 183:184], in0=rt[:, 182:183], scalar1=1.0, scalar2=None, op0=ALU.add))
        yield
        dve(lambda e: e.reciprocal(out=rt[:, 184:185], in_=rt[:, 183:184]))
        yield
        dve(lambda e: e.tensor_tensor(out=rt[:, 185:186], in0=rt[:, 184:185], in1=rt[:, 90:91], op=ALU.mult))
        yield
        dve(lambda e: e.tensor_tensor(out=rt[:, 186:187], in0=rt[:, 185:186], in1=rt[:, 182:183], op=ALU.mult))
        yield
        jk, b_jk = L.jk_r.next()
        for k in range(2):
            dve(lambda e: e.scalar_tensor_tensor(out=jk[:], in0=oh[:, k, :], scalar=1.0, in1=pf[:],
                                                 op0=ALU.mult, op1=ALU.mult, accum_out=rt[:, 187 + k:188 + k]),
                reads=[b_oh, b_pf], writes=[b_jk])
        yield
        dve(lambda e: e.tensor_scalar(out=rt[:, 189:191], in0=rt[:, 187:189], scalar1=float(C), scalar2=None, op0=ALU.is_lt))
        dve(lambda e: e.scalar_tensor_tensor(out=rt[:, 191:193], in0=rt[:, 179:181], scalar=float(C), in1=rt[:, 187:189],
                                             op0=ALU.mult, op1=ALU.add))
        yield
        dve(lambda e: e.tensor_tensor(out=rt[:, 193:195], in0=rt[:, 191:193], in1=rt[:, 189:191], op=ALU.mult))
        dve(lambda e: e.tensor_scalar(out=rt[:, 195:197], in0=rt[:, 189:191], scalar1=-1.0e6, scalar2=1.0e6,
                                      op0=ALU.mult, op1=ALU.add))
        yield
        dve(lambda e: e.tensor_copy(out=self.SL[:, ti, :], in_=rt[:, 193:195]), writes=[self.b_SL[ti]])
        dve(lambda e: e.tensor_tensor(out=rt[:, 197:199], in0=rt[:, 195:197], in1=rt[:, 191:193], op=ALU.add))
        yield
        ss, b_ss = L.ss_r.next()
        dve(lambda e: e.tensor_copy(out=ss[:], in_=rt[:, 197:199]), writes=[b_ss])
        dve(lambda e: e.tensor_tensor(out=self.GT[:, ti, :], in0=rt[:, 185:187], in1=rt[:, 189:191], op=ALU.mult),
            writes=[self.b_SL[ti]])
        yield
        def scatter():
            for k in range(2):
                P.dma("pool", lambda e: e.indirect_dma_start(out=self.XS[:, :],
                                                             out_offset=bass.IndirectOffsetOnAxis(ap=ss[:, k:k + 1], axis=0),
                                                             in_=hb[:, :], in_offset=None,
                                                             bounds_check=self.bound_reg, oob_is_err=False),
                      reads=[b_hb, b_ss])
        if defer is None:
            scatter()
        else:
            defer.append(scatter)

    def post_attn(self):
        P, S = self.P, self.S
        wo = P.sb("wo", [128, NH, D], BF16); b_wo = Buf("wo")
        P.dma("pool", lambda e: e.dma_start(out=wo[:], in_=self.a_w_out.ap().rearrange("(h p) f -> p h f", p=128)),
              writes=[b_wo])
        L = self.ln_alloc(0, 0, True, nbuf=4, xn_psum=1, nhb=8)
        bgr = []
        ot_r = Ring(P, "ot4", 2, [128, NH, 512], BF16)
        x_r = Ring(P, "xres", 3, [128, D], F32)
        mix_r = Ring(P, "mix", 2, [128, D], F32, psum=True)
        prev = []
        for s in range(self.nseq):
            for g in range(S // 512):
                ot, b_ot = ot_r.next()
                P.dma("sp", lambda e: e.dma_start(out=ot[:], in_=self.OT[s, :, :, g * 512:(g + 1) * 512].rearrange("h p t -> p h t")),
                      reads=[self.b_OT[s]], writes=[b_ot])
                cur = []
                lgens = []
                for tl in range(4):
                    ti = (s * S + g * 512 + tl * 128) // 128
                    xr, b_xr = x_r.next()
                    P.dma("sp", lambda e: e.dma_start(out=xr[:], in_=self.x[ti * 128:(ti + 1) * 128, :]), writes=[b_xr])
                    mix, b_mix = mix_r.next()
                    for hf in range(2):
                        for h in range(NH):
                            P.op("pe", lambda e: e.matmul(mix[:, hf * 512:(hf + 1) * 512], lhsT=ot[:, h, tl * 128:(tl + 1) * 128],
                                                          rhs=wo[:, h, hf * 512:(hf + 1) * 512], start=(h == 0), stop=(h == NH - 1)),
                                 reads=[b_ot, b_wo], writes=[b_mix])

                    def lgen(mix=mix, b_mix=b_mix, xr=xr, b_xr=b_xr, ti=ti):
                        r = yield from self.ln_part_gen(L, mix[:], b_mix, xr[:], b_xr, self.H1, self.b_H["H1"], ti)
                        cur.append((r[0], r[1], ti))
                    gen = lgen()
                    next(gen)
                    lgens.append(gen)
                    if len(lgens) == 2:
                        self.run_fg_bg(lgens, bgr)
                        lgens = []
                self.run_gens(bgr)
                bgr = [self.route_gen(L, hb, b_hb, ti) for hb, b_hb, ti in cur]
        self.run_gens(bgr)

    def moe(self, layer):
        P, C = self.P, self.C
        nb = C // 128
        wg_r = Ring(P, "wg", 2, [128, 8, HID], BF16)
        wu_r = Ring(P, "wu", 2, [128, 8, HID], BF16)
        wd_r = Ring(P, "wd", 2, [128, 4, D], BF16)
        xs_r = Ring(P, "xs", 2, [128, nb, D], BF16)
        pT_r = Ring(P, "mpT", 2, [128, 8, 128], BF16, psum=True)
        xT_r = Ring(P, "mxT", 2, [128, 8, C], BF16)
        pG_r = Ring(P, "pG", 2, [128, 512], F32, psum=True)
        pU_r = Ring(P, "pU", 2, [128, 512], F32, psum=True)
        pY_r = Ring(P, "pY", 2, [128, 512], F32, psum=True)
        sg_r = Ring(P, "sg", 2, [128, C], F32)
        hT_r = Ring(P, "hT", 2, [128, 4, C], BF16)
        y_r = Ring(P, "y", 3, [128, D], BF16)
        loaded = {}

        def load(e):
            wg, b_wg = wg_r.next(); wu, b_wu = wu_r.next(); wd, b_wd = wd_r.next(); xs, b_xs = xs_r.next()
            P.dma("sp", lambda en: en.dma_start(out=xs[:], in_=self.XS[e * C:(e + 1) * C, :].rearrange("(b p) d -> p b d", p=128)),
                  reads=[self.b_XS], writes=[b_xs])
            P.dma("pool", lambda en: en.dma_start(out=wg[:], in_=self.m_wg[layer, e].rearrange("(c p) f -> p c f", p=128)),
                  writes=[b_wg])
            P.dma("pool", lambda en: en.dma_start(out=wu[:], in_=self.m_wu[layer, e].rearrange("(c p) f -> p c f", p=128)),
                  writes=[b_wu])
            for hf in range(2):
                P.dma("pool", lambda en: en.dma_start(out=wd[:, :, hf * 512:(hf + 1) * 512],
                                                      in_=self.m_wd[layer, e, :, hf * 512:(hf + 1) * 512].rearrange("(c p) f -> p c f", p=128)),
                      writes=[b_wd])
            loaded[e] = (wg, b_wg, wu, b_wu, wd, b_wd, xs, b_xs)

        load(0)
        ev = 0
        for e in range(NE):
            if e + 1 < NE:
                load(e + 1)
            wg, b_wg, wu, b_wu, wd, b_wd, xs, b_xs = loaded.pop(e)
            xT, b_xT = xT_r.next()
            for b in range(nb):
                pT, b_pT = pT_r.next()
                for c in range(8):
                    P.op("pe", lambda en: en.transpose(pT[:, c, :], xs[:, b, c * 128:(c + 1) * 128], self.ident[:]),
                         reads=[b_xs, self.b_const], writes=[b_pT])
                if ev % 2 == 0:
                    P.op("act", lambda en: en.activation(out=xT[:, :, b * 128:(b + 1) * 128], in_=pT[:], func=AF.Copy),
                         reads=[b_pT], writes=[b_xT])
                else:
                    P.op("dve", lambda en: en.tensor_copy(out=xT[:, :, b * 128:(b + 1) * 128], in_=pT[:]),
                         reads=[b_pT], writes=[b_xT])
                ev += 1
            hT, b_hT = hT_r.next()
            for hc in range(4):
                pG, b_pG = pG_r.next(); pU, b_pU = pU_r.next()
                for c in range(8):
                    P.op("pe", lambda en: en.matmul(pG[:, 0:C], lhsT=wg[:, c, hc * 128:(hc + 1) * 128], rhs=xT[:, c, :],
                                                    start=(c == 0), stop=(c == 7)), reads=[b_wg, b_xT], writes=[b_pG])
                for c in range(8):
                    P.op("pe", lambda en: en.matmul(pU[:, 0:C], lhsT=wu[:, c, hc * 128:(hc + 1) * 128], rhs=xT[:, c, :],
                                                    start=(c == 0), stop=(c == 7)), reads=[b_wu, b_xT], writes=[b_pU])
                sg, b_sg = sg_r.next()
                P.op("act", lambda en: en.activation(out=sg[:], in_=pG[:, 0:C], func=AF.Silu), reads=[b_pG], writes=[b_sg])
                P.op("dve", lambda en: en.tensor_tensor(out=hT[:, hc, :], in0=sg[:], in1=pU[:, 0:C], op=ALU.mult),
                     reads=[b_sg, b_pU], writes=[b_hT])
            for b in range(nb):
                y, b_y = y_r.next()
                for hf in range(2):
                    pY, b_pY = pY_r.next()
                    for hc in range(4):
                        P.op("pe", lambda en: en.matmul(pY[:], lhsT=hT[:, hc, b * 128:(b + 1) * 128],
                                                        rhs=wd[:, hc, hf * 512:(hf + 1) * 512], start=(hc == 0), stop=(hc == 3)),
                             reads=[b_hT, b_wd], writes=[b_pY])
                    if ev % 2 == 0:
                        P.op("act", lambda en: en.activation(out=y[:, hf * 512:(hf + 1) * 512], in_=pY[:], func=AF.Copy),
                             reads=[b_pY], writes=[b_y])
                    else:
                        P.op("dve", lambda en: en.tensor_copy(out=y[:, hf * 512:(hf + 1) * 512], in_=pY[:]),
                             reads=[b_pY], writes=[b_y])
                    ev += 1
                r0 = e * C + b * 128
                P.dma("sp", lambda en: en.dma_start(out=self.YS[r0:r0 + 128, :], in_=y[:]), reads=[b_y])

    def combine(self, layer, Hin, b_Hin, Hout, b_Hout):
        P = self.P
        L = self.ln_alloc(layer, 1, False, nbuf=3, xn_psum=3)
        PF = 3
        G = 3
        y0_r = Ring(P, "y0", PF + G, [128, D], BF16)
        y1_r = Ring(P, "y1", PF + G, [128, D], BF16)
        hi_r = Ring(P, "hin", PF + G, [128, D], F32)
        loads = {}

        def issue(ti):
            ys = []
            for k, r in enumerate((y0_r, y1_r)):
                y, b_y = r.next()
                P.dma("pool", lambda e: e.indirect_dma_start(out=y[:, :], out_offset=None, in_=self.YS[:, :],
                                                             in_offset=bass.IndirectOffsetOnAxis(ap=self.SL[:, ti, k:k + 1], axis=0)),
                      reads=[self.b_YS, self.b_SL[ti]], writes=[b_y])
                ys.append((y, b_y))
            hi, b_hi = hi_r.next()
            P.dma("sp", lambda e: e.dma_start(out=hi[:], in_=Hin[ti * 128:(ti + 1) * 128, :]), reads=[b_Hin], writes=[b_hi])
            loads[ti] = (ys, hi, b_hi)

        def tile_gen(ti):
            ys, hi, b_hi = loads.pop(ti)
            u, b_u = L.xn_r.next()
            P.op("act", lambda e: e.activation(out=u[:], in_=hi[:], func=AF.Copy, scale=DN_ALPHA), reads=[b_hi], writes=[b_u])
            yield
            P.op("dve", lambda e: e.scalar_tensor_tensor(out=u[:], in0=ys[0][0][:], scalar=self.GT[:, ti, 0:1], in1=u[:],
                                                         op0=ALU.mult, op1=ALU.add),
                 reads=[ys[0][1], self.b_SL[ti], b_u], writes=[b_u])
            yield
            tt, b_tt = L.tt_r.next()
            P.op("dve", lambda e: e.scalar_tensor_tensor(out=tt[:], in0=ys[1][0][:], scalar=self.GT[:, ti, 1:2], in1=u[:],
                                                         op0=ALU.mult, op1=ALU.add),
                 reads=[ys[1][1], self.b_SL[ti], b_u], writes=[b_tt])
            yield
            h, b_h = yield from self.ln_core_gen(L, tt, b_tt, "dve", xn_slot=(u, b_u))
            P.dma("sp", lambda e: e.dma_start(out=Hout[ti * 128:(ti + 1) * 128, :], in_=h[:]), reads=[b_h])

        NTL = self.NTL
        for ti in range(min(PF, NTL)):
            issue(ti)
        for t0 in range(0, NTL, G):
            tiles = list(range(t0, min(NTL, t0 + G)))
            for ti in tiles:
                if ti + PF < NTL:
                    issue(ti + PF)
            self.run_gens([tile_gen(ti) for ti in tiles])

    def gmlp(self):
        P, nc = self.P, self.nc
        NSUP = self.NT // 512
        bc = Buf("gconst")
        wo = P.sb("gwo", [128, 24, D], BF16)
        for q in range(6):
            P.dma("pool", lambda e: e.dma_start(out=wo[:, q * 4:(q + 1) * 4, :],
                                                in_=self.s_w_out[q * 512:(q + 1) * 512, :].rearrange("(c p) f -> p c f", p=128)),
                  writes=[bc])
        bu = P.sb("gbu", [128, 24], F32)
        with nc.allow_non_contiguous_dma(reason="one-time per-partition bias columns"):
            P.dma("sp", lambda e: e.dma_start(out=bu[:], in_=self.s_b_in[0, 0:SGH].rearrange("(c p) -> p c", p=128)), writes=[bc])
        lgb = P.sb("glgb", [128, SGH], BF16)
        for hf in range(2):
            P.dma("pool", lambda e: e.dma_start(out=lgb[:, hf * 1536:(hf + 1) * 1536],
                                                in_=self.s_ln_g[0, hf * 1536:(hf + 1) * 1536].partition_broadcast(128)), writes=[bc])
        sel32 = P.sb("gsel32", [128, 128], BF16)
        P.op("pool", lambda e: e.memset(sel32[:], 0.0), writes=[bc])
        P.op("pool", lambda e: e.memset(sel32[32:33, :], 1.0), reads=[bc], writes=[bc])
        wTb = P.sb("gwTb", [128, 8, 128], BF16)
        L4 = P.sb("gL4", [128, SGH], BF16)
        P.op("pool", lambda e: e.memset(L4[:], 0.0), writes=[bc])
        bv = L4[32:33, :]
        P.dma("pool", lambda e: e.dma_start(out=bv, in_=self.s_b_in[0:1, SGH:2 * SGH]), reads=[bc], writes=[bc])
        R4 = P.sb("gR4", [128, 8, 128], BF16)
        P.op("pool", lambda e: e.memset(R4[:], 0.0), writes=[bc])
        snap_sb = (nc.sbuf_base, nc.sbuf_top)
        identf = P.sb("gidf", [128, 128], F32)
        P.op("dve", lambda e: e.tensor_copy(out=identf[:], in_=self.ident[:]), reads=[self.b_const], writes=[bc])
        onesf = P.sb("gonesf", [128, 1], F32)
        P.op("dve", lambda e: e.memset(onesf[:], 1.0), writes=[bc])
        wnat = P.sb("gwnat", [128, 8, 128], F32)
        P.dma("sp", lambda e: e.dma_start(out=wnat[:], in_=self.s_w_s.ap().rearrange("g t s -> t g s")), writes=[bc])
        for g in range(8):
            P.op("pool", lambda e: e.affine_select(out=wnat[:, g, :], in_=wnat[:, g, :], pattern=[[-1, 128]],
                                                    compare_op=ALU.is_ge, fill=self.zero_reg, base=0, channel_multiplier=1),
                 reads=[bc], writes=[bc])
        wTf = P.sb("gwTf", [128, 8, 128], F32)
        pS = P.ps("gpS", [128, 512], F32); b_pS = Buf("gpS")
        rowf = P.sb("growf", [1, 2, SGH], F32)
        rowb = P.sb("growb", [1, 2, SGH], BF16)
        wsf = P.sb("gwsf", [1, 8, 128], F32)
        bsf = P.sb("gbsf", [1, 2, 8, 128], F32)
        row2 = P.sb("grow2", [1, 3, 8, 128], BF16)
        for g in range(8):
            P.op("pe", lambda e: e.transpose(pS[:, 0:128], wnat[:, g, :], identf[:]), reads=[bc], writes=[b_pS])
            P.op("dve", lambda e: e.tensor_copy(out=wTf[:, g, :], in_=pS[:, 0:128]), reads=[b_pS], writes=[bc])
            P.op("act", lambda e: e.activation(out=wTb[:, g, :], in_=pS[:, 0:128], func=AF.Copy), reads=[b_pS], writes=[bc])
            P.op("pe", lambda e: e.matmul(pS[0:1, 128:256], lhsT=onesf[:], rhs=wTf[:, g, :], start=True, stop=True),
                 reads=[bc], writes=[b_pS])
            P.op("dve", lambda e: e.tensor_copy(out=wsf[0:1, g, :], in_=pS[0:1, 128:256]), reads=[b_pS], writes=[bc])
        P.op("dve", lambda e: e.tensor_copy(out=row2[:, 0, :, :], in_=wsf[:]), reads=[bc], writes=[bc])
        P.dma("sp", lambda e: e.dma_start(out=rowf[:, 0, :], in_=self.s_ln_b[0:1, :]), writes=[bc])
        P.op("dve", lambda e: e.tensor_copy(out=rowb[:, 0, :], in_=rowf[:, 0, :]), reads=[bc], writes=[bc])
        P.op("dve", lambda e: e.tensor_copy(out=rowf[:, 1, :], in_=rowb[:, 0, :]), reads=[bc], writes=[bc])
        P.op("dve", lambda e: e.tensor_tensor(out=rowb[:, 1, :], in0=rowf[:, 0, :], in1=rowf[:, 1, :], op=ALU.subtract),
             reads=[bc], writes=[bc])
        P.dma("sp", lambda e: e.dma_start(out=bsf[:, 0, :, :], in_=self.s_b_s.ap().rearrange("(o g) t -> o g t", o=1)), writes=[bc])
        P.op("dve", lambda e: e.tensor_copy(out=row2[:, 1, :, :], in_=bsf[:, 0, :, :]), reads=[bc], writes=[bc])
        P.op("dve", lambda e: e.tensor_copy(out=bsf[:, 1, :, :], in_=row2[:, 1, :, :]), reads=[bc], writes=[bc])
        P.op("dve", lambda e: e.tensor_tensor(out=row2[:, 2, :, :], in0=bsf[:, 0, :, :], in1=bsf[:, 1, :, :], op=ALU.subtract),
             reads=[bc], writes=[bc])
        P.op("dve", lambda e: e.memset(L4[0:4, :], 1.0), reads=[bc], writes=[bc])
        P.dma("sp", lambda e: e.dma_start(out=L4[0:1, :], in_=rowb[:, 0, :]), reads=[bc], writes=[bc])
        P.dma("sp", lambda e: e.dma_start(out=L4[1:2, :], in_=rowb[:, 1, :]), reads=[bc], writes=[bc])
        P.dma("sp", lambda e: e.dma_start(out=R4[0:1, :, :], in_=row2[:, 0, :, :]), reads=[bc], writes=[bc])
        P.dma("sp", lambda e: e.dma_start(out=R4[1:2, :, :], in_=row2[:, 0, :, :]), reads=[bc], writes=[bc])
        P.dma("sp", lambda e: e.dma_start(out=R4[2:3, :, :], in_=row2[:, 1, :, :]), reads=[bc], writes=[bc])
        P.dma("sp", lambda e: e.dma_start(out=R4[3:4, :, :], in_=row2[:, 2, :, :]), reads=[bc], writes=[bc])

        P.barrier()
        nc.sbuf_base, nc.sbuf_top = snap_sb
        L = self.ln_alloc(1, 0, True, share_pT=True, nbuf=2, nhb=4)
        hb_r = Ring(P, "ghb", 4, [128, D], BF16)
        hT, b_hT = P.sb("ghT", [128, 8, 512], BF16), Buf("ghT")
        uT, b_uT = P.sb("guT", [128, 24, 512], BF16), [Buf("uT%d" % i) for i in range(24)]
        wp_r = Ring(P, "gwp", 3, [128, 8, 512], BF16)
        pUV_r = Ring(P, "gpUV", 2, [128, 512], F32, psum=True)
        pR2 = P.ps("gpR2", [128, 512], F32)
        pR_slots = [(pS, b_pS), (pR2, Buf("gpR2"))]
        mix_r = Ring(P, "gmix", 1, [128, D], F32, psum=True)
        vgb = P.sb("gvgb", [128, 4, SGH], BF16); b_vgb = [Buf("vgb%d" % i) for i in range(4)]
        st = P.sb("gst", [128, 4, 6, 6], F32); b_st = [Buf("gst%d" % i) for i in range(4)]
        mv_r = Ring(P, "gmv", 4, [128, 8], F32)
        res_r = Ring(P, "gres", 2, [128, D], F32)

        pieces = []
        for sp in range(NSUP):
            for pv in range(6):
                pieces.append(SGH + pv * 512)
            for pu in range(6):
                pieces.append(pu * 512)
        wq = []
        nxt = [0]

        def prefetch(n):
            while nxt[0] < len(pieces) and len(wq) < n:
                c0 = pieces[nxt[0]]
                wp, b_wp = wp_r.next()
                P.dma("pool", lambda e: e.dma_start(out=wp[:], in_=self.s_w_in[:, c0:c0 + 512].rearrange("(c p) f -> p c f", p=128)),
                      writes=[b_wp])
                wq.append((wp, b_wp))
                nxt[0] += 1

        def take():
            prefetch(1)
            w = wq.pop(0)
            prefetch(2)
            return w

        hbq = []

        def load_hb(sp):
            for tl in range(4):
                ti = sp * 4 + tl
                hb, b_hb = hb_r.next()
                P.dma("pool", lambda e: e.dma_start(out=hb[:], in_=self.H2[ti * 128:(ti + 1) * 128, :]),
                      reads=[self.b_H["H2"]], writes=[b_hb])
                hbq.append((hb, b_hb))

        def stage_A(sp):
            for tl in range(4):
                hb, b_hb = hbq.pop(0)
                pT, b_pT = L.pT_r.next()
                for c in range(8):
                    P.op("pe", lambda e: e.transpose(pT[:, c, :], hb[:, c * 128:(c + 1) * 128], self.ident[:]),
                         reads=[b_hb, self.b_const], writes=[b_pT])
                P.op("dve", lambda e: e.tensor_copy(out=hT[:, :, tl * 128:(tl + 1) * 128], in_=pT[:]), reads=[b_pT], writes=[b_hT])
            for pv in range(6):
                wp, b_wp = take()
                for tl in range(4):
                    pV, b_pV = pUV_r.next()
                    for c in range(8):
                        P.op("pe", lambda e: e.matmul(pV[:], lhsT=hT[:, c, tl * 128:(tl + 1) * 128], rhs=wp[:, c, :],
                                                      start=(c == 0), stop=False), reads=[b_wp, b_hT], writes=[b_pV])
                    P.op("pe", lambda e: e.matmul(pV[:], lhsT=sel32[:], rhs=L4[:, pv * 512:(pv + 1) * 512],
                                                  start=False, stop=True), reads=[bc], writes=[b_pV])
                    P.op("act", lambda e: e.activation(out=vgb[:, tl, pv * 512:(pv + 1) * 512], in_=pV[:], func=AF.Gelu_apprx_tanh),
                         reads=[b_pV], writes=[b_vgb[tl]])
                    P.op("dve", lambda e: e.bn_stats(out=st[:, tl, pv, :], in_=vgb[:, tl, pv * 512:(pv + 1) * 512]),
                         reads=[b_vgb[tl]], writes=[b_st[tl]])
                    tick()
        def stage_A_tail(sp):
            for tl in range(4):
                mv, b_mv = mv_r.next()
                P.op("dve", lambda e: e.bn_aggr(out=mv[:, 0:2], in_=st[:, tl, :, :].rearrange("p a b -> p (a b)")),
                     reads=[b_st[tl]], writes=[b_mv])
                P.op("dve", lambda e: e.tensor_scalar(out=mv[:, 2:3], in0=mv[:, 1:2], scalar1=LN_EPS, scalar2=None, op0=ALU.add),
                     reads=[b_mv], writes=[b_mv])
                P.op("pool", lambda e: e.tensor_tensor(out=mv[:, 3:4], in0=mv[:, 2:3], in1=self.mhalf[:], op=ALU.pow),
                     reads=[b_mv, self.b_const], writes=[b_mv])
                P.op("dve", lambda e: e.tensor_scalar(out=vgb[:, tl, :], in0=vgb[:, tl, :], scalar1=mv[:, 0:1], scalar2=mv[:, 3:4],
                                                      op0=ALU.subtract, op1=ALU.mult), reads=[b_vgb[tl], b_mv], writes=[b_vgb[tl]])
                P.op("dve", lambda e: e.tensor_tensor(out=vgb[:, tl, :], in0=vgb[:, tl, :], in1=lgb[:], op=ALU.mult),
                     reads=[b_vgb[tl], bc], writes=[b_vgb[tl]])

        def stage_B(sp):
            for pu in range(6):
                wp, b_wp = take()
                for f4 in range(4):
                    fc = pu * 4 + f4
                    pU, b_pU = pUV_r.next()
                    for c in range(8):
                        P.op("pe", lambda e: e.matmul(pU[:], lhsT=wp[:, c, f4 * 128:(f4 + 1) * 128], rhs=hT[:, c, :],
                                                      start=(c == 0), stop=(c == 7)), reads=[b_wp, b_hT], writes=[b_pU])
                    P.op("act", lambda e: e.activation(out=uT[:, fc, :], in_=pU[:], func=AF.Gelu_apprx_tanh,
                                                       bias=bu[:, fc:fc + 1], scale=1.0), reads=[b_pU, bc], writes=[b_uT[fc]])
                if late_ln:
                    finish_ln(*late_ln.pop(0))
                elif tail_pending:
                    stage_A_tail(tail_pending.pop(0))

        def stage_C(sp):
            for fc in range(24):
                g = fc // 3
                pR, b_pR = pR_slots[fc % 2]
                P.op("pe", lambda e: e.matmul(pR[:], lhsT=L4[:, fc * 128:(fc + 1) * 128],
                                              rhs=R4[:, g, :].unsqueeze(1).to_broadcast([128, 4, 128]),
                                              start=True, stop=False), reads=[bc], writes=[b_pR])
                for tl in range(4):
                    P.op("pe", lambda e: e.matmul(pR[:, tl * 128:(tl + 1) * 128], lhsT=vgb[:, tl, fc * 128:(fc + 1) * 128], rhs=wTb[:, g, :],
                                                  start=False, stop=(tl == 3)), reads=[b_vgb[tl], bc], writes=[b_pR])
                P.op("dve", lambda e: e.tensor_tensor(out=uT[:, fc, :], in0=pR[:], in1=uT[:, fc, :], op=ALU.mult),
                     reads=[b_pR, b_uT[fc]], writes=[b_uT[fc]])
                tick()

        pend_route = []
        late_ln = []
        tail_pending = []

        def finish_ln(gen, ti):
            try:
                while True:
                    next(gen)
            except StopIteration as stop:
                hb, b_hb = stop.value
            pend_route.append((hb, b_hb, ti))

        def stage_Dproj(sp):
            prev_gen = None
            for tl in range(4):
                ti = sp * 4 + tl
                res, b_res = res_r.next()
                P.dma("sp", lambda e: e.dma_start(out=res[:], in_=self.H2[ti * 128:(ti + 1) * 128, :]),
                      reads=[self.b_H["H2"]], writes=[b_res])
                mix, b_mix = mix_r.next()
                for hf in range(2):
                    for fc in range(24):
                        P.op("pe", lambda e: e.matmul(mix[:, hf * 512:(hf + 1) * 512], lhsT=uT[:, fc, tl * 128:(tl + 1) * 128],
                                                      rhs=wo[:, fc, hf * 512:(hf + 1) * 512], start=(fc == 0), stop=(fc == 23)),
                             reads=[b_uT[fc], bc], writes=[b_mix])
                gen = self.ln_part_gen(L, mix[:], b_mix, res[:], b_res, self.H3, self.b_H["H3"], ti, gmul="dve")
                next(gen)
                if tl < 3 and prev_gen is not None:
                    finish_ln(*prev_gen)
                    prev_gen = None
                if prev_gen is not None:
                    late_ln.append(prev_gen)
                prev_gen = (gen, ti)
            late_ln.append(prev_gen)

        bg = []
        scat = []

        def tick():
            if not bg and pend_route:
                for _ in range(min(2, len(pend_route))):
                    hb, b_hb, ti = pend_route.pop(0)
                    bg.append(self.route_gen(L, hb, b_hb, ti, defer=scat))
            for g_ in list(bg):
                try:
                    next(g_)
                except StopIteration:
                    bg.remove(g_)

        def stage_Droute():
            while bg or pend_route:
                tick()
            while scat:
                scat.pop(0)()

        load_hb(0)
        prefetch(2)
        stage_A(0)
        stage_A_tail(0)
        stage_B(0)
        for sp in range(NSUP):
            if sp + 1 < NSUP:
                load_hb(sp + 1)
            stage_C(sp)
            if sp + 1 < NSUP:
                stage_A(sp + 1)
            stage_Droute()
            stage_Dproj(sp)
            if sp + 1 < NSUP:
                tail_pending.append(sp + 1)
                stage_B(sp + 1)
            while late_ln:
                finish_ln(*late_ln.pop(0))
            while tail_pending:
                stage_A_tail(tail_pending.pop(0))
        stage_Droute()

    def build(self, upto=99):
        self.setup()
        self.setup_route()
        if upto >= 1:
            self.phase(self._attn_layer)
        if upto >= 2:
            self.phase(self.moe, 0)
        if upto >= 3:
            self.phase(self.combine, 0, self.H1, self.b_H["H1"], self.H2, self.b_H["H2"])
        if upto >= 4:
            self.phase(self.gmlp)
        if upto >= 5:
            self.phase(self.moe, 1)
        if upto >= 6:
            self.phase(self.combine, 1, self.H3, self.b_H["H3"], self.out, self.b_H["out"])
        self.P.finish()
        return self.nc

    def _attn_layer(self):
        self.setup_attn()
        self.phase(self.proj_qkv)
        self.phase(self.attn)
        self.phase(self.post_attn)


def _t5_bucket_np(n):
    n = np.maximum(n, 0)
    nf = np.maximum(n, 1).astype(np.float32)
    large = 16 + (np.log(nf / np.float32(16)) / np.float32(math.log(128 / 16)) * np.float32(16)).astype(np.int32)
    large = np.minimum(large, 31)
    return np.where(n < 16, n, large)


def prep_shared(inp):
    f = lambda a: np.ascontiguousarray(np.asarray(a, dtype=np.float32))
    rel = f(inp["rel_bias"])
    k = np.arange(128)[:, None]
    q = np.arange(128)[None, :]
    bt = np.stack([rel[_t5_bucket_np(q - k)], rel[_t5_bucket_np(128 + q - k)]], 0)
    bt = np.ascontiguousarray(np.transpose(bt, (0, 3, 1, 2)))
    sh = {
        "bt": bt, "cfar": f(rel[31:32, :]),
        "attn_w_in": f(inp["attn_w_in"][0]), "attn_w_out": f(inp["attn_w_out"][0]),
        "attn_lambda": f(np.asarray(inp["attn_lambda"][0]).reshape(1, 256)), "attn_subln_g": f(inp["attn_subln_g"]),
        "sg_w_in": f(inp["sg_w_in"][0]), "sg_b_in": f(inp["sg_b_in"]), "sg_ln_g": f(inp["sg_ln_g"]), "sg_ln_b": f(inp["sg_ln_b"]),
        "sg_w_s": f(inp["sg_w_s"][0]), "sg_b_s": f(inp["sg_b_s"][0]), "sg_w_out": f(inp["sg_w_out"][0]),
        "moe_wr": f(np.concatenate([np.asarray(inp["moe_w_group"]), np.asarray(inp["moe_w_router"])], axis=-1)),
        "moe_br": f(np.concatenate([np.asarray(inp["moe_b_group"]), np.asarray(inp["moe_b_router"])], axis=-1)),
        "moe_w_gate": f(inp["moe_w_gate"]), "moe_w_up": f(inp["moe_w_up"]), "moe_w_down": f(inp["moe_w_down"]),
        "ln_g": f(inp["ln_g"]), "ln_b": f(inp["ln_b"]),
    }
    return sh


_CACHE = {}


def kernel(**inputs):
    sh = prep_shared(inputs)
    x = np.ascontiguousarray(np.asarray(inputs["x"], dtype=np.float32))
    B, S, _ = x.shape
    nseq = B // N_CORES
    if "nc" not in _CACHE:
        k = K(nseq=nseq, S=S, C=384)
        _CACHE["nc"] = k.build()
    nc = _CACHE["nc"]
    in_maps = []
    for c in range(N_CORES):
        m = dict(sh)
        m["x"] = x[c * nseq:(c + 1) * nseq].reshape(nseq * S, D)
        in_maps.append(m)
    res = run_bass_kernel_spmd(nc, in_maps, core_ids=list(range(N_CORES)))
    out = np.concatenate([np.asarray(r["out"]).reshape(nseq, S, D) for r in res.results], axis=0)
    return out.astype(np.float32, copy=False)
```

```python
import math
import numpy as np
import concourse.bass as bass
import concourse.mybir as mybir
from concourse.bass_utils import run_bass_kernel_spmd
from concourse.alu_op_type import AluOpType as ALU

AF = mybir.ActivationFunctionType
F32, BF16, I32, U32 = mybir.dt.float32, mybir.dt.bfloat16, mybir.dt.int32, mybir.dt.uint32

D = 1024
NH = 8
NE = 64
HID = 512
SGH = 3072
LN_EPS = 1e-5
DEPTH = 2
DN_ALPHA = (2 * DEPTH) ** 0.25
N_CORES = 8


class Buf:
    __slots__ = ("name", "w", "r")

    def __init__(self, name=""):
        self.name = name
        self.w = None
        self.r = {}


class Prog:
    NDMA = {"sp": 24, "act": 4, "pool": 24}

    def __init__(self):
        nc = self.nc = bass.Bass("TRN2", target_bir_lowering=False)
        self.engs = {"pe": nc.tensor, "act": nc.scalar, "dve": nc.vector,
                     "pool": nc.gpsimd, "sp": nc.sync}
        self.sems = {}
        self.count = {}
        for e in self.engs:
            self.sems[e] = nc.alloc_semaphore(name="c_" + e)
            self.count[e] = 0
        self.dma_pool = {}
        self.dma_rr = {}
        self.dma_uses = {}
        for q, n in self.NDMA.items():
            keys = []
            for i in range(n):
                k = "d_%s_%d" % (q, i)
                self.sems[k] = nc.alloc_semaphore(name=k)
                self.dma_uses[k] = 0
                keys.append(k)
            self.dma_pool[q] = keys
            self.dma_rr[q] = 0
        self.known = {e: {} for e in self.engs}
        self.n_inst = 0
        self._names = 0

    def sb(self, name, shape, dt):
        self._names += 1
        return self.nc.alloc_sbuf_tensor("%s_%d" % (name, self._names), list(shape), dt)

    def ps(self, name, shape, dt=F32):
        self._names += 1
        return self.nc.alloc_psum_tensor("%s_%d" % (name, self._names), list(shape), dt)

    def _wait(self, eng, key, val):
        if val <= 0:
            return
        kn = self.known[eng]
        if kn.get(key, 0) >= val:
            return
        self.engs[eng].wait_ge(self.sems[key], val)
        kn[key] = val

    def _deps(self, eng, reads, writes):
        deps = {}

        def add(tok, raw):
            if tok is None:
                return
            k, v = tok
            if k == eng and (not raw or eng == "pe"):
                return
            if deps.get(k, 0) < v:
                deps[k] = v
        for b in reads:
            add(b.w, True)
        for b in writes:
            add(b.w, False)
            for k, v in b.r.items():
                add((k, v), False)
        return deps

    def _mark(self, tok, reads, writes):
        k, v = tok
        for b in reads:
            if b.r.get(k, 0) < v:
                b.r[k] = v
        for b in writes:
            b.w = tok
            b.r = {}

    def op(self, eng, fn, reads=(), writes=()):
        for k, v in self._deps(eng, reads, writes).items():
            self._wait(eng, k, v)
        ins = fn(self.engs[eng])
        self.count[eng] += 1
        ins.then_inc(self.sems[eng], 1)
        tok = (eng, self.count[eng])
        self._mark(tok, reads, writes)
        self.n_inst += 1
        return tok

    def dma(self, q, fn, reads=(), writes=()):
        for k, v in self._deps(q, reads, writes).items():
            self._wait(q, k, v)
        pool = self.dma_pool[q]
        key = pool[self.dma_rr[q] % len(pool)]
        self.dma_rr[q] += 1
        prior = self.dma_uses[key]
        self._wait(q, key, 16 * prior)
        ins = fn(self.engs[q])
        ins.then_inc(self.sems[key], 16)
        self.dma_uses[key] = prior + 1
        tok = (key, 16 * (prior + 1))
        self._mark(tok, reads, writes)
        self.n_inst += 1
        return tok

    def barrier(self):
        for e in self.engs:
            for e2 in self.engs:
                if e2 != e:
                    self._wait(e, e2, self.count[e2])
            for k, n in self.dma_uses.items():
                self._wait(e, k, 16 * n)

    def finish(self):
        for e in self.engs:
            if e != "sp":
                self._wait("sp", e, self.count[e])
        for k, n in self.dma_uses.items():
            self._wait("sp", k, 16 * n)


class Ring:
    def __init__(self, P, name, n, shape, dt, psum=False):
        self.slots = []
        for i in range(n):
            t = P.ps(name, shape, dt) if psum else P.sb(name, shape, dt)
            self.slots.append((t, Buf(name)))
        self.i = 0

    def next(self):
        s = self.slots[self.i % len(self.slots)]
        self.i += 1
        return s


class K:
    def __init__(self, nseq=2, S=4096, C=384, test_outputs=(), lite=False):
        self.nseq, self.S, self.C = nseq, S, C
        self.lite = lite
        self.NT = nseq * S
        self.NTL = self.NT // 128
        self.P = Prog()
        self.nc = self.P.nc
        self.test_outputs = set(test_outputs)
        self._declare_dram()

    def _dt(self, name, shape, dt, kind=None):
        if kind is None:
            kind = "ExternalOutput" if name in self.test_outputs else "Internal"
        return self.nc.dram_tensor(name, list(shape), dt, kind=kind)

    def _declare_dram(self):
        NT, S, nseq, C = self.NT, self.S, self.nseq, self.C
        i = lambda n, s: self._dt(n, s, F32, kind="ExternalInput")
        self.x = i("x", [NT, D])
        self.bt = i("bt", [2, NH, 128, 128])
        self.cfar = i("cfar", [1, NH])
        self.a_w_in = i("attn_w_in", [D, 3 * D])
        self.a_w_out = i("attn_w_out", [D, D])
        self.a_lam = i("attn_lambda", [1, 256])
        self.a_g = i("attn_subln_g", [1, 128])
        self.s_w_in = i("sg_w_in", [D, 2 * SGH])
        self.s_b_in = i("sg_b_in", [1, 2 * SGH])
        self.s_ln_g = i("sg_ln_g", [1, SGH])
        self.s_ln_b = i("sg_ln_b", [1, SGH])
        self.s_w_s = i("sg_w_s", [8, 128, 128])
        self.s_b_s = i("sg_b_s", [8, 128])
        self.s_w_out = i("sg_w_out", [SGH, D])
        self.m_wr = i("moe_wr", [2, D, 72])
        self.m_br = i("moe_br", [2, 72])
        ne = 1 if self.lite else NE
        self.m_wg = i("moe_w_gate", [2, ne, D, HID])
        self.m_wu = i("moe_w_up", [2, ne, D, HID])
        self.m_wd = i("moe_w_down", [2, ne, HID, D])
        self.ln_g = i("ln_g", [2, 2, D])
        self.ln_b = i("ln_b", [2, 2, D])
        self.out = self._dt("out", [NT, D], F32, kind="ExternalOutput")
        self.QKT = self._dt("QKT", [nseq, 16, 128, S], BF16)
        self.V = self._dt("V", [nseq, S, D], BF16)
        self.OT = self._dt("OT", [nseq, NH, 128, S], BF16)
        self.H1 = self._dt("H1", [NT, D], F32)
        self.H2 = self._dt("H2", [NT, D], F32)
        self.H3 = self._dt("H3", [NT, D], F32)
        self.XS = self._dt("XS", [NE * C, D], BF16)
        self.YS = self._dt("YS", [NE * C, D], BF16)
        self.b_QKT = [Buf() for _ in range(nseq)]
        self.b_V = [Buf() for _ in range(nseq)]
        self.b_OT = [Buf() for _ in range(nseq)]
        self.b_H = {n: Buf(n) for n in ("H1", "H2", "H3", "out")}
        self.b_XS = Buf("XS")
        self.b_YS = Buf("YS")

    def setup(self):
        P = self.P
        self.zero_reg = self.nc.gpsimd.to_reg(0.0)
        self.ident = P.sb("ident", [128, 128], BF16); self.b_const = Buf("const")
        bc = self.b_const
        P.op("pool", lambda e: e.memset(self.ident[:], 1.0), writes=[bc])
        P.op("pool", lambda e: e.affine_select(out=self.ident[:], in_=self.ident[:], pattern=[[-1, 128]],
                                                compare_op=ALU.is_equal, fill=self.zero_reg, base=0, channel_multiplier=1),
             reads=[bc], writes=[bc])
        self.ones = P.sb("ones", [128, 128], BF16)
        P.op("pool", lambda e: e.memset(self.ones[:], 1.0), writes=[bc])
        self.ustrict = P.sb("ustrict", [128, 128], BF16)
        P.op("pool", lambda e: e.memset(self.ustrict[:], 1.0), writes=[bc])
        P.op("pool", lambda e: e.affine_select(out=self.ustrict[:], in_=self.ustrict[:], pattern=[[1, 128]],
                                                compare_op=ALU.is_gt, fill=self.zero_reg, base=0, channel_multiplier=-1),
             reads=[bc], writes=[bc])
        self.iota_i = P.sb("iota_i", [128, NE], I32)
        self.iota_e = P.sb("iota_e", [128, NE], F32)
        P.op("pool", lambda e: e.iota(self.iota_i[:], pattern=[[1, NE]], base=0, channel_multiplier=0), writes=[bc])
        P.op("dve", lambda e: e.tensor_copy(out=self.iota_e[:], in_=self.iota_i[:]), reads=[bc], writes=[bc])
        self.mhalf = P.sb("mhalf", [128, 1], F32)
        P.op("dve", lambda e: e.memset(self.mhalf[:], -0.5), writes=[bc])

    def setup_attn(self):
        P = self.P
        bc = self.b_attn_c = Buf("attn_c")
        lam_init = 0.8 - 0.6 * math.exp(-0.3 * 0)
        self.cb = P.sb("cb", [128, NH], F32)
        self.ncb = P.sb("ncb", [128, NH], F32)
        P.dma("sp", lambda e: e.dma_start(out=self.cb[:], in_=self.cfar[0].partition_broadcast(128)), writes=[bc])
        P.op("dve", lambda e: e.tensor_scalar(out=self.ncb[:], in0=self.cb[:], scalar1=-1.0, scalar2=None, op0=ALU.mult),
             reads=[bc], writes=[bc])
        btf = P.sb("btf", [128, 2, NH, 128], F32)
        self.Eb = P.sb("Eb", [128, 2, NH, 128], BF16)
        P.dma("sp", lambda e: e.dma_start(out=btf[:], in_=self.bt.ap().rearrange("d h k q -> k d h q")), writes=[bc])
        for d in range(2):
            for h in range(NH):
                P.op("act", lambda e: e.activation(out=btf[:, d, h, :], in_=btf[:, d, h, :], func=AF.Exp,
                                                   bias=self.ncb[:, h:h + 1], scale=1.0), reads=[bc], writes=[bc])
        for h in range(NH):
            P.op("pool", lambda e: e.affine_select(out=btf[:, 0, h, :], in_=btf[:, 0, h, :], pattern=[[1, 128]],
                                                    compare_op=ALU.is_ge, fill=self.zero_reg, base=0, channel_multiplier=-1),
                 reads=[bc], writes=[bc])
        P.op("dve", lambda e: e.tensor_copy(out=self.Eb[:], in_=btf[:]), reads=[bc], writes=[bc])
        lv = P.sb("lv", [128, 256], F32)
        P.dma("sp", lambda e: e.dma_start(out=lv[:], in_=self.a_lam[0].partition_broadcast(128)), writes=[bc])
        junk = P.sb("junk", [128, 64], F32)
        s12 = P.sb("s12", [128, 2], F32)
        for i in range(2):
            P.op("dve", lambda e: e.scalar_tensor_tensor(out=junk[:], in0=lv[:, 128 * i:128 * i + 64], scalar=1.0, in1=lv[:, 128 * i + 64:128 * i + 128], op0=ALU.mult, op1=ALU.mult, accum_out=s12[:, i:i + 1]),
                 reads=[bc], writes=[bc])
        P.op("act", lambda e: e.activation(out=s12[:], in_=s12[:], func=AF.Exp), reads=[bc], writes=[bc])
        self.neg_lam = P.sb("neg_lam", [128, 1], F32)
        P.op("dve", lambda e: e.scalar_tensor_tensor(out=self.neg_lam[:], in0=s12[:, 1:2], scalar=-lam_init,
                                                     in1=s12[:, 0:1], op0=ALU.add, op1=ALU.subtract),
             reads=[bc], writes=[bc])
        self.gsub = P.sb("gsub", [128, 128], F32)
        P.dma("sp", lambda e: e.dma_start(out=self.gsub[:], in_=self.a_g[0].partition_broadcast(128)), writes=[bc])
        P.op("dve", lambda e: e.tensor_scalar(out=self.gsub[:], in0=self.gsub[:], scalar1=1.0 - lam_init, scalar2=None,
                                              op0=ALU.mult), reads=[bc], writes=[bc])

    def proj_qkv(self):
        P, S = self.P, self.S
        wi = P.sb("wi", [128, 8, 3 * D], BF16); b_wis = []
        for hf in range(2):
            for c in range(8):
                b_one = Buf("wi")
                b_wis.append(b_one)
                P.dma("pool", lambda e: e.dma_start(out=wi[:, c, hf * 1536:(hf + 1) * 1536],
                                                    in_=self.a_w_in[c * 128:(c + 1) * 128, hf * 1536:(hf + 1) * 1536]),
                      writes=[b_one])
        xb_r = Ring(P, "xb", 3, [128, D], BF16)
        pT_r = Ring(P, "pT", 2, [128, 8, 128], BF16, psum=True)
        xT_r = Ring(P, "xT", 2, [128, 8, 512], BF16)
        pq_r = Ring(P, "pq", 4, [128, 512], F32, psum=True)
        qs_r = Ring(P, "qs", 2, [128, 16, 512], BF16)
        pv_r = Ring(P, "pv", 2, [128, 512], F32, psum=True)
        vs_r = Ring(P, "vs", 2, [128, D], BF16)
        ev = 0
        for s in range(self.nseq):
            for g in range(S // 512):
                xT, b_xT = xT_r.next()
                for tl in range(4):
                    r0 = s * S + g * 512 + tl * 128
                    xb, b_xb = xb_r.next()
                    P.dma("pool", lambda e: e.dma_start(out=xb[:], in_=self.x[r0:r0 + 128, :]), writes=[b_xb])
                    pT, b_pT = pT_r.next()
                    for c in range(8):
                        P.op("pe", lambda e: e.transpose(pT[:, c, :], xb[:, c * 128:(c + 1) * 128], self.ident[:]),
                             reads=[b_xb, self.b_const], writes=[b_pT])
                    P.op("dve", lambda e: e.tensor_copy(out=xT[:, :, tl * 128:(tl + 1) * 128], in_=pT[:]),
                         reads=[b_pT], writes=[b_xT])
                qs, b_qs = qs_r.next()
                for cg in range(16):
                    pq, b_pq = pq_r.next()
                    for c in range(8):
                        P.op("pe", lambda e: e.matmul(pq[:], lhsT=wi[:, c, cg * 128:(cg + 1) * 128], rhs=xT[:, c, :],
                                                      start=(c == 0), stop=(c == 7)), reads=b_wis + [b_xT], writes=[b_pq])
                    sc = 0.125 if cg < 8 else 1.0
                    if ev % 2 == 0:
                        P.op("act", lambda e: e.activation(out=qs[:, cg, :], in_=pq[:], func=AF.Copy, scale=sc),
                             reads=[b_pq], writes=[b_qs])
                    else:
                        P.op("dve", lambda e: e.tensor_scalar(out=qs[:, cg, :], in0=pq[:], scalar1=sc, scalar2=None,
                                                              op0=ALU.mult), reads=[b_pq], writes=[b_qs])
                    ev += 1
                for q8 in range(2):
                    P.dma("sp", lambda e: e.dma_start(out=self.QKT[s, q8 * 8:(q8 + 1) * 8, :, g * 512:(g + 1) * 512].rearrange("g p t -> p g t"),
                                                      in_=qs[:, q8 * 8:(q8 + 1) * 8, :]), reads=[b_qs])
                for tl in range(4):
                    vs, b_vs = vs_r.next()
                    for hf in range(2):
                        pv, b_pv = pv_r.next()
                        for c in range(8):
                            P.op("pe", lambda e: e.matmul(pv[:], lhsT=xT[:, c, tl * 128:(tl + 1) * 128],
                                                          rhs=wi[:, c, 2048 + hf * 512:2048 + (hf + 1) * 512],
                                                          start=(c == 0), stop=(c == 7)), reads=b_wis + [b_xT], writes=[b_pv])
                        if ev % 2 == 0:
                            P.op("act", lambda e: e.activation(out=vs[:, hf * 512:(hf + 1) * 512], in_=pv[:], func=AF.Copy),
                                 reads=[b_pv], writes=[b_vs])
                        else:
                            P.op("dve", lambda e: e.tensor_copy(out=vs[:, hf * 512:(hf + 1) * 512], in_=pv[:]),
                                 reads=[b_pv], writes=[b_vs])
                        ev += 1
                    t0 = g * 512 + tl * 128
                    P.dma("sp", lambda e: e.dma_start(out=self.V[s, t0:t0 + 128, :], in_=vs[:]),
                          reads=[b_vs])

    def attn(self):
        P, S = self.P, self.S
        nqb = S // 128
        nsb = nqb // 4
        QT_r = Ring(P, "QT", 2, [128, S], BF16)
        KT_r = Ring(P, "KT", 2, [128, S], BF16)
        Va_r = Ring(P, "Va", 2, [128, nqb, 132], BF16)
        for va, b in Va_r.slots:
            P.op("pool", lambda e: e.memset(va[:, :, 128:129], 1.0), writes=[b])
        S_r = [Ring(P, "S%d" % m, 2, [128, 512], F32, psum=True) for m in range(2)]
        PT_r = Ring(P, "PT", 6, [128, 512], BF16)
        Ob = [P.ps("Ob", [128, 512], F32) for _ in range(3)]
        b_Ob = [Buf("Ob%d" % i) for i in range(3)]
        acc = {}
        order = [(0, 0), (0, 1), (0, 2), (0, 3), (1, 0), (1, 1), (1, 2), (1, 3)]
        for n, key in enumerate(order):
            acc[key] = (n // 3, (n % 3) * 130)
        Tb, b_Tb = P.ps("Tb", [128, 4, 128], BF16), Buf("Tb")
        os_r = Ring(P, "os", 2, [128, 512], BF16)

        steps = []
        for s in range(self.nseq):
            for h in range(NH):
                for sbi in range(nsb):
                    i0 = 4 * sbi
                    for j in range(i0 + 4):
                        steps.append((s, h, sbi, j))
        cur = {}

        def load_head(s, h):
            QT, b_QT = QT_r.next(); KT, b_KT = KT_r.next(); Va, b_Va = Va_r.next()
            P.dma("sp", lambda e: e.dma_start(out=QT[:], in_=self.QKT[s, h]), reads=[self.b_QKT[s]], writes=[b_QT])
            P.dma("sp", lambda e: e.dma_start(out=KT[:], in_=self.QKT[s, 8 + h]), reads=[self.b_QKT[s]], writes=[b_KT])
            JB = 8
            for j0 in range(0, nqb, JB):
                j1 = min(nqb, j0 + JB)
                P.dma("sp", lambda e: e.dma_start(out=Va[:, j0:j1, 0:128],
                                                  in_=self.V[s, j0 * 128:j1 * 128, h * 128:(h + 1) * 128].rearrange("(j p) e -> p j e", p=128)),
                      reads=[self.b_V[s]], writes=[b_Va])
            return (QT, b_QT, KT, b_KT, Va, b_Va)

        heads = {}
        hl = [(s, h) for s in range(self.nseq) for h in range(NH)]
        heads[hl[0]] = load_head(*hl[0])

        def emit_S(step):
            s, h, sbi, j = step
            if (s, h) not in heads:
                heads[(s, h)] = load_head(s, h)
            QT, b_QT, KT, b_KT, Va, b_Va = heads[(s, h)]
            i0 = 4 * sbi
            qlo = max(j, i0)
            N = (i0 + 4 - qlo) * 128
            res = []
            for m in range(2):
                St, b_S = S_r[m].next()
                P.op("pe", lambda e: e.matmul(St[:, 0:N], lhsT=KT[m * 64:(m + 1) * 64, j * 128:(j + 1) * 128],
                                              rhs=QT[m * 64:(m + 1) * 64, qlo * 128:(i0 + 4) * 128],
                                              start=True, stop=True), reads=[b_KT, b_QT], writes=[b_S])
                res.append((St, b_S))
            return res

        def emit_rest(step, Sres, touched):
            s, h, sbi, j = step
            QT, b_QT, KT, b_KT, Va, b_Va = heads[(s, h)]
            i0 = 4 * sbi
            qlo = max(j, i0)
            N = (i0 + 4 - qlo) * 128
            for m in range(2):
                St, b_S = Sres[m]
                PT, b_PT = PT_r.next()
                P.op("act", lambda e: e.activation(out=PT[:, 0:N], in_=St[:, 0:N], func=AF.Exp),
                     reads=[b_S], writes=[b_PT])
                for ii in range(qlo - i0, 4):
                    dd = (i0 + ii) - j
                    if dd in (0, 1):
                        c0 = (ii - (qlo - i0)) * 128
                        P.op("dve", lambda e: e.tensor_tensor(out=PT[:, c0:c0 + 128], in0=PT[:, c0:c0 + 128],
                                                              in1=self.Eb[:, dd, h, :], op=ALU.mult),
                             reads=[b_PT, self.b_attn_c], writes=[b_PT])
                for ii in range(qlo - i0, 4):
                    bank, off = acc[(m, ii)]
                    c0 = (ii - (qlo - i0)) * 128
                    first = bank not in touched
                    touched.add(bank)
                    P.op("pe", lambda e: e.matmul(Ob[bank][:, off:off + 129], lhsT=PT[:, c0:c0 + 128],
                                                  rhs=Va[:, j, 0:129], start=first, stop=(j == i0 + ii),
                                                  skip_group_check=True),
                         reads=[b_PT, b_Va], writes=[b_Ob[bank]])

        Osb_r = Ring(P, "Osb", 2, [128, 8, 130], F32)
        cm_r = Ring(P, "cm", 2, [128, 32], F32)
        t4_r = Ring(P, "t4", 2, [128, 4, 128], F32)
        o4_r = Ring(P, "o4", 2, [128, 4, 128], F32)
        q4_r = Ring(P, "q4", 2, [128, 4, 128], F32)
        ob3_r = Ring(P, "ob3", 3, [128, 4, 128], BF16)

        def combine_gen(s, h, sbi):
            i0 = 4 * sbi
            Osb, b_Osb = Osb_r.next()
            for bank in range(3):
                n0 = bank * 3
                n1 = min(8, n0 + 3)
                P.op("dve", lambda e: e.tensor_copy(out=Osb[:, n0:n1, :].rearrange("p a b -> p (a b)"),
                                                    in_=Ob[bank][:, 0:(n1 - n0) * 130]),
                     reads=[b_Ob[bank]], writes=[b_Osb])
            yield
            cm, b_cm = cm_r.next()
            R = [b_cm]
            bc3 = lambda ap: ap.unsqueeze(2).to_broadcast([128, 4, 128])
            P.op("dve", lambda e: e.reciprocal(out=cm[:, 0:8].unsqueeze(2), in_=Osb[:, :, 128:129]), reads=[b_Osb], writes=R)
            yield
            P.op("dve", lambda e: e.tensor_scalar(out=cm[:, 8:12], in0=cm[:, 4:8], scalar1=self.neg_lam[:, 0:1], scalar2=None,
                                                  op0=ALU.mult), reads=R + [self.b_attn_c], writes=R)
            t4, b_t4 = t4_r.next()
            P.op("dve", lambda e: e.tensor_tensor(out=t4[:], in0=Osb[:, 0:4, 0:128], in1=bc3(cm[:, 0:4]), op=ALU.mult),
                 reads=[b_Osb] + R, writes=[b_t4])
            yield
            o4, b_o4 = o4_r.next()
            P.op("dve", lambda e: e.tensor_tensor(out=o4[:], in0=Osb[:, 4:8, 0:128], in1=bc3(cm[:, 8:12]), op=ALU.mult),
                 reads=[b_Osb] + R, writes=[b_o4])
            yield
            P.op("dve", lambda e: e.tensor_tensor(out=o4[:], in0=o4[:], in1=t4[:], op=ALU.add), reads=[b_o4, b_t4], writes=[b_o4])
            yield
            q4, b_q4 = q4_r.next()
            P.op("dve", lambda e: e.tensor_tensor(out=q4[:], in0=o4[:], in1=o4[:], op=ALU.mult), reads=[b_o4], writes=[b_q4])
            yield
            P.op("dve", lambda e: e.tensor_reduce(out=cm[:, 12:16], in_=q4[:], axis=mybir.AxisListType.X, op=ALU.add),
                 reads=[b_q4], writes=R)
            yield
            P.op("dve", lambda e: e.tensor_scalar(out=cm[:, 16:20], in0=cm[:, 12:16], scalar1=1.0 / 128, scalar2=LN_EPS,
                                                  op0=ALU.mult, op1=ALU.add), reads=R, writes=R)
            P.op("pool", lambda e: e.tensor_tensor(out=cm[:, 20:24], in0=cm[:, 16:20], in1=self.mhalf[:, 0:1].to_broadcast([128, 4]),
                                                   op=ALU.pow), reads=R + [self.b_const], writes=R)
            yield
            yield
            P.op("dve", lambda e: e.tensor_tensor(out=o4[:], in0=o4[:], in1=bc3(cm[:, 20:24]), op=ALU.mult),
                 reads=[b_o4] + R, writes=[b_o4])
            yield
            ob, b_ob = ob3_r.next()
            P.op("dve", lambda e: e.tensor_tensor(out=ob[:], in0=o4[:], in1=self.gsub[:].unsqueeze(1).to_broadcast([128, 4, 128]),
                                                  op=ALU.mult), reads=[b_o4, self.b_attn_c], writes=[b_ob])
            yield
            yield
            for ii in range(4):
                P.op("pe", lambda e: e.transpose(Tb[:, ii, :], ob[:, ii, :], self.ident[:]),
                     reads=[b_ob, self.b_const], writes=[b_Tb])
            os_, b_os = os_r.next()
            P.op("dve", lambda e: e.tensor_copy(out=os_[:], in_=Tb[:].rearrange("p a b -> p (a b)")),
                 reads=[b_Tb], writes=[b_os])
            P.dma("sp", lambda e: e.dma_start(out=self.OT[s, h, :, i0 * 128:(i0 + 4) * 128], in_=os_[:]),
                  reads=[b_os])

        def finish_gen(g):
            for _ in g:
                pass

        Sres_next = emit_S(steps[0])
        touched = set()
        active = []
        for t, step in enumerate(steps):
            Sres = Sres_next
            s, h, sbi, j = step
            if j == 0 and sbi == 0:
                idx = hl.index((s, h))
                if idx + 1 < len(hl) and hl[idx + 1] not in heads:
                    heads[hl[idx + 1]] = load_head(*hl[idx + 1])
            if t + 1 < len(steps):
                Sres_next = emit_S(steps[t + 1])
            if j == 0:
                touched = set()
            emit_rest(step, Sres, touched)
            for g in list(active):
                try:
                    next(g)
                except StopIteration:
                    active.remove(g)
            if j == 4 * sbi + 3:
                while len(active) >= 2:
                    finish_gen(active.pop(0))
                g = combine_gen(s, h, sbi)
                next(g)
                active.append(g)
        for g in active:
            finish_gen(g)

    def phase(self, fn, *a, **k):
        nc = self.nc
        snap = (nc.psum_base, nc.psum_top, nc.sbuf_base, nc.sbuf_top)
        fn(*a, **k)
        self.P.barrier()
        nc.psum_base, nc.psum_top, nc.sbuf_base, nc.sbuf_top = snap

    def setup_route(self):
        P = self.P
        self.SL = P.sb("SL", [128, self.NTL, 2], I32)
        self.GT = P.sb("GT", [128, self.NTL, 2], F32)
        self.cnt = P.sb("cnt", [128, NE], F32)
        self.b_SL = [Buf("SL%d" % i) for i in range(self.NTL)]
        self.b_cnt = Buf("cnt")
        self.bound_reg = self.nc.gpsimd.to_reg(NE * self.C - 1)

    def ln_alloc(self, layer, j, route, share_pT=False, nbuf=2, nhb=None, xn_psum=0):
        P = self.P

        class L:
            pass
        L.layer, L.j, L.route = layer, j, route
        L.g = P.sb("lng", [128, D], F32); L.b = P.sb("lnb", [128, D], F32); L.b_gb = Buf("lngb")
        P.dma("sp", lambda e: e.dma_start(out=L.g[:], in_=self.ln_g[layer, j].partition_broadcast(128)), writes=[L.b_gb])
        P.dma("sp", lambda e: e.dma_start(out=L.b[:], in_=self.ln_b[layer, j].partition_broadcast(128)), writes=[L.b_gb])
        L.tt_r = Ring(P, "ltt", nbuf, [128, D], F32)
        L.st_r = Ring(P, "lst", nbuf, [128, 2, 6], F32)
        L.mv_r = Ring(P, "lmv", nbuf, [128, 8], F32)
        L.xn_psum = xn_psum
        if xn_psum:
            L.xn_r = Ring(P, "lxnp", xn_psum, [128, D], F32, psum=True)
        else:
            L.xn_r = Ring(P, "lxn", max(1, nbuf - 1), [128, D], F32)
        L.h_r = Ring(P, "lh", nbuf, [128, D], F32)
        if route:
            L.hb_r = Ring(P, "lhb", nhb or nbuf, [128, D], BF16)
            L.pT_r = Ring(P, "lpT", 1, [128, 8, 128], BF16, psum=True)
            L.hT_r = Ring(P, "lhT", max(1, nbuf - 1), [128, 8, 128], BF16)
            L.wr = P.sb("wr", [128, 8, 72], BF16); L.br = P.sb("br", [128, 72], F32); L.b_wr = Buf("wr")
            P.dma("pool", lambda e: e.dma_start(out=L.wr[:], in_=self.m_wr[layer].rearrange("(c p) f -> p c f", p=128)),
                  writes=[L.b_wr])
            P.dma("sp", lambda e: e.dma_start(out=L.br[:], in_=self.m_br[layer].partition_broadcast(128)), writes=[L.b_wr])
            L.pLC = P.ps("pLC", [128, 512], F32); L.b_pLC = Buf("pLC")
            L.rt_r = Ring(P, "rt", nbuf, [128, 200], F32)
            L.i8_r = Ring(P, "i8", nbuf, [128, 8], U32)
            L.oh_r = Ring(P, "oh", nbuf, [128, 2, NE], F32)
            L.M_r = Ring(P, "Mb", nbuf, [128, NE], BF16)
            L.pf_r = Ring(P, "pf", nbuf, [128, NE], F32)
            L.jk_r = Ring(P, "rjk", nbuf, [128, NE], F32)
            L.ss_r = Ring(P, "ss", nbuf + 3, [128, 2], I32)
            P.op("dve", lambda e: e.memset(self.cnt[:], 0.0), writes=[self.b_cnt])
        return L

    @staticmethod
    def run_gens(gens):
        gens = list(gens)
        while gens:
            for g in list(gens):
                try:
                    next(g)
                except StopIteration:
                    gens.remove(g)

    @staticmethod
    def run_fg_bg(fg, bg):
        fg = list(fg)
        while fg:
            for g in list(fg):
                try:
                    next(g)
                except StopIteration:
                    fg.remove(g)
            for g in list(bg):
                try:
                    next(g)
                except StopIteration:
                    bg.remove(g)

    def ln_part_gen(self, L, mix, b_mix, resid, b_resid, Hd, b_Hd, ti, gmul="pool"):
        P = self.P
        tt, b_tt = L.tt_r.next()
        P.op("dve", lambda e: e.scalar_tensor_tensor(out=tt[:], in0=resid, scalar=DN_ALPHA, in1=mix,
                                                     op0=ALU.mult, op1=ALU.add), reads=[b_resid, b_mix], writes=[b_tt])
        yield
        h, b_h = yield from self.ln_core_gen(L, tt, b_tt, gmul)
        P.dma("sp", lambda e: e.dma_start(out=Hd[ti * 128:(ti + 1) * 128, :], in_=h[:]), reads=[b_h])
        if not L.route:
            return None
        hb, b_hb = L.hb_r.next()
        P.op("act", lambda e: e.activation(out=hb[:], in_=h[:], func=AF.Copy), reads=[b_h], writes=[b_hb])
        yield
        return hb, b_hb

    def ln_route_gen(self, L, mix, b_mix, resid, b_resid, Hd, b_Hd, ti, gmul="pool"):
        r = yield from self.ln_part_gen(L, mix, b_mix, resid, b_resid, Hd, b_Hd, ti, gmul)
        if r is not None:
            yield from self.route_gen(L, r[0], r[1], ti)

    def ln_core_gen(self, L, tt, b_tt, gmul="pool", xn_slot=None):
        P = self.P
        st, b_st = L.st_r.next()
        for i in range(2):
            P.op("dve", lambda e: e.bn_stats(out=st[:, i, :], in_=tt[:, i * 512:(i + 1) * 512]), reads=[b_tt], writes=[b_st])
        yield
        mv, b_mv = L.mv_r.next()
        P.op("dve", lambda e: e.bn_aggr(out=mv[:, 0:2], in_=st[:].rearrange("p a b -> p (a b)")), reads=[b_st], writes=[b_mv])
        yield
        P.op("dve", lambda e: e.tensor_scalar(out=mv[:, 2:3], in0=mv[:, 1:2], scalar1=LN_EPS, scalar2=None, op0=ALU.add),
             reads=[b_mv], writes=[b_mv])
        yield
        P.op("pool", lambda e: e.tensor_tensor(out=mv[:, 3:4], in0=mv[:, 2:3], in1=self.mhalf[:], op=ALU.pow),
             reads=[b_mv, self.b_const], writes=[b_mv])
        yield
        P.op("dve", lambda e: e.scalar_tensor_tensor(out=mv[:, 4:5], in0=mv[:, 0:1], scalar=-1.0, in1=mv[:, 3:4],
                                                     op0=ALU.mult, op1=ALU.mult), reads=[b_mv], writes=[b_mv])
        yield
        xn, b_xn = xn_slot if xn_slot is not None else L.xn_r.next()
        P.op("act", lambda e: e.activation(out=xn[:], in_=tt[:], func=AF.Identity, bias=mv[:, 4:5], scale=mv[:, 3:4]),
             reads=[b_tt, b_mv], writes=[b_xn])
        P.op("dve" if L.xn_psum else gmul, lambda e: e.tensor_tensor(out=xn[:], in0=xn[:], in1=L.g[:], op=ALU.mult),
             reads=[b_xn, L.b_gb], writes=[b_xn])
        h, b_h = L.h_r.next()
        P.op("dve", lambda e: e.tensor_tensor(out=h[:], in0=xn[:], in1=L.b[:], op=ALU.add),
             reads=[b_xn, L.b_gb], writes=[b_h])
        yield
        return h, b_h

    def route_gen(self, L, hb, b_hb, ti, defer=None):
        P, C = self.P, self.C
        BIG = 1.0e4
        pT, b_pT = L.pT_r.next()
        for c in range(8):
            P.op("pe", lambda e: e.transpose(pT[:, c, :], hb[:, c * 128:(c + 1) * 128], self.ident[:]),
                 reads=[b_hb, self.b_const], writes=[b_pT])
        hT, b_hT = L.hT_r.next()
        P.op("dve", lambda e: e.tensor_copy(out=hT[:], in_=pT[:]), reads=[b_pT], writes=[b_hT])
        pL, b_pL = L.pLC[:, 0:128], L.b_pLC
        for c in range(8):
            P.op("pe", lambda e: e.matmul(pL[:, 0:72], lhsT=hT[:, c, :], rhs=L.wr[:, c, :], start=(c == 0), stop=(c == 7)),
                 reads=[b_hT, L.b_wr], writes=[b_pL])
        rt, b_rt = L.rt_r.next()
        R = [b_rt]

        def dve(fn, reads=(), writes=()):
            P.op("dve", fn, reads=list(reads) + R, writes=list(writes) + R)
        P.op("dve", lambda e: e.tensor_tensor(out=rt[:, 0:72], in0=pL[:, 0:72], in1=L.br[:], op=ALU.add),
             reads=[b_pL, L.b_wr], writes=R)
        yield
        dve(lambda e: e.max(out=rt[:, 72:80], in_=rt[:, 0:8]))
        yield
        dve(lambda e: e.tensor_scalar(out=rt[:, 80:81], in0=rt[:, 72:73], scalar1=-1.0, scalar2=None, op0=ALU.mult))
        dve(lambda e: e.tensor_scalar(out=rt[:, 91:99], in0=rt[:, 0:8], scalar1=rt[:, 72:73], scalar2=None, op0=ALU.is_equal))
        yield
        P.op("act", lambda e: e.activation(out=rt[:, 81:89], in_=rt[:, 0:8], func=AF.Exp, bias=rt[:, 80:81], scale=1.0,
                                           accum_out=rt[:, 89:90]), reads=R, writes=R)
        dve(lambda e: e.tensor_scalar(out=rt[:, 99:107], in0=rt[:, 91:99], scalar1=BIG, scalar2=-BIG, op0=ALU.mult, op1=ALU.add))
        yield
        dve(lambda e: e.tensor_tensor(out=rt[:, 107:171].rearrange("p (g j) -> p g j", g=8),
                                      in0=rt[:, 8:72].rearrange("p (g j) -> p g j", g=8),
                                      in1=rt[:, 99:107].unsqueeze(2).to_broadcast([128, 8, 8]), op=ALU.add))
        yield
        dve(lambda e: e.max(out=rt[:, 171:179], in_=rt[:, 107:171]))
        yield
        i8, b_i8 = L.i8_r.next()
        dve(lambda e: e.max_index(out=i8[:], in_max=rt[:, 171:179], in_values=rt[:, 107:171]), writes=[b_i8])
        dve(lambda e: e.tensor_tensor(out=rt[:, 181:182], in0=rt[:, 172:173], in1=rt[:, 171:172], op=ALU.subtract))
        yield
        dve(lambda e: e.tensor_copy(out=rt[:, 179:181], in_=i8[:, 0:2]), reads=[b_i8])
        P.op("act", lambda e: e.activation(out=rt[:, 182:183], in_=rt[:, 181:182], func=AF.Exp), reads=R, writes=R)
        yield
        oh, b_oh = L.oh_r.next()
        for k in range(2):
            dve(lambda e: e.tensor_scalar(out=oh[:, k, :], in0=self.iota_e[:], scalar1=rt[:, 179 + k:180 + k], scalar2=None,
                                          op0=ALU.is_equal), reads=[self.b_const], writes=[b_oh])
        yield
        Mb, b_M = L.M_r.next()
        P.op("dve", lambda e: e.tensor_tensor(out=Mb[:], in0=oh[:, 0, :], in1=oh[:, 1, :], op=ALU.add),
             reads=[b_oh], writes=[b_M])
        yield
        pC, b_pC = L.pLC[:, 128:256].rearrange("p (a b) -> p a b", a=2), L.b_pLC
        P.op("pe", lambda e: e.matmul(pC[:, 0, :], lhsT=self.ustrict[:], rhs=Mb[:], start=True, stop=True),
             reads=[b_M, self.b_const], writes=[b_pC])
        P.op("pe", lambda e: e.matmul(pC[:, 1, :], lhsT=self.ones[:], rhs=Mb[:], start=True, stop=True),
             reads=[b_M, self.b_const], writes=[b_pC])
        pf, b_pf = L.pf_r.next()
        P.op("dve", lambda e: e.tensor_tensor(out=pf[:], in0=pC[:, 0, :], in1=self.cnt[:], op=ALU.add),
             reads=[b_pC, self.b_cnt], writes=[b_pf])
        P.op("dve", lambda e: e.tensor_tensor(out=self.cnt[:], in0=pC[:, 1, :], in1=self.cnt[:], op=ALU.add),
             reads=[b_pC, self.b_cnt], writes=[self.b_cnt])
        yield
        dve(lambda e: e.reciprocal(out=rt[:, 90:91], in_=rt[:, 89:90]))
        dve(lambda e: e.tensor_scalar(out=rt[:, 183:184], in0=rt[:, 182:183], scalar1=1.0, scalar2=None, op0=ALU.add))
        yield
        dve(lambda e: e.reciprocal(out=rt[:, 184:185], in_=rt[:, 183:184]))
        yield
        dve(lambda e: e.tensor_tensor(out=rt[:, 185:186], in0=rt[:, 184:185], in1=rt[:, 90:91], op=ALU.mult))
        yield
        dve(lambda e: e.tensor_tensor(out=rt[:, 186:187], in0=rt[:, 185:186], in1=rt[:, 182:183], op=ALU.mult))
        yield
        jk, b_jk = L.jk_r.next()
        for k in range(2):
            dve(lambda e: e.scalar_tensor_tensor(out=jk[:], in0=oh[:, k, :], scalar=1.0, in1=pf[:],
                                                 op0=ALU.mult, op1=ALU.mult, accum_out=rt[:, 187 + k:188 + k]),
                reads=[b_oh, b_pf], writes=[b_jk])
        yield
        dve(lambda e: e.tensor_scalar(out=rt[:, 189:191], in0=rt[:, 187:189], scalar1=float(C), scalar2=None, op0=ALU.is_lt))
        dve(lambda e: e.scalar_tensor_tensor(out=rt[:, 191:193], in0=rt[:, 179:181], scalar=float(C), in1=rt[:, 187:189],
                                             op0=ALU.mult, op1=ALU.add))
        yield
        dve(lambda e: e.tensor_tensor(out=rt[:, 193:195], in0=rt[:, 191:193], in1=rt[:, 189:191], op=ALU.mult))
        dve(lambda e: e.tensor_scalar(out=rt[:, 195:197], in0=rt[:, 189:191], scalar1=-1.0e6, scalar2=1.0e6,
                                      op0=ALU.mult, op1=ALU.add))
        yield
        dve(lambda e: e.tensor_copy(out=self.SL[:, ti, :], in_=rt[:, 193:195]), writes=[self.b_SL[ti]])
        dve(lambda e: e.tensor_tensor(out=rt[:, 197:199], in0=rt[:, 195:197], in1=rt[:, 191:193], op=ALU.add))
        yield
        ss, b_ss = L.ss_r.next()
        dve(lambda e: e.tensor_copy(out=ss[:], in_=rt[:, 197:199]), writes=[b_ss])
        dve(lambda e: e.tensor_tensor(out=self.GT[:, ti, :], in0=rt[:, 185:187], in1=rt[:, 189:191], op=ALU.mult),
            writes=[self.b_SL[ti]])
        yield
        def scatter():
            for k in range(2):
                P.dma("pool", lambda e: e.indirect_dma_start(out=self.XS[:, :],
                                                             out_offset=bass.IndirectOffsetOnAxis(ap=ss[:, k:k + 1], axis=0),
                                                             in_=hb[:, :], in_offset=None,
                                                             bounds_check=self.bound_reg, oob_is_err=False),
                      reads=[b_hb, b_ss])
        if defer is None:
            scatter()
        else:
            defer.append(scatter)

    def post_attn(self):
        P, S = self.P, self.S
        wo = P.sb("wo", [128, NH, D], BF16); b_wo = Buf("wo")
        P.dma("pool", lambda e: e.dma_start(out=wo[:], in_=self.a_w_out.ap().rearrange("(h p) f -> p h f", p=128)),
              writes=[b_wo])
        L = self.ln_alloc(0, 0, True, nbuf=4, xn_psum=1, nhb=8)
        bgr = []
        ot_r = Ring(P, "ot4", 2, [128, NH, 512], BF16)
        x_r = Ring(P, "xres", 3, [128, D], F32)
        mix_r = Ring(P, "mix", 2, [128, D], F32, psum=True)
        prev = []
        for s in range(self.nseq):
            for g in range(S // 512):
                ot, b_ot = ot_r.next()
                P.dma("sp", lambda e: e.dma_start(out=ot[:], in_=self.OT[s, :, :, g * 512:(g + 1) * 512].rearrange("h p t -> p h t")),
                      reads=[self.b_OT[s]], writes=[b_ot])
                cur = []
                lgens = []
                for tl in range(4):
                    ti = (s * S + g * 512 + tl * 128) // 128
                    xr, b_xr = x_r.next()
                    P.dma("sp", lambda e: e.dma_start(out=xr[:], in_=self.x[ti * 128:(ti + 1) * 128, :]), writes=[b_xr])
                    mix, b_mix = mix_r.next()
                    for hf in range(2):
                        for h in range(NH):
                            P.op("pe", lambda e: e.matmul(mix[:, hf * 512:(hf + 1) * 512], lhsT=ot[:, h, tl * 128:(tl + 1) * 128],
                                                          rhs=wo[:, h, hf * 512:(hf + 1) * 512], start=(h == 0), stop=(h == NH - 1)),
                                 reads=[b_ot, b_wo], writes=[b_mix])

                    def lgen(mix=mix, b_mix=b_mix, xr=xr, b_xr=b_xr, ti=ti):
                        r = yield from self.ln_part_gen(L, mix[:], b_mix, xr[:], b_xr, self.H1, self.b_H["H1"], ti)
                        cur.append((r[0], r[1], ti))
                    gen = lgen()
                    next(gen)
                    lgens.append(gen)
                    if len(lgens) == 2:
                        self.run_fg_bg(lgens, bgr)
                        lgens = []
                self.run_gens(bgr)
                bgr = [self.route_gen(L, hb, b_hb, ti) for hb, b_hb, ti in cur]
        self.run_gens(bgr)

    def moe(self, layer):
        P, C = self.P, self.C
        nb = C // 128
        wg_r = Ring(P, "wg", 2, [128, 8, HID], BF16)
        wu_r = Ring(P, "wu", 2, [128, 8, HID], BF16)
        wd_r = Ring(P, "wd", 2, [128, 4, D], BF16)
        xs_r = Ring(P, "xs", 2, [128, nb, D], BF16)
        pT_r = Ring(P, "mpT", 2, [128, 8, 128], BF16, psum=True)
        xT_r = Ring(P, "mxT", 2, [128, 8, C], BF16)
        pG_r = Ring(P, "pG", 2, [128, 512], F32, psum=True)
        pU_r = Ring(P, "pU", 2, [128, 512], F32, psum=True)
        pY_r = Ring(P, "pY", 2, [128, 512], F32, psum=True)
        sg_r = Ring(P, "sg", 2, [128, C], F32)
        hT_r = Ring(P, "hT", 2, [128, 4, C], BF16)
        y_r = Ring(P, "y", 3, [128, D], BF16)
        loaded = {}
        wd2_bufs = {id(b): Buf("wd2") for _, b in wd_r.slots}

        def load(e):
            wg, b_wg = wg_r.next(); wu, b_wu = wu_r.next(); wd, b_wd0 = wd_r.next(); xs, b_xs = xs_r.next()
            b_wd = [b_wd0, wd2_bufs[id(b_wd0)]]
            P.dma("sp", lambda en: en.dma_start(out=xs[:], in_=self.XS[e * C:(e + 1) * C, :].rearrange("(b p) d -> p b d", p=128)),
                  reads=[self.b_XS], writes=[b_xs])
            P.dma("pool", lambda en: en.dma_start(out=wg[:], in_=self.m_wg[layer, e].rearrange("(c p) f -> p c f", p=128)),
                  writes=[b_wg])
            P.dma("pool", lambda en: en.dma_start(out=wu[:], in_=self.m_wu[layer, e].rearrange("(c p) f -> p c f", p=128)),
                  writes=[b_wu])
            for hf in range(2):
                P.dma("pool", lambda en: en.dma_start(out=wd[:, :, hf * 512:(hf + 1) * 512],
                                                      in_=self.m_wd[layer, e, :, hf * 512:(hf + 1) * 512].rearrange("(c p) f -> p c f", p=128)),
                      writes=[b_wd[hf]])
            loaded[e] = (wg, b_wg, wu, b_wu, wd, b_wd, xs, b_xs)

        load(0)
        ev = 0
        for e in range(NE):
            if e + 1 < NE:
                load(e + 1)
            wg, b_wg, wu, b_wu, wd, b_wd, xs, b_xs = loaded.pop(e)
            xT, b_xT = xT_r.next()
            for b in range(nb):
                pT, b_pT = pT_r.next()
                for c in range(8):
                    P.op("pe", lambda en: en.transpose(pT[:, c, :], xs[:, b, c * 128:(c + 1) * 128], self.ident[:]),
                         reads=[b_xs, self.b_const], writes=[b_pT])
                if ev % 2 == 0:
                    P.op("act", lambda en: en.activation(out=xT[:, :, b * 128:(b + 1) * 128], in_=pT[:], func=AF.Copy),
                         reads=[b_pT], writes=[b_xT])
                else:
                    P.op("dve", lambda en: en.tensor_copy(out=xT[:, :, b * 128:(b + 1) * 128], in_=pT[:]),
                         reads=[b_pT], writes=[b_xT])
                ev += 1
            hT, b_hT = hT_r.next()
            for hc in range(4):
                pG, b_pG = pG_r.next(); pU, b_pU = pU_r.next()
                for c in range(8):
                    P.op("pe", lambda en: en.matmul(pG[:, 0:C], lhsT=wg[:, c, hc * 128:(hc + 1) * 128], rhs=xT[:, c, :],
                                                    start=(c == 0), stop=(c == 7)), reads=[b_wg, b_xT], writes=[b_pG])
                for c in range(8):
                    P.op("pe", lambda en: en.matmul(pU[:, 0:C], lhsT=wu[:, c, hc * 128:(hc + 1) * 128], rhs=xT[:, c, :],
                                                    start=(c == 0), stop=(c == 7)), reads=[b_wu, b_xT], writes=[b_pU])
                sg, b_sg = sg_r.next()
                P.op("act", lambda en: en.activation(out=sg[:], in_=pG[:, 0:C], func=AF.Silu), reads=[b_pG], writes=[b_sg])
                P.op("dve", lambda en: en.tensor_tensor(out=hT[:, hc, :], in0=sg[:], in1=pU[:, 0:C], op=ALU.mult),
                     reads=[b_sg, b_pU], writes=[b_hT])
            for b in range(nb):
                y, b_y = y_r.next()
                for hf in range(2):
                    pY, b_pY = pY_r.next()
                    for hc in range(4):
                        P.op("pe", lambda en: en.matmul(pY[:], lhsT=hT[:, hc, b * 128:(b + 1) * 128],
                                                        rhs=wd[:, hc, hf * 512:(hf + 1) * 512], start=(hc == 0), stop=(hc == 3)),
                             reads=[b_hT] + b_wd, writes=[b_pY])
                    if ev % 2 == 0:
                        P.op("act", lambda en: en.activation(out=y[:, hf * 512:(hf + 1) * 512], in_=pY[:], func=AF.Copy),
                             reads=[b_pY], writes=[b_y])
                    else:
                        P.op("dve", lambda en: en.tensor_copy(out=y[:, hf * 512:(hf + 1) * 512], in_=pY[:]),
                             reads=[b_pY], writes=[b_y])
                    ev += 1
                r0 = e * C + b * 128
                P.dma("sp", lambda en: en.dma_start(out=self.YS[r0:r0 + 128, :], in_=y[:]), reads=[b_y])

    def combine(self, layer, Hin, b_Hin, Hout, b_Hout, after_prefetch=None):
        P = self.P
        L = self.ln_alloc(layer, 1, False, nbuf=3, xn_psum=3)
        PF = 3
        G = 3
        y0_r = Ring(P, "y0", PF + G, [128, D], BF16)
        y1_r = Ring(P, "y1", PF + G, [128, D], BF16)
        hi_r = Ring(P, "hin", PF + G, [128, D], F32)
        loads = {}

        def issue(ti):
            ys = []
            for k, r in enumerate((y0_r, y1_r)):
                y, b_y = r.next()
                P.dma("pool", lambda e: e.indirect_dma_start(out=y[:, :], out_offset=None, in_=self.YS[:, :],
                                                             in_offset=bass.IndirectOffsetOnAxis(ap=self.SL[:, ti, k:k + 1], axis=0)),
                      reads=[self.b_YS, self.b_SL[ti]], writes=[b_y])
                ys.append((y, b_y))
            hi, b_hi = hi_r.next()
            P.dma("sp", lambda e: e.dma_start(out=hi[:], in_=Hin[ti * 128:(ti + 1) * 128, :]), reads=[b_Hin], writes=[b_hi])
            loads[ti] = (ys, hi, b_hi)

        def tile_gen(ti):
            ys, hi, b_hi = loads.pop(ti)
            u, b_u = L.xn_r.next()
            P.op("act", lambda e: e.activation(out=u[:], in_=hi[:], func=AF.Copy, scale=DN_ALPHA), reads=[b_hi], writes=[b_u])
            yield
            P.op("dve", lambda e: e.scalar_tensor_tensor(out=u[:], in0=ys[0][0][:], scalar=self.GT[:, ti, 0:1], in1=u[:],
                                                         op0=ALU.mult, op1=ALU.add),
                 reads=[ys[0][1], self.b_SL[ti], b_u], writes=[b_u])
            yield
            tt, b_tt = L.tt_r.next()
            P.op("dve", lambda e: e.scalar_tensor_tensor(out=tt[:], in0=ys[1][0][:], scalar=self.GT[:, ti, 1:2], in1=u[:],
                                                         op0=ALU.mult, op1=ALU.add),
                 reads=[ys[1][1], self.b_SL[ti], b_u], writes=[b_tt])
            yield
            h, b_h = yield from self.ln_core_gen(L, tt, b_tt, "dve", xn_slot=(u, b_u))
            P.dma("sp", lambda e: e.dma_start(out=Hout[ti * 128:(ti + 1) * 128, :], in_=h[:]), reads=[b_h])

        NTL = self.NTL
        for ti in range(min(PF, NTL)):
            issue(ti)
        if after_prefetch is not None:
            after_prefetch()
        for t0 in range(0, NTL, G):
            tiles = list(range(t0, min(NTL, t0 + G)))
            for ti in tiles:
                if ti + PF < NTL:
                    issue(ti + PF)
            self.run_gens([tile_gen(ti) for ti in tiles])

    def gmlp_setup(self):
        P, nc = self.P, self.nc
        bc = Buf("gconst")
        wo = P.sb("gwo", [128, 24, D], BF16)
        bu = P.sb("gbu", [128, 24], F32)
        lgb = P.sb("glgb", [128, SGH], BF16)
        sel32 = P.sb("gsel32", [128, 128], BF16)
        P.op("pool", lambda e: e.memset(sel32[:], 0.0), writes=[bc])
        P.op("pool", lambda e: e.memset(sel32[32:33, :], 1.0), reads=[bc], writes=[bc])
        wTb = P.sb("gwTb", [128, 8, 128], BF16)
        L4 = P.sb("gL4", [128, SGH], BF16)
        P.op("pool", lambda e: e.memset(L4[:], 0.0), writes=[bc])
        bv = L4[32:33, :]
        P.dma("pool", lambda e: e.dma_start(out=bv, in_=self.s_b_in[0:1, SGH:2 * SGH]), reads=[bc], writes=[bc])
        R4 = P.sb("gR4", [128, 8, 128], BF16)
        P.op("pool", lambda e: e.memset(R4[:], 0.0), writes=[bc])
        snap_sb = (nc.sbuf_base, nc.sbuf_top, nc.psum_base, nc.psum_top)
        identf = P.sb("gidf", [128, 128], F32)
        P.op("dve", lambda e: e.tensor_copy(out=identf[:], in_=self.ident[:]), reads=[self.b_const], writes=[bc])
        onesf = P.sb("gonesf", [128, 1], F32)
        P.op("dve", lambda e: e.memset(onesf[:], 1.0), writes=[bc])
        wnat = P.sb("gwnat", [128, 8, 128], F32)
        P.dma("sp", lambda e: e.dma_start(out=wnat[:], in_=self.s_w_s.ap().rearrange("g t s -> t g s")), writes=[bc])
        for g in range(8):
            P.op("pool", lambda e: e.affine_select(out=wnat[:, g, :], in_=wnat[:, g, :], pattern=[[-1, 128]],
                                                    compare_op=ALU.is_ge, fill=self.zero_reg, base=0, channel_multiplier=1),
                 reads=[bc], writes=[bc])
        wTf = P.sb("gwTf", [128, 8, 128], F32)
        pS = P.ps("gpS", [128, 512], F32); b_pS = Buf("gpS")
        rowf = P.sb("growf", [1, 2, SGH], F32)
        rowb = P.sb("growb", [1, 2, SGH], BF16)
        wsf = P.sb("gwsf", [1, 8, 128], F32)
        bsf = P.sb("gbsf", [1, 2, 8, 128], F32)
        row2 = P.sb("grow2", [1, 3, 8, 128], BF16)
        for g in range(8):
            P.op("pe", lambda e: e.transpose(pS[:, 0:128], wnat[:, g, :], identf[:]), reads=[bc], writes=[b_pS])
            P.op("dve", lambda e: e.tensor_copy(out=wTf[:, g, :], in_=pS[:, 0:128]), reads=[b_pS], writes=[bc])
            P.op("act", lambda e: e.activation(out=wTb[:, g, :], in_=pS[:, 0:128], func=AF.Copy), reads=[b_pS], writes=[bc])
            P.op("pe", lambda e: e.matmul(pS[0:1, 128:256], lhsT=onesf[:], rhs=wTf[:, g, :], start=True, stop=True),
                 reads=[bc], writes=[b_pS])
            P.op("dve", lambda e: e.tensor_copy(out=wsf[0:1, g, :], in_=pS[0:1, 128:256]), reads=[b_pS], writes=[bc])
        P.op("dve", lambda e: e.tensor_copy(out=row2[:, 0, :, :], in_=wsf[:]), reads=[bc], writes=[bc])
        P.dma("sp", lambda e: e.dma_start(out=rowf[:, 0, :], in_=self.s_ln_b[0:1, :]), writes=[bc])
        P.op("dve", lambda e: e.tensor_copy(out=rowb[:, 0, :], in_=rowf[:, 0, :]), reads=[bc], writes=[bc])
        P.op("dve", lambda e: e.tensor_copy(out=rowf[:, 1, :], in_=rowb[:, 0, :]), reads=[bc], writes=[bc])
        P.op("dve", lambda e: e.tensor_tensor(out=rowb[:, 1, :], in0=rowf[:, 0, :], in1=rowf[:, 1, :], op=ALU.subtract),
             reads=[bc], writes=[bc])
        P.dma("sp", lambda e: e.dma_start(out=bsf[:, 0, :, :], in_=self.s_b_s.ap().rearrange("(o g) t -> o g t", o=1)), writes=[bc])
        P.op("dve", lambda e: e.tensor_copy(out=row2[:, 1, :, :], in_=bsf[:, 0, :, :]), reads=[bc], writes=[bc])
        P.op("dve", lambda e: e.tensor_copy(out=bsf[:, 1, :, :], in_=row2[:, 1, :, :]), reads=[bc], writes=[bc])
        P.op("dve", lambda e: e.tensor_tensor(out=row2[:, 2, :, :], in0=bsf[:, 0, :, :], in1=bsf[:, 1, :, :], op=ALU.subtract),
             reads=[bc], writes=[bc])
        P.op("dve", lambda e: e.memset(L4[0:4, :], 1.0), reads=[bc], writes=[bc])
        P.dma("sp", lambda e: e.dma_start(out=L4[0:1, :], in_=rowb[:, 0, :]), reads=[bc], writes=[bc])
        P.dma("sp", lambda e: e.dma_start(out=L4[1:2, :], in_=rowb[:, 1, :]), reads=[bc], writes=[bc])
        P.dma("sp", lambda e: e.dma_start(out=R4[0:1, :, :], in_=row2[:, 0, :, :]), reads=[bc], writes=[bc])
        P.dma("sp", lambda e: e.dma_start(out=R4[1:2, :, :], in_=row2[:, 0, :, :]), reads=[bc], writes=[bc])
        P.dma("sp", lambda e: e.dma_start(out=R4[2:3, :, :], in_=row2[:, 1, :, :]), reads=[bc], writes=[bc])
        P.dma("sp", lambda e: e.dma_start(out=R4[3:4, :, :], in_=row2[:, 2, :, :]), reads=[bc], writes=[bc])

        P.barrier()
        nc.sbuf_base, nc.sbuf_top, nc.psum_base, nc.psum_top = snap_sb
        self.G = dict(bc=bc, wo=wo, bu=bu, bv=bv, lgb=lgb, sel32=sel32, wTb=wTb, L4=L4, R4=R4)

    def gmlp_load_big(self):
        P, nc, G = self.P, self.nc, self.G
        wo, bu, lgb = G["wo"], G["bu"], G["lgb"]
        for q in range(6):
            P.dma("pool", lambda e: e.dma_start(out=wo[:, q * 4:(q + 1) * 4, :],
                                                in_=self.s_w_out[q * 512:(q + 1) * 512, :].rearrange("(c p) f -> p c f", p=128)))
        with nc.allow_non_contiguous_dma(reason="one-time per-partition bias columns"):
            P.dma("sp", lambda e: e.dma_start(out=bu[:], in_=self.s_b_in[0, 0:SGH].rearrange("(c p) -> p c", p=128)))
        for hf in range(2):
            P.dma("pool", lambda e: e.dma_start(out=lgb[:, hf * 1536:(hf + 1) * 1536],
                                                in_=self.s_ln_g[0, hf * 1536:(hf + 1) * 1536].partition_broadcast(128)))

    def gmlp(self):
        P, nc, G = self.P, self.nc, self.G
        NSUP = self.NT // 512
        bc, wo, bu, bv, lgb, sel32, wTb, L4, R4 = (G[k] for k in ("bc", "wo", "bu", "bv", "lgb", "sel32", "wTb", "L4", "R4"))
        pS = P.ps("gpS", [128, 512], F32); b_pS = Buf("gpS")
        L = self.ln_alloc(1, 0, True, share_pT=True, nbuf=2, nhb=4)
        hb_r = Ring(P, "ghb", 4, [128, D], BF16)
        hT, b_hT = P.sb("ghT", [128, 8, 512], BF16), Buf("ghT")
        uT, b_uT = P.sb("guT", [128, 24, 512], BF16), [Buf("uT%d" % i) for i in range(24)]
        wp_r = Ring(P, "gwp", 3, [128, 8, 512], BF16)
        pUV_r = Ring(P, "gpUV", 2, [128, 512], F32, psum=True)
        pR2 = P.ps("gpR2", [128, 512], F32)
        pR_slots = [(pS, b_pS), (pR2, Buf("gpR2"))]
        mix_r = Ring(P, "gmix", 1, [128, D], F32, psum=True)
        vgb = P.sb("gvgb", [128, 4, SGH], BF16); b_vgb = [Buf("vgb%d" % i) for i in range(4)]
        st = P.sb("gst", [128, 4, 6, 6], F32); b_st = [Buf("gst%d" % i) for i in range(4)]
        mv_r = Ring(P, "gmv", 4, [128, 8], F32)
        res_r = Ring(P, "gres", 2, [128, D], F32)

        pieces = []
        for sp in range(NSUP):
            for pv in range(6):
                pieces.append(SGH + pv * 512)
            for pu in range(6):
                pieces.append(pu * 512)
        wq = []
        nxt = [0]

        def prefetch(n):
            while nxt[0] < len(pieces) and len(wq) < n:
                c0 = pieces[nxt[0]]
                wp, b_wp = wp_r.next()
                P.dma("pool", lambda e: e.dma_start(out=wp[:], in_=self.s_w_in[:, c0:c0 + 512].rearrange("(c p) f -> p c f", p=128)),
                      writes=[b_wp])
                wq.append((wp, b_wp))
                nxt[0] += 1

        def take():
            prefetch(1)
            w = wq.pop(0)
            prefetch(2)
            return w

        hbq = []

        def load_hb(sp):
            for tl in range(4):
                ti = sp * 4 + tl
                hb, b_hb = hb_r.next()
                P.dma("pool", lambda e: e.dma_start(out=hb[:], in_=self.H2[ti * 128:(ti + 1) * 128, :]),
                      reads=[self.b_H["H2"]], writes=[b_hb])
                hbq.append((hb, b_hb))

        def stage_A(sp):
            for tl in range(4):
                hb, b_hb = hbq.pop(0)
                pT, b_pT = L.pT_r.next()
                for c in range(8):
                    P.op("pe", lambda e: e.transpose(pT[:, c, :], hb[:, c * 128:(c + 1) * 128], self.ident[:]),
                         reads=[b_hb, self.b_const], writes=[b_pT])
                P.op("dve", lambda e: e.tensor_copy(out=hT[:, :, tl * 128:(tl + 1) * 128], in_=pT[:]), reads=[b_pT], writes=[b_hT])
            for pv in range(6):
                wp, b_wp = take()
                for tl in range(4):
                    pV, b_pV = pUV_r.next()
                    for c in range(8):
                        P.op("pe", lambda e: e.matmul(pV[:], lhsT=hT[:, c, tl * 128:(tl + 1) * 128], rhs=wp[:, c, :],
                                                      start=(c == 0), stop=False), reads=[b_wp, b_hT], writes=[b_pV])
                    P.op("pe", lambda e: e.matmul(pV[:], lhsT=sel32[:], rhs=L4[:, pv * 512:(pv + 1) * 512],
                                                  start=False, stop=True), reads=[bc], writes=[b_pV])
                    P.op("act", lambda e: e.activation(out=vgb[:, tl, pv * 512:(pv + 1) * 512], in_=pV[:], func=AF.Gelu_apprx_tanh),
                         reads=[b_pV], writes=[b_vgb[tl]])
                    P.op("dve", lambda e: e.bn_stats(out=st[:, tl, pv, :], in_=vgb[:, tl, pv * 512:(pv + 1) * 512]),
                         reads=[b_vgb[tl]], writes=[b_st[tl]])
                    tick()
        def stage_A_tail(sp):
            for tl in range(4):
                mv, b_mv = mv_r.next()
                P.op("dve", lambda e: e.bn_aggr(out=mv[:, 0:2], in_=st[:, tl, :, :].rearrange("p a b -> p (a b)")),
                     reads=[b_st[tl]], writes=[b_mv])
                P.op("dve", lambda e: e.tensor_scalar(out=mv[:, 2:3], in0=mv[:, 1:2], scalar1=LN_EPS, scalar2=None, op0=ALU.add),
                     reads=[b_mv], writes=[b_mv])
                P.op("pool", lambda e: e.tensor_tensor(out=mv[:, 3:4], in0=mv[:, 2:3], in1=self.mhalf[:], op=ALU.pow),
                     reads=[b_mv, self.b_const], writes=[b_mv])
                P.op("dve", lambda e: e.tensor_scalar(out=vgb[:, tl, :], in0=vgb[:, tl, :], scalar1=mv[:, 0:1], scalar2=mv[:, 3:4],
                                                      op0=ALU.subtract, op1=ALU.mult), reads=[b_vgb[tl], b_mv], writes=[b_vgb[tl]])
                P.op("dve", lambda e: e.tensor_tensor(out=vgb[:, tl, :], in0=vgb[:, tl, :], in1=lgb[:], op=ALU.mult),
                     reads=[b_vgb[tl], bc], writes=[b_vgb[tl]])

        def stage_B(sp):
            for pu in range(6):
                wp, b_wp = take()
                for f4 in range(4):
                    fc = pu * 4 + f4
                    pU, b_pU = pUV_r.next()
                    for c in range(8):
                        P.op("pe", lambda e: e.matmul(pU[:], lhsT=wp[:, c, f4 * 128:(f4 + 1) * 128], rhs=hT[:, c, :],
                                                      start=(c == 0), stop=(c == 7)), reads=[b_wp, b_hT], writes=[b_pU])
                    P.op("act", lambda e: e.activation(out=uT[:, fc, :], in_=pU[:], func=AF.Gelu_apprx_tanh,
                                                       bias=bu[:, fc:fc + 1], scale=1.0), reads=[b_pU, bc], writes=[b_uT[fc]])
                if late_ln:
                    finish_ln(*late_ln.pop(0))
                elif tail_pending:
                    stage_A_tail(tail_pending.pop(0))

        def stage_C(sp):
            for fc in range(24):
                g = fc // 3
                pR, b_pR = pR_slots[fc % 2]
                P.op("pe", lambda e: e.matmul(pR[:], lhsT=L4[:, fc * 128:(fc + 1) * 128],
                                              rhs=R4[:, g, :].unsqueeze(1).to_broadcast([128, 4, 128]),
                                              start=True, stop=False), reads=[bc], writes=[b_pR])
                for tl in range(4):
                    P.op("pe", lambda e: e.matmul(pR[:, tl * 128:(tl + 1) * 128], lhsT=vgb[:, tl, fc * 128:(fc + 1) * 128], rhs=wTb[:, g, :],
                                                  start=False, stop=(tl == 3)), reads=[b_vgb[tl], bc], writes=[b_pR])
                P.op("dve", lambda e: e.tensor_tensor(out=uT[:, fc, :], in0=pR[:], in1=uT[:, fc, :], op=ALU.mult),
                     reads=[b_pR, b_uT[fc]], writes=[b_uT[fc]])
                tick()

        pend_route = []
        late_ln = []
        tail_pending = []

        def finish_ln(gen, ti):
            try:
                while True:
                    next(gen)
            except StopIteration as stop:
                hb, b_hb = stop.value
            pend_route.append((hb, b_hb, ti))

        def stage_Dproj(sp):
            prev_gen = None
            for tl in range(4):
                ti = sp * 4 + tl
                res, b_res = res_r.next()
                P.dma("sp", lambda e: e.dma_start(out=res[:], in_=self.H2[ti * 128:(ti + 1) * 128, :]),
                      reads=[self.b_H["H2"]], writes=[b_res])
                mix, b_mix = mix_r.next()
                for hf in range(2):
                    for fc in range(24):
                        P.op("pe", lambda e: e.matmul(mix[:, hf * 512:(hf + 1) * 512], lhsT=uT[:, fc, tl * 128:(tl + 1) * 128],
                                                      rhs=wo[:, fc, hf * 512:(hf + 1) * 512], start=(fc == 0), stop=(fc == 23)),
                             reads=[b_uT[fc], bc], writes=[b_mix])
                gen = self.ln_part_gen(L, mix[:], b_mix, res[:], b_res, self.H3, self.b_H["H3"], ti, gmul="dve")
                next(gen)
                if tl < 3 and prev_gen is not None:
                    finish_ln(*prev_gen)
                    prev_gen = None
                if prev_gen is not None:
                    late_ln.append(prev_gen)
                prev_gen = (gen, ti)
            late_ln.append(prev_gen)

        bg = []
        scat = []

        def tick():
            if not bg and pend_route:
                for _ in range(min(2, len(pend_route))):
                    hb, b_hb, ti = pend_route.pop(0)
                    bg.append(self.route_gen(L, hb, b_hb, ti, defer=scat))
            for g_ in list(bg):
                try:
                    next(g_)
                except StopIteration:
                    bg.remove(g_)

        def stage_Droute():
            while bg or pend_route:
                tick()
            while scat:
                scat.pop(0)()

        load_hb(0)
        prefetch(2)
        stage_A(0)
        stage_A_tail(0)
        stage_B(0)
        for sp in range(NSUP):
            if sp + 1 < NSUP:
                load_hb(sp + 1)
            stage_C(sp)
            if sp + 1 < NSUP:
                stage_A(sp + 1)
            stage_Droute()
            stage_Dproj(sp)
            if sp + 1 < NSUP:
                tail_pending.append(sp + 1)
                stage_B(sp + 1)
            while late_ln:
                finish_ln(*late_ln.pop(0))
            while tail_pending:
                stage_A_tail(tail_pending.pop(0))
        stage_Droute()

    def build(self, upto=99):
        self.setup()
        self.setup_route()
        if upto >= 4:
            self.gmlp_setup()
        if upto >= 1:
            self.phase(self._attn_layer)
        if upto >= 2:
            self.phase(self.moe, 0)
        if upto >= 3:
            self.phase(self.combine, 0, self.H1, self.b_H["H1"], self.H2, self.b_H["H2"],
                       after_prefetch=(self.gmlp_load_big if upto >= 4 else None))
        if upto >= 4:
            self.phase(self.gmlp)
        if upto >= 5:
            self.phase(self.moe, 1)
        if upto >= 6:
            self.phase(self.combine, 1, self.H3, self.b_H["H3"], self.out, self.b_H["out"])
        self.P.finish()
        return self.nc

    def _attn_layer(self):
        self.setup_attn()
        self.phase(self.proj_qkv)
        self.phase(self.attn)
        self.phase(self.post_attn)


def _t5_bucket_np(n):
    n = np.maximum(n, 0)
    nf = np.maximum(n, 1).astype(np.float32)
    large = 16 + (np.log(nf / np.float32(16)) / np.float32(math.log(128 / 16)) * np.float32(16)).astype(np.int32)
    large = np.minimum(large, 31)
    return np.where(n < 16, n, large)


def prep_shared(inp):
    f = lambda a: np.ascontiguousarray(np.asarray(a, dtype=np.float32))
    rel = f(inp["rel_bias"])
    k = np.arange(128)[:, None]
    q = np.arange(128)[None, :]
    bt = np.stack([rel[_t5_bucket_np(q - k)], rel[_t5_bucket_np(128 + q - k)]], 0)
    bt = np.ascontiguousarray(np.transpose(bt, (0, 3, 1, 2)))
    sh = {
        "bt": bt, "cfar": f(rel[31:32, :]),
        "attn_w_in": f(inp["attn_w_in"][0]), "attn_w_out": f(inp["attn_w_out"][0]),
        "attn_lambda": f(np.asarray(inp["attn_lambda"][0]).reshape(1, 256)), "attn_subln_g": f(inp["attn_subln_g"]),
        "sg_w_in": f(inp["sg_w_in"][0]), "sg_b_in": f(inp["sg_b_in"]), "sg_ln_g": f(inp["sg_ln_g"]), "sg_ln_b": f(inp["sg_ln_b"]),
        "sg_w_s": f(inp["sg_w_s"][0]), "sg_b_s": f(inp["sg_b_s"][0]), "sg_w_out": f(inp["sg_w_out"][0]),
        "moe_wr": f(np.concatenate([np.asarray(inp["moe_w_group"]), np.asarray(inp["moe_w_router"])], axis=-1)),
        "moe_br": f(np.concatenate([np.asarray(inp["moe_b_group"]), np.asarray(inp["moe_b_router"])], axis=-1)),
        "moe_w_gate": f(inp["moe_w_gate"]), "moe_w_up": f(inp["moe_w_up"]), "moe_w_down": f(inp["moe_w_down"]),
        "ln_g": f(inp["ln_g"]), "ln_b": f(inp["ln_b"]),
    }
    return sh


_CACHE = {}


def kernel(**inputs):
    sh = prep_shared(inputs)
    x = np.ascontiguousarray(np.asarray(inputs["x"], dtype=np.float32))
    B, S, _ = x.shape
    nseq = B // N_CORES
    if "nc" not in _CACHE:
        k = K(nseq=nseq, S=S, C=384)
        _CACHE["nc"] = k.build()
    nc = _CACHE["nc"]
    in_maps = []
    for c in range(N_CORES):
        m = dict(sh)
        m["x"] = x[c * nseq:(c + 1) * nseq].reshape(nseq * S, D)
        in_maps.append(m)
    res = run_bass_kernel_spmd(nc, in_maps, core_ids=list(range(N_CORES)))
    out = np.concatenate([np.asarray(r["out"]).reshape(nseq, S, D) for r in res.results], axis=0)
    return out.astype(np.float32, copy=False)
```
